# Optimizing a Trainium2 kernel written in Bass

```python
import math
import jax, jax.numpy as jnp
from jax import lax
import numpy as np

D_MODEL = 2048
BATCH = 4
SEQ = 2048
DEPTH = 1

D_MIX = D_MODEL
HEAD_DIM = 128
ATTN_WIDTH = D_MIX // 2
N_Q_HEADS = ATTN_WIDTH // HEAD_DIM
N_KV_HEADS = 2
KV_GROUP = N_Q_HEADS // N_KV_HEADS
HGRN_WIDTH = D_MIX - ATTN_WIDTH
HGRN_EXPAND = 128
N_HGRN_HEADS = HGRN_WIDTH // HGRN_EXPAND
HGRN_CHUNK = 64
Q_BLOCK = 128
GRID_W = 64
ROPE_THETA = 10000.0
ROPE_AXIS_DIM = HEAD_DIM // 2
N_EXPERTS = 32
TOP_K = 4
D_EXPERT = D_MODEL
SWIGLU_LIMIT = 7.0
SWIGLU_ALPHA = 1.702
EXPERT_BLOCK = 128
NORM_EPS = 1e-6
DEEPNORM_ALPHA = (2 * DEPTH) ** 0.25
DEEPNORM_BETA = (8 * DEPTH) ** -0.25
PROJ_SIZES = (ATTN_WIDTH, N_KV_HEADS * HEAD_DIM, N_KV_HEADS * HEAD_DIM,
              HGRN_WIDTH, HGRN_WIDTH, HGRN_WIDTH, HGRN_WIDTH, HGRN_WIDTH)
PROJ_WIDTH = sum(PROJ_SIZES)
PROJ_OFFSETS = tuple(int(o) for o in np.cumsum(PROJ_SIZES)[:-1])

kernel_name = "hymba_attn_hgrn2_gptoss_moe_deepnorm_adaln"


def layer_norm(x, g=None, b=None):
    xf = x.astype(jnp.float32)
    mu = jnp.mean(xf, axis=-1, keepdims=True)
    var = jnp.mean(jnp.square(xf - mu), axis=-1, keepdims=True)
    y = (xf - mu) * lax.rsqrt(var + NORM_EPS)
    if g is not None:
        y = y * g.astype(jnp.float32) + b.astype(jnp.float32)
    return y.astype(x.dtype)


def rms_norm(x, w):
    xf = x.astype(jnp.float32)
    y = xf * lax.rsqrt(jnp.mean(jnp.square(xf), axis=-1, keepdims=True) + NORM_EPS)
    return (y * w.astype(jnp.float32)).astype(x.dtype)


def modulate(h, shift, scale):
    return h * (1.0 + scale[:, None, :]) + shift[:, None, :]


def axial_angles(seq_len):
    rows = seq_len // GRID_W
    t = jnp.arange(seq_len, dtype=jnp.int32)
    row = (t // GRID_W - rows // 2).astype(jnp.float32)
    col = (t % GRID_W - GRID_W // 2).astype(jnp.float32)
    inv_freq = ROPE_THETA ** (-jnp.arange(0, ROPE_AXIS_DIM, 2, dtype=jnp.float32) / ROPE_AXIS_DIM)
    return row[:, None] * inv_freq[None, :], col[:, None] * inv_freq[None, :]


def rotate(xh, ang):
    c = jnp.cos(ang)[:, None, :]
    s = jnp.sin(ang)[:, None, :]
    x1, x2 = jnp.split(xh, 2, axis=-1)
    return jnp.concatenate([x1 * c - x2 * s, x2 * c + x1 * s], axis=-1)


def apply_axial_rope(x, ang_row, ang_col):
    xf = x.astype(jnp.float32)
    out = jnp.concatenate([rotate(xf[..., :ROPE_AXIS_DIM], ang_row),
                           rotate(xf[..., ROPE_AXIS_DIM:], ang_col)], axis=-1)
    return out.astype(x.dtype)


def block_attention(q, k, v):
    B, HKV, G, S, D = q.shape
    n_blocks = S // Q_BLOCK
    scale = 1.0 / math.sqrt(D)
    qb = q.reshape(B, HKV, G, n_blocks, Q_BLOCK, D).transpose(3, 0, 1, 2, 4, 5)

    def one_block(q_blk):
        s = jnp.einsum('bkgqd,bksd->bkgqs', q_blk, k, preferred_element_type=jnp.float32) * scale
        p = jax.nn.softmax(s, axis=-1)
        return jnp.einsum('bkgqs,bksd->bkgqd', p.astype(v.dtype), v)

    ob = lax.map(one_block, qb)
    return ob.transpose(1, 2, 3, 0, 4, 5).reshape(B, HKV * G, S, D)


def gla_chunked(q, k, v, log_f):
    B, H, S, DK = q.shape
    DV = v.shape[-1]
    nc = S // HGRN_CHUNK
    q, k, v, log_f = (t.reshape(B, H, nc, HGRN_CHUNK, t.shape[-1]) for t in (q, k, v, log_f))
    b = jnp.cumsum(log_f, axis=3)
    b_last = b[:, :, :, -1:, :]
    q_dec = q * jnp.exp(b)
    scores = jnp.einsum('bhnid,bhnjd->bhnij', q_dec, k * jnp.exp(-b))
    mask = jnp.tril(jnp.ones((HGRN_CHUNK, HGRN_CHUNK), dtype=bool))
    intra = jnp.einsum('bhnij,bhnje->bhnie', jnp.where(mask, scores, 0.0), v)
    u = jnp.einsum('bhncd,bhnce->bhnde', k * jnp.exp(b_last - b), v)
    decay = jnp.exp(b_last[:, :, :, 0, :])

    def step(state, inp):
        dec, uu = inp
        return dec[..., None] * state + uu, state

    _, s_prev = lax.scan(step, jnp.zeros((B, H, DK, DV), jnp.float32),
                         (jnp.moveaxis(decay, 2, 0), jnp.moveaxis(u, 2, 0)))
    s_prev = jnp.moveaxis(s_prev, 0, 2)
    inter = jnp.einsum('bhncd,bhnde->bhnce', q_dec, s_prev)
    return (intra + inter).reshape(B, H, S, DV)


def token_mixer(h, w_in, q_norm_w, k_norm_w, attn_norm_w, hgrn_lb, hgrn_norm_w, w_out, layer_idx):
    B, S, _ = h.shape
    proj = jnp.einsum('bsd,dp->bsp', h, w_in)
    q, k, v, q_r, f_fw, f_bw, i_in, g_out = jnp.split(proj, PROJ_OFFSETS, axis=-1)

    q = rms_norm(q.reshape(B, S, N_Q_HEADS, HEAD_DIM), q_norm_w)
    k = rms_norm(k.reshape(B, S, N_KV_HEADS, HEAD_DIM), k_norm_w)
    v = v.reshape(B, S, N_KV_HEADS, HEAD_DIM)
    ang_row, ang_col = axial_angles(S)
    q = apply_axial_rope(q, ang_row, ang_col)
    k = apply_axial_rope(k, ang_row, ang_col)
    q = q.transpose(0, 2, 1, 3).reshape(B, N_KV_HEADS, KV_GROUP, S, HEAD_DIM)
    o_attn = block_attention(q, k.transpose(0, 2, 1, 3), v.transpose(0, 2, 1, 3))
    o_attn = rms_norm(o_attn, attn_norm_w.reshape(N_Q_HEADS, 1, HEAD_DIM))
    o_attn = o_attn.transpose(0, 2, 1, 3).reshape(B, S, ATTN_WIDTH)

    def to_heads(t):
        return t.reshape(B, S, N_HGRN_HEADS, HGRN_EXPAND).transpose(0, 2, 1, 3).astype(jnp.float32)

    q_r = to_heads(jax.nn.silu(q_r))
    v_r = to_heads(i_in)
    lb = jnp.cumsum(jax.nn.softmax(hgrn_lb.astype(jnp.float32), axis=1), axis=1)[:, layer_idx]
    lb = lb.reshape(2, N_HGRN_HEADS, 1, HGRN_EXPAND)

    def gates(f_logit, lb_d):
        fg = lb_d + (1.0 - lb_d) * jax.nn.sigmoid(to_heads(f_logit))
        return 1.0 - fg, jnp.log(fg)

    k_fw, lf_fw = gates(f_fw, lb[0])
    k_bw, lf_bw = gates(f_bw, lb[1])
    flip = lambda t: jnp.flip(t, axis=2)
    o_r = gla_chunked(q_r, k_fw, v_r, lf_fw) + flip(
        gla_chunked(flip(q_r), flip(k_bw), flip(v_r), flip(lf_bw)))
    o_r = rms_norm(o_r, hgrn_norm_w.reshape(N_HGRN_HEADS, 1, HGRN_EXPAND))
    o_r = o_r.transpose(0, 2, 1, 3).reshape(B, S, HGRN_WIDTH) * jax.nn.silu(g_out.astype(jnp.float32))

    mixed = jnp.concatenate([o_attn.astype(h.dtype), o_r.astype(h.dtype)], axis=-1)
    return jnp.einsum('bsm,md->bsd', mixed, w_out)


def clamped_swiglu(hid):
    x_glu, x_lin = jnp.split(hid, 2, axis=-1)
    x_glu = jnp.minimum(x_glu, SWIGLU_LIMIT)
    x_lin = jnp.clip(x_lin, -SWIGLU_LIMIT, SWIGLU_LIMIT)
    return x_glu * jax.nn.sigmoid(SWIGLU_ALPHA * x_glu) * (x_lin + 1.0)


def moe_ffn(h, w_router, b_router, w1, b1, w2, b2):
    B, S, D = h.shape
    N = B * S
    A = N * TOP_K
    xf = h.reshape(N, D)
    logits = (xf @ w_router + b_router).astype(jnp.float32)
    top_v, top_i = lax.top_k(logits, TOP_K)
    gates = jax.nn.softmax(top_v, axis=-1)

    eid = top_i.reshape(A)
    tok = jnp.arange(A, dtype=jnp.int32) // TOP_K
    order = jnp.argsort(eid)
    e_s, tok_s, g_s = eid[order], tok[order], gates.reshape(A)[order]
    counts = jnp.zeros((N_EXPERTS,), jnp.int32).at[eid].add(1)
    padded = (counts + EXPERT_BLOCK - 1) // EXPERT_BLOCK * EXPERT_BLOCK
    start = jnp.cumsum(counts) - counts
    p_end = jnp.cumsum(padded)
    p_start = p_end - padded
    dest = p_start[e_s] + (jnp.arange(A, dtype=jnp.int32) - start[e_s])
    P = A + N_EXPERTS * EXPERT_BLOCK
    n_blocks = P // EXPERT_BLOCK
    buf_tok = jnp.full((P,), N, jnp.int32).at[dest].set(tok_s)
    buf_gate = jnp.zeros((P,), jnp.float32).at[dest].set(g_s)
    blk_start = jnp.arange(n_blocks, dtype=jnp.int32) * EXPERT_BLOCK
    blk_exp = jnp.minimum(jnp.sum(blk_start[:, None] >= p_end[None, :], axis=1), N_EXPERTS - 1)

    x_pad = jnp.concatenate([xf, jnp.zeros((1, D), xf.dtype)], axis=0)
    xb = x_pad[buf_tok].reshape(n_blocks, EXPERT_BLOCK, D)

    def expert_block(args):
        x_blk, e = args
        hid = x_blk @ w1[e] + b1[e]
        return clamped_swiglu(hid) @ w2[e] + b2[e]

    yb = lax.map(expert_block, (xb, blk_exp)).reshape(P, D)
    yb = yb * buf_gate[:, None].astype(yb.dtype)
    out = jnp.zeros((N + 1, D), yb.dtype).at[buf_tok].add(yb)[:N]
    return out.reshape(B, S, D)


def setup_inputs(seed: int = 0) -> dict:
    key = jax.random.key(seed)
    ks = jax.random.split(key, 21)
    f32 = jnp.float32

    def nrm(k, shape, s):
        return s * jax.random.normal(k, shape, f32)

    return {
        "x": nrm(ks[0], (BATCH, SEQ, D_MODEL), 1.0),
        "c": nrm(ks[1], (BATCH, D_MODEL), 1.0),
        "w_ada": nrm(ks[2], (DEPTH, D_MODEL, 6 * D_MODEL), D_MODEL ** -0.5),
        "b_ada": nrm(ks[3], (DEPTH, 6 * D_MODEL), 0.02),
        "w_in": nrm(ks[4], (DEPTH, D_MODEL, PROJ_WIDTH), D_MODEL ** -0.5),
        "q_norm_w": 1.0 + nrm(ks[5], (DEPTH, HEAD_DIM), 0.02),
        "k_norm_w": 1.0 + nrm(ks[6], (DEPTH, HEAD_DIM), 0.02),
        "attn_norm_w": 1.0 + nrm(ks[7], (DEPTH, ATTN_WIDTH), 0.02),
        "hgrn_lb": 1.0 + nrm(ks[8], (2, DEPTH + 1, HGRN_WIDTH), 0.1),
        "hgrn_norm_w": 1.0 + nrm(ks[9], (DEPTH, HGRN_WIDTH), 0.02),
        "w_out": nrm(ks[10], (DEPTH, D_MIX, D_MODEL), DEEPNORM_BETA * D_MIX ** -0.5),
        "ln1_g": 1.0 + nrm(ks[11], (DEPTH, D_MODEL), 0.02),
        "ln1_b": nrm(ks[12], (DEPTH, D_MODEL), 0.02),
        "w_router": nrm(ks[13], (DEPTH, D_MODEL, N_EXPERTS), D_MODEL ** -0.5),
        "b_router": nrm(ks[14], (DEPTH, N_EXPERTS), 0.01),
        "w_exp_in": nrm(ks[15], (DEPTH, N_EXPERTS, D_MODEL, 2 * D_EXPERT), D_MODEL ** -0.5),
        "b_exp_in": nrm(ks[16], (DEPTH, N_EXPERTS, 2 * D_EXPERT), 0.02),
        "w_exp_out": nrm(ks[17], (DEPTH, N_EXPERTS, D_EXPERT, D_MODEL), DEEPNORM_BETA * D_EXPERT ** -0.5),
        "b_exp_out": nrm(ks[18], (DEPTH, N_EXPERTS, D_MODEL), 0.02),
        "ln2_g": 1.0 + nrm(ks[19], (DEPTH, D_MODEL), 0.02),
        "ln2_b": nrm(ks[20], (DEPTH, D_MODEL), 0.02),
    }


def reference(x, c, w_ada, b_ada, w_in, q_norm_w, k_norm_w, attn_norm_w, hgrn_lb, hgrn_norm_w,
              w_out, ln1_g, ln1_b, w_router, b_router, w_exp_in, b_exp_in, w_exp_out, b_exp_out,
              ln2_g, ln2_b):
    c_act = jax.nn.silu(c)
    for l in range(DEPTH):
        mod = c_act @ w_ada[l] + b_ada[l]
        sh1, sc1, g1, sh2, sc2, g2 = jnp.split(mod, 6, axis=-1)
        h = modulate(layer_norm(x), sh1, sc1)
        y = token_mixer(h, w_in[l], q_norm_w[l], k_norm_w[l], attn_norm_w[l], hgrn_lb,
                        hgrn_norm_w[l], w_out[l], l)
        x = layer_norm(DEEPNORM_ALPHA * x + g1[:, None, :] * y, ln1_g[l], ln1_b[l])
        h = modulate(layer_norm(x), sh2, sc2)
        y = moe_ffn(h, w_router[l], b_router[l], w_exp_in[l], b_exp_in[l], w_exp_out[l], b_exp_out[l])
        x = layer_norm(DEEPNORM_ALPHA * x + g2[:, None, :] * y, ln2_g[l], ln2_b[l])
    return x
```

```python
import contextlib
import numpy as np
import concourse.bass as bass
import concourse.mybir as mybir
from concourse.bass_utils import run_bass_kernel_spmd

F32 = mybir.dt.float32
BF16 = mybir.dt.bfloat16
AF = mybir.ActivationFunctionType
ALU = mybir.AluOpType
AX = mybir.AxisListType

COMPUTE = ('pe', 'act', 'dve')
DMAQ = ('sp', 'pool')
ALLENG = COMPUTE + DMAQ
NDS = 8
SAME_ENGINE_SYNC = True


class Sched:
    _uid = 0

    def __init__(self, nc):
        self.nc = nc
        Sched._uid += 1
        self.ops = {e: [] for e in ALLENG}
        self.n = {e: 0 for e in ALLENG}
        self.lastw = {}
        self.readers = {}
        self.seen = {e: {} for e in ALLENG}

    @staticmethod
    def _tgt(eng, idx):
        if eng in DMAQ:
            return (eng, (idx - 1) % NDS), 16 * ((idx - 1) // NDS + 1)
        return (eng, 0), idx

    def op(self, eng, fn, reads=(), writes=()):
        deps = {}

        def add(t, v):
            if deps.get(t, 0) < v:
                deps[t] = v

        for k in reads:
            if k in self.lastw:
                add(*self.lastw[k])
        for k in writes:
            if k in self.lastw:
                add(*self.lastw[k])
            for t, v in self.readers.get(k, {}).items():
                add(t, v)
        idx = None
        if fn is not None:
            self.n[eng] += 1
            idx = self.n[eng]
            if eng in DMAQ and idx > NDS:
                add(*self._tgt(eng, idx - NDS))
        waits = []
        for t, v in deps.items():
            if t[0] == eng and eng == 'pe':
                continue
            if t[0] == eng and eng in COMPUTE and not SAME_ENGINE_SYNC:
                continue
            if self.seen[eng].get(t, 0) >= v:
                continue
            self.seen[eng][t] = v
            waits.append((t, v))
        self.ops[eng].append((waits, fn, idx))
        if fn is None:
            return
        me = self._tgt(eng, idx)
        for k in writes:
            self.lastw[k] = me
            self.readers[k] = {}
        for k in reads:
            if k not in writes:
                r = self.readers.setdefault(k, {})
                if r.get(me[0], 0) < me[1]:
                    r[me[0]] = me[1]

    def dma(self, q, out, in_, reads=(), writes=(), **kw):
        self.op(q, lambda e: e.dma_start(out=out, in_=in_, **kw), reads=reads, writes=writes)

    def alloc_sems(self, stack):
        nc = self.nc
        self.sems = {}
        for e in COMPUTE:
            self.sems[(e, 0)] = stack.enter_context(nc.semaphore(f"s_{e}"))
        for e in DMAQ:
            for s in range(NDS):
                self.sems[(e, s)] = stack.enter_context(nc.semaphore(f"s_{e}{s}"))

    def emit(self, final_wait_keys=()):
        nc = self.nc
        Sched._uid += 1
        self.op('sp', None, reads=list(final_wait_keys))
        finals = []
        for e in ALLENG:
            if self.n[e] == 0:
                continue
            if e in DMAQ:
                for s in range(min(NDS, self.n[e])):
                    last = ((self.n[e] - 1 - s) // NDS) * NDS + s + 1
                    finals.append(self._tgt(e, last))
            else:
                finals.append(self._tgt(e, self.n[e]))
        for e in ALLENG:
            w = []
            for (t, v) in finals:
                if self.seen[e].get(t, 0) < v and not (t[0] == e and e == 'pe'):
                    w.append((t, v))
                    self.seen[e][t] = v
            self.ops[e].append((w, None, None))
        sems = self.sems
        ops = self.ops
        self.ops = {e: [] for e in ALLENG}
        with nc.Block() as blk:
            def run(eng, h):
                for waits, fn, idx in ops[eng]:
                    for t, v in waits:
                        h.wait_ge(sems[t], v)
                    if fn is not None:
                        ins = fn(h)
                        t, _ = self._tgt(eng, idx)
                        ins.then_inc(sems[t], 16 if eng in DMAQ else 1)

            blk.tensor(lambda h: run('pe', h))
            blk.scalar(lambda h: run('act', h))
            blk.vector(lambda h: run('dve', h))
            blk.gpsimd(lambda h: run('pool', h))
            blk.sync(lambda h: run('sp', h))


P = 128
D = 2048
KT = 16
S_ALL = 2048
S_OWN = 1024
NT_ALL = 16
NT_OWN = 8
CW = 256
EPS = 1e-6


class Prog:
    def __init__(self, stages, debug=False, n_experts=32):
        self.NE = n_experts
        self.stages = stages
        self.debug = debug
        nc = self.nc = bass.Bass("TRN2", target_bir_lowering=False)
        di = lambda n, s: nc.dram_tensor(n, s, F32, kind="ExternalInput").ap()
        self.x = di("x_loc", [S_ALL, D])
        self.c_col = di("c_col", [P, KT])
        self.w_ada1 = di("w_ada1", [D, 2 * D])
        self.b_ada1 = di("b_ada1", [1, 2 * D])
        self.ident_d = di("ident", [P, P])
        self.w_qkv = di("w_qkv", [6, P, KT, CW])
        self.qkw = di("qkw", [2, P])
        self.cs = di("cs", [2, S_ALL, P])
        self.anw = di("anw", [1, 1024])
        self.y = nc.dram_tensor("y_out", [S_OWN, D], F32, kind="ExternalOutput").ap()
        self.w_hg = di("w_hg", [8, 3, P, KT, CW])
        self.lb_a = di("lb_a", [2, P, 16])
        self.hnw = di("hnw", [1, 1024])
        self.hmask = di("hmask", [2, P, P])
        self.rmask_d = di("rmask", [1, S_OWN])
        self.halfm_d = di("halfm", [P, 2])
        self.w_ada2 = di("w_ada2", [D, 3 * D])
        self.b_ada2 = di("b_ada2", [1, 3 * D])
        self.w_ada3 = di("w_ada3", [D, D])
        self.b_g2col = di("b_g2col", [P, KT])
        self.w_out_t = di("w_out_t", [8, P, KT, CW])
        self.ln1 = di("ln1", [2, D])
        self.w_r = di("w_r", [P, KT, 32])
        self.b_r = di("b_r", [1, 32])
        self.w1_r = di("w1_r", [self.NE, 16, P, KT, CW])
        self.w2_r = di("w2_r", [self.NE, 2, 4, P, 8, 512])
        self.b1_r = di("b1_r", [P, 32, 32])
        self.b2 = di("b2", [32, D])
        self.ln2 = di("ln2", [2, D])
        if debug:
            do = lambda n, s: nc.dram_tensor(n, s, F32, kind="ExternalOutput").ap()
            self.dbg_h = do("dbg_h", [S_ALL, D])
            self.dbg_q = do("dbg_q", [S_OWN, 1024])
            self.dbg_k = do("dbg_k", [S_ALL, 256])
            self.dbg_oa = do("dbg_oa", [S_OWN, 1024])
            self.dbg_mod = do("dbg_mod", [P, 2 * D])
            self.dbg_or = do("dbg_or", [S_OWN, 1024])
            self.dbg_os = do("dbg_os", [S_OWN, 1024])
            self.dbg_x1 = do("dbg_x1", [S_OWN, D])
            self.dbg_h2 = do("dbg_h2", [S_OWN, D])
            self.dbg_lg = do("dbg_lg", [S_OWN, 32])
            self.dbg_gt = do("dbg_gt", [32, S_OWN])
            self.dbg_acc = do("dbg_acc", [P, 8 * D])
            self.dbg_acc2 = do("dbg_acc2", [P, 8 * D])
        self.build()

    def mm_group(self, e, out, pairs):
        last = None
        n = len(pairs)
        for i, (l, r) in enumerate(pairs):
            last = e.matmul(out, l, r, start=(i == 0), stop=(i == n - 1))
        return last

    def build(self):
        nc = self.nc
        with contextlib.ExitStack() as top:
            sb = lambda n, s, d=F32: top.enter_context(nc.sbuf_tensor(n, s, d))
            self.S = Sched(nc)
            self.S.alloc_sems(top)
            self.ident = sb("ident_s", [P, P])
            self.eps_t = sb("eps_t", [P, 1])
            self.gatesT = sb("gatesT", [32, S_OWN])
            self.g2col = sb("g2col", [P, KT])
            self.R1 = sb("R1", [P, KT, S_ALL], BF16)
            self.hT = self.R1
            self.acc = self.R1[:].rearrange("p a b -> p (a b)").bitcast(F32).rearrange("p (t d) -> p t d", t=NT_OWN)
            self.R2 = sb("R2", [P, 16, S_OWN], BF16)
            self.mixedT = self.R2
            self.h2T = self.R2
            self.phase_A()
            if 'attn' in self.stages:
                self.phase_B1()
            if 'hgrn' in self.stages:
                self.phase_B2()
            if 'c' in self.stages:
                import os
                ccut = int(os.environ.get("C_CUT", "9"))
                self.phase_C1()
                with contextlib.ExitStack() as cs:
                    self.mod2 = cs.enter_context(nc.sbuf_tensor("mod2", [P, 3 * D], F32))
                    if ccut >= 2:
                        self.phase_C0()
                    if ccut >= 3:
                        self.phase_C2()
            if 'moe' in self.stages:
                self.phase_MoE()
                self.phase_final()

    def ada_part(self, S, st, w_ap, b_ap, ncols, dest, carep, plus_one_from):
        nc = self.nc
        wbuf = [st.enter_context(nc.sbuf_tensor(f"adaw{i}_{Sched._uid}", [P, KT, 512], BF16)) for i in range(2)]
        bb = [st.enter_context(nc.sbuf_tensor(f"adab{i}_{Sched._uid}", [P, 512], F32)) for i in range(2)]
        ps = [st.enter_context(nc.psum_tensor(f"adap{i}_{Sched._uid}", [P, 512], F32)) for i in range(2)]
        for c in range(ncols // 512):
            s = c % 2
            S.dma('pool', wbuf[s][:], w_ap[:, c * 512:(c + 1) * 512].rearrange("(kt p) n -> p kt n", p=P),
                  writes=[('adaw', s)])
            S.dma('sp', bb[s][:], b_ap[:, c * 512:(c + 1) * 512].partition_broadcast(P), writes=[('adab', s)])
            S.op('pe', lambda e, s=s: self.mm_group(e, ps[s][:], [(carep[:, kt, :], wbuf[s][:, kt, :]) for kt in range(KT)]),
                 reads=[('adaw', s), 'carep'], writes=[('adap', s)])
            d = dest[:, c * 512:(c + 1) * 512]
            if c * 512 >= plus_one_from:
                S.op('dve', lambda e, s=s, d=d: e.scalar_tensor_tensor(d, ps[s][:], 1.0, bb[s][:], ALU.add, ALU.add),
                     reads=[('adap', s), ('adab', s)], writes=[('mod', c)])
            else:
                S.op('dve', lambda e, s=s, d=d: e.tensor_tensor(d, ps[s][:], bb[s][:], ALU.add),
                     reads=[('adap', s), ('adab', s)], writes=[('mod', c)])

    def ln_stats(self, S, st_t, mv_t, src, key_src, tag):
        def f(e):
            last = None
            for j in range(4):
                last = e.bn_stats(st_t[:, j, :], src[:, j * 512:(j + 1) * 512])
            return last
        S.op('dve', f, reads=[key_src], writes=[(tag, 'st')])
        S.op('dve', lambda e: e.bn_aggr(mv_t[:, 0:2], st_t[:].rearrange("p a b -> p (a b)")),
             reads=[(tag, 'st')], writes=[(tag, 'mv')])
        S.op('act', lambda e: e.activation(mv_t[:, 2:3], mv_t[:, 1:2], AF.Sqrt, bias=self.eps_t[:, 0:1], scale=1.0),
             reads=[(tag, 'mv')], writes=[(tag, 'sd')])
        r, v, t = mv_t[:, 2:3], mv_t[:, 4:5], mv_t[:, 5:6]
        S.op('dve', lambda e: e.reciprocal(r, r), reads=[(tag, 'sd')], writes=[(tag, 'rs')])
        S.op('dve', lambda e: e.tensor_scalar(v, mv_t[:, 1:2], EPS, None, ALU.add), reads=[(tag, 'mv')], writes=[(tag, 'v')])
        for _ in range(1):
            S.op('dve', lambda e: e.tensor_tensor(t, r, r, ALU.mult), reads=[(tag, 'rs')], writes=[(tag, 't')])
            S.op('dve', lambda e: e.tensor_scalar(t, t, v, -0.5, ALU.mult, ALU.mult), reads=[(tag, 't'), (tag, 'v')], writes=[(tag, 't')])
            S.op('dve', lambda e: e.scalar_tensor_tensor(r, t, 1.5, r, ALU.add, ALU.mult), reads=[(tag, 't'), (tag, 'rs')], writes=[(tag, 'rs')])
        S.op('dve', lambda e: e.tensor_scalar(mv_t[:, 3:4], mv_t[:, 0:1], mv_t[:, 2:3], -1.0, ALU.mult, ALU.mult),
             reads=[(tag, 'rs'), (tag, 'mv')], writes=[(tag, 'nb')])

    def phase_A(self):
        nc = self.nc
        with contextlib.ExitStack() as st:
            sb = lambda n, s, d=F32: st.enter_context(nc.sbuf_tensor(n, s, d))
            S = self.S
            S.op('dve', lambda e: e.memset(self.eps_t[:], EPS), writes=['eps'])
            S.dma('sp', self.ident[:], self.ident_d, writes=['ident'])
            carep = self.make_carep(S, st)[0]
            mod1 = sb("mod1", [P, 2 * D])
            self.ada_part(S, st, self.w_ada1, self.b_ada1, 2 * D, mod1, carep, D)
            modkeys = [('mod', c) for c in range(8)]
            if self.debug:
                S.dma('sp', self.dbg_mod, mod1[:], reads=modkeys, writes=['dbg_mod'])
            xin = [sb(f"xin{i}", [P, D]) for i in range(2)]
            hn = [sb(f"hn{i}", [P, D]) for i in range(2)]
            stt = [sb(f"stt{i}", [P, 4, 6]) for i in range(2)]
            mv = [sb(f"mv{i}", [P, 6]) for i in range(2)]
            pT = [st.enter_context(nc.psum_tensor(f"pT{i}", [P, 8, P], F32)) for i in range(3)]
            npt = 0
            for t in range(NT_ALL):
                s = t % 2
                S.dma('sp', xin[s][:], self.x[t * P:(t + 1) * P, :], writes=[('xin', s)])
                self.ln_stats(S, stt[s], mv[s], xin[s], ('xin', s), ('lnA', s))
                S.op('act', lambda e, s=s: e.activation(hn[s][:], xin[s][:], AF.Identity, bias=mv[s][:, 3:4], scale=mv[s][:, 2:3]),
                     reads=[('xin', s), (('lnA', s), 'nb'), (('lnA', s), 'rs')], writes=[('hn', s)])
                S.op('dve', lambda e, s=s: e.tensor_tensor(hn[s][:], hn[s][:], mod1[:, D:2 * D], ALU.mult),
                     reads=[('hn', s)] + modkeys, writes=[('hn', s)])
                S.op('dve', lambda e, s=s: e.tensor_tensor(hn[s][:], hn[s][:], mod1[:, 0:D], ALU.add),
                     reads=[('hn', s)] + modkeys, writes=[('hn', s)])
                if self.debug:
                    S.dma('sp', self.dbg_h[t * P:(t + 1) * P, :], hn[s][:], reads=[('hn', s)], writes=[('dbg_h', t)])
                for hf in range(2):
                    pi = npt % 3
                    npt += 1

                    def tr(e, s=s, hf=hf, pi=pi):
                        last = None
                        for k8 in range(8):
                            kt = hf * 8 + k8
                            last = e.transpose(pT[pi][:, k8, :], hn[s][:, kt * P:(kt + 1) * P], self.ident[:])
                        return last
                    S.op('pe', tr, reads=[('hn', s), 'ident'], writes=[('pT', pi)])
                    S.op('act', lambda e, t=t, hf=hf, pi=pi: e.activation(self.hT[:, hf * 8:(hf + 1) * 8, t * P:(t + 1) * P], pT[pi][:], AF.Copy),
                         reads=[('pT', pi)], writes=[('hT', t, hf)])
            S.emit(final_wait_keys=[('dbg_h', t) for t in range(NT_ALL)] if self.debug else [])

    def phase_B1(self):
        nc = self.nc
        with contextlib.ExitStack() as st:
            sb = lambda n, s, d=F32: st.enter_context(nc.sbuf_tensor(n, s, d))
            qT = sb("qT", [P, 8, S_OWN], BF16)
            kT = sb("kT", [P, 2, S_ALL], BF16)
            v_aug = sb("v_aug", [P, NT_ALL, 2, 132], BF16)
            self.B1a(qT, kT, v_aug)
            self.B1b(qT, kT, v_aug)

    def B1a(self, qT, kT, v_aug):
        nc = self.nc
        with contextlib.ExitStack() as st:
            sb = lambda n, s, d=F32: st.enter_context(nc.sbuf_tensor(n, s, d))
            S = self.S
            S.op('dve', lambda e: e.memset(v_aug[:, :, :, 128:129], 1.0), writes=['vones'])
            nw = sb("nw", [P, 2, P])
            S.dma('sp', nw[:, 0, :], self.qkw[0:1, :].partition_broadcast(P), writes=['nw0'])
            S.dma('sp', nw[:, 1, :], self.qkw[1:2, :].partition_broadcast(P), writes=['nw1'])
            wb = [sb(f"wqkv{i}", [P, KT, CW], BF16) for i in range(3)]
            cst = [sb(f"cst{i}", [P, 2, P]) for i in range(2)]
            sq = [sb(f"sq{i}", [P, 2, P]) for i in range(2)]
            qn = [sb(f"qn{i}", [P, 2, P]) for i in range(2)]
            t1 = [sb(f"t1{i}", [P, 2, P]) for i in range(2)]
            t2 = [sb(f"t2{i}", [P, 2, P]) for i in range(2)]
            ss = [sb(f"ss{i}", [P, 4]) for i in range(2)]
            pq = [st.enter_context(nc.psum_tensor(f"pq{i}", [P, CW], F32)) for i in range(2)]
            ptr = [st.enter_context(nc.psum_tensor(f"ptr{i}", [P, 2, P], F32)) for i in range(2)]
            it = 0
            for j in range(6):
                ws = j % 3
                S.dma('pool', wb[ws][:], self.w_qkv[j], writes=[('wb', ws)], max_dma_last_dim=4096)
                ntiles = NT_OWN if j < 4 else NT_ALL
                for t in range(ntiles):
                    s = it % 2
                    it += 1
                    S.op('pe', lambda e, s=s, t=t, ws=ws: self.mm_group(
                        e, pq[s][:], [(self.hT[:, kt, t * P:(t + 1) * P], wb[ws][:, kt, :]) for kt in range(KT)]),
                        reads=[('wb', ws), ('hT', t, 0), ('hT', t, 1)], writes=[('pq', s)])
                    pq3 = pq[s][:].rearrange("p (h d) -> p h d", h=2)
                    if j == 5:
                        S.op('act', lambda e, t=t, pq3=pq3: e.activation(v_aug[:, t, :, 0:128], pq3, AF.Copy),
                             reads=[('pq', s)], writes=[('v', t)])
                        continue
                    wi = 0 if j < 4 else 1
                    S.dma('sp', cst[s][:], self.cs[:, t * P:(t + 1) * P, :].rearrange("c t d -> t c d"), writes=[('cst', s)])
                    S.op('act', lambda e, s=s: e.activation(sq[s][:].rearrange("p h d -> p (h d)"), pq[s][:], AF.Square),
                         reads=[('pq', s)], writes=[('sq', s)])
                    S.op('dve', lambda e, s=s: e.tensor_reduce(ss[s][:, 0:2], sq[s][:], AX.X, ALU.add),
                         reads=[('sq', s)], writes=[('ss', s)])
                    S.op('act', lambda e, s=s: e.activation(ss[s][:, 2:4], ss[s][:, 0:2], AF.Sqrt, bias=self.eps_t[:, 0:1], scale=1.0 / 128),
                         reads=[('ss', s)], writes=[('sd', s)])
                    S.op('dve', lambda e, s=s: e.reciprocal(ss[s][:, 2:4], ss[s][:, 2:4]), reads=[('sd', s)], writes=[('rs', s)])
                    S.op('dve', lambda e, s=s, pq3=pq3: e.tensor_tensor(qn[s][:], pq3, ss[s][:, 2:4].unsqueeze(2).to_broadcast([P, 2, P]), ALU.mult),
                         reads=[('pq', s), ('rs', s)], writes=[('qn', s)])
                    S.op('dve', lambda e, s=s, wi=wi: e.tensor_tensor(qn[s][:], qn[s][:], nw[:, wi:wi + 1, :].to_broadcast([P, 2, P]), ALU.mult),
                         reads=[('qn', s), 'nw0', 'nw1'], writes=[('qn', s)])
                    S.op('dve', lambda e, s=s: e.tensor_tensor(t1[s][:], qn[s][:], cst[s][:, 0:1, :].to_broadcast([P, 2, P]), ALU.mult),
                         reads=[('qn', s), ('cst', s)], writes=[('t1', s)])
                    v5 = lambda a: a.rearrange("p h (a f d) -> p h a f d", a=2, f=2)
                    for f in range(2):
                        S.op('dve', lambda e, s=s, f=f: e.tensor_tensor(
                            v5(t2[s][:])[:, :, :, f, :], v5(qn[s][:])[:, :, :, 1 - f, :],
                            cst[s][:, 1, :].rearrange("p (a f d) -> p a f d", a=2, f=2)[:, :, f, :].unsqueeze(1).to_broadcast([P, 2, 2, 32]),
                            ALU.mult), reads=[('qn', s), ('cst', s)], writes=[('t2', s, f)])
                    S.op('dve', lambda e, s=s: e.tensor_tensor(t1[s][:], t1[s][:], t2[s][:], ALU.add),
                         reads=[('t1', s), ('t2', s, 0), ('t2', s, 1)], writes=[('t1', s)])
                    if self.debug:
                        dst = self.dbg_q[t * P:(t + 1) * P, j * CW:(j + 1) * CW] if j < 4 else self.dbg_k[t * P:(t + 1) * P, :]
                        S.dma('sp', dst, t1[s][:].rearrange("p h d -> p (h d)"), reads=[('t1', s)], writes=[('dbgqk', j, t)])

                    def tr(e, s=s):
                        e.transpose(ptr[s][:, 0, :], t1[s][:, 0, :], self.ident[:])
                        return e.transpose(ptr[s][:, 1, :], t1[s][:, 1, :], self.ident[:])
                    S.op('pe', tr, reads=[('t1', s)], writes=[('ptr', s)])
                    dstT = qT[:, 2 * j:2 * j + 2, t * P:(t + 1) * P] if j < 4 else kT[:, :, t * P:(t + 1) * P]
                    S.op('act', lambda e, s=s, dstT=dstT: e.activation(dstT, ptr[s][:], AF.Copy),
                         reads=[('ptr', s)], writes=[('qkT', j, t)])
            fk = [('dbgqk', j, t) for j in range(5) for t in range(NT_OWN if j < 4 else NT_ALL)] if self.debug else []
            S.emit(final_wait_keys=fk)

    def B1b(self, qT, kT, v_aug):
        nc = self.nc
        with contextlib.ExitStack() as st:
            sb = lambda n, s, d=F32: st.enter_context(nc.sbuf_tensor(n, s, d))
            S = self.S
            anw = sb("anw_rep", [P, 1024])
            S.dma('sp', anw[:], self.anw.partition_broadcast(P), writes=['anw'])
            zt = sb("zt", [P, 1024])
            S.op('dve', lambda e: e.memset(zt[:], 0.0), writes=['zt'])
            dk = []
            for t in range(NT_OWN if 'moe' not in self.stages else 0):
                S.dma('sp', self.y[t * P:(t + 1) * P, 1024:2048], zt[:], reads=['zt'], writes=[('yz', t)])
                dk.append(('yz', t))
            E = [sb(f"E{i}", [P, NT_ALL, 512], BF16) for i in range(2)]
            o = [sb(f"o{i}", [P, P]) for i in range(2)]
            on = [sb(f"on{i}", [P, P]) for i in range(2)]
            junk = [sb(f"junk{i}", [P, P]) for i in range(2)]
            sc = [sb(f"sc{i}", [P, 4]) for i in range(2)]
            pS = [st.enter_context(nc.psum_tensor(f"pS{i}", [P, 512], F32)) for i in range(3)]
            pO = [st.enter_context(nc.psum_tensor(f"pO{i}", [P, 132], F32)) for i in range(2)]
            pX = [st.enter_context(nc.psum_tensor(f"pX{i}", [P, P], F32)) for i in range(2)]
            ns = 0
            no = 0
            for h in range(8):
                kvh = h // 4
                for qc in range(2):
                    es = (h * 2 + qc) % 2
                    for j in range(NT_ALL):
                        s = ns % 3
                        ns += 1
                        S.op('pe', lambda e, s=s, j=j, h=h, qc=qc, kvh=kvh: e.matmul(
                            pS[s][:], kT[:, kvh, j * P:(j + 1) * P], qT[:, h, qc * 512:(qc + 1) * 512], start=True, stop=True),
                            reads=[], writes=[('pS', s)])
                        S.op('act', lambda e, s=s, j=j, es=es: e.activation(E[es][:, j, :], pS[s][:], AF.Exp, scale=float(1.0 / np.sqrt(128.0))),
                             reads=[('pS', s)], writes=[('E', es, j)])
                    for qt in range(4):
                        s = no % 2
                        no += 1
                        tq = qc * 4 + qt
                        S.op('pe', lambda e, s=s, es=es, qt=qt, kvh=kvh: self.mm_group(
                            e, pO[s][:, 0:129], [(E[es][:, j, qt * P:(qt + 1) * P], v_aug[:, j, kvh, 0:129]) for j in range(NT_ALL)]),
                            reads=[('E', es, j) for j in range(NT_ALL)], writes=[('pO', s)])
                        S.op('dve', lambda e, s=s: e.reciprocal(sc[s][:, 0:1], pO[s][:, 128:129]), reads=[('pO', s)], writes=[('rden', s)])
                        S.op('dve', lambda e, s=s: e.tensor_scalar(o[s][:], pO[s][:, 0:128], sc[s][:, 0:1], None, ALU.mult),
                             reads=[('pO', s), ('rden', s)], writes=[('o', s)])
                        S.op('act', lambda e, s=s: e.activation(junk[s][:], o[s][:], AF.Square, accum_out=sc[s][:, 1:2]),
                             reads=[('o', s)], writes=[('oss', s), ('junk', s)])
                        S.op('act', lambda e, s=s: e.activation(sc[s][:, 2:3], sc[s][:, 1:2], AF.Sqrt, bias=self.eps_t[:, 0:1], scale=1.0 / 128),
                             reads=[('oss', s)], writes=[('osd', s)])
                        S.op('dve', lambda e, s=s: e.reciprocal(sc[s][:, 2:3], sc[s][:, 2:3]), reads=[('osd', s)], writes=[('ors', s)])
                        S.op('dve', lambda e, s=s, h=h: e.scalar_tensor_tensor(on[s][:], o[s][:], sc[s][:, 2:3], anw[:, h * P:(h + 1) * P], ALU.mult, ALU.mult),
                             reads=[('o', s), ('ors', s), 'anw'], writes=[('on', s)])
                        if 'moe' not in self.stages:
                            S.dma('sp', self.y[tq * P:(tq + 1) * P, h * P:(h + 1) * P], on[s][:], reads=[('on', s)], writes=[('yo', h, tq)])
                            dk.append(('yo', h, tq))
                        if self.debug:
                            S.dma('sp', self.dbg_oa[tq * P:(tq + 1) * P, h * P:(h + 1) * P], on[s][:], reads=[('on', s)], writes=[('dbgoa', h, tq)])
                            dk.append(('dbgoa', h, tq))
                        S.op('pe', lambda e, s=s: e.transpose(pX[s][:], on[s][:], self.ident[:]), reads=[('on', s)], writes=[('pX', s)])
                        S.op('act', lambda e, s=s, h=h, tq=tq: e.activation(self.mixedT[:, h, tq * P:(tq + 1) * P], pX[s][:], AF.Copy),
                             reads=[('pX', s)], writes=[('mixedT', h, tq)])
            S.emit(final_wait_keys=dk)

    def phase_B2(self):
        nc = self.nc
        hT = self.hT
        with contextlib.ExitStack() as st:
            sb = lambda n, s, d=F32: st.enter_context(nc.sbuf_tensor(n, s, d))
            ps = lambda n, s: st.enter_context(nc.psum_tensor(n, s, F32))
            S = self.S
            N = S_OWN
            NCH = 16
            lba = sb("lba", [P, 2, 16]); lb = sb("lb", [P, 16]); oml = sb("oml", [P, 16])
            S.dma('sp', lba[:], self.lb_a.rearrange("s p c -> p s c"), writes=['lba'])
            S.op('dve', lambda e: e.tensor_tensor(oml[:], lba[:, 0, :], lba[:, 1, :], ALU.subtract), reads=['lba'], writes=['oml'])
            S.op('act', lambda e: e.activation(lb[:], oml[:], AF.Sigmoid), reads=['oml'], writes=['lb'])
            S.op('dve', lambda e: e.tensor_scalar(oml[:], lb[:], -1.0, 1.0, ALU.mult, ALU.add), reads=['lb'], writes=['oml'])
            msk = sb("hmask_s", [P, 2, P])
            S.dma('sp', msk[:], self.hmask.rearrange("m j i -> j m i"), writes=['msk'])
            rmask = sb("rmask_s", [P, N])
            S.dma('sp', rmask[:], self.rmask_d.partition_broadcast(P), writes=['rmask'])
            hnw = [sb(f"hnw{i}", [P, P]) for i in range(2)]
            wb = [sb(f"whg{i}", [P, KT, CW], BF16) for i in range(2)]
            qr = sb("qr", [P, N]); sgA = sb("sgA", [P, N]); sgB = sb("sgB", [P, N])
            v_tm = sb("v_tm", [P, NT_ALL, P], BF16); sg_tm = sb("sg_tm", [64, NCH, P], BF16)
            X2 = sb("X2", [P, N]); X3 = sb("X3", [P, N]); X4 = sb("X4", [P, N]); X5 = sb("X5", [P, N])
            kdec = [sb(f"kdec{i}", [P, N], BF16) for i in range(2)]
            qdec = [sb(f"qdec{i}", [P, N], BF16) for i in range(2)]
            kend_tm = sb("kend_tm", [P, 2, NT_OWN, P], BF16)
            halfm = sb("halfm_s", [P, 2])
            S.dma('sp', halfm[:], self.halfm_d, writes=['halfm'])
            Sbf = [sb(f"Sbf{i}", [P, NCH, P], BF16) for i in range(2)]
            S32 = sb("S32", [P, P]); etot = sb("etot", [P, NCH])
            scm = [sb(f"scm{i}", [P, P], BF16) for i in range(2)]
            oh = sb("oh", [64, 2, P]); oh2 = sb("oh2", [64, 2, P]); junk = sb("hjunk", [64, P]); sc = sb("hsc", [64, 8])
            pP = [ps(f"pP{i}", [P, 512]) for i in range(2)]
            pV = ps("pV", [P, 4, P]); pTk = ps("pTk", [P, 4, P]); pU = ps("pU", [P, 4, P])
            pSc = ps("pSc", [P, 2, P]); pOh = ps("pOh", [64, 2, P]); pX = ps("pXh", [P, 2, 64])
            npp = [0]
            nwb = [0]
            dk = []

            def load_w(hh, c):
                s = nwb[0] % 2
                nwb[0] += 1
                S.dma('pool', wb[s][:], self.w_hg[hh, c], writes=[('whg', s)], max_dma_last_dim=4096)
                return s

            def proj_fm(ws, col0, tok0, ntok, func, dest, key):
                for c in range(ntok // 512):
                    s = npp[0] % 2
                    npp[0] += 1
                    S.op('pe', lambda e, s=s, c=c: self.mm_group(e, pP[s][:], [
                        (wb[ws][:, kt, col0:col0 + P], hT[:, kt, tok0 + c * 512:tok0 + (c + 1) * 512]) for kt in range(KT)]),
                        reads=[('whg', ws)], writes=[('pP', s)])
                    S.op('act', lambda e, s=s, c=c: e.activation(dest[:, c * 512:(c + 1) * 512], pP[s][:], func),
                         reads=[('pP', s)], writes=[(key, c)])

            def proj_tm(ws, col0, ntiles, func, dest, key):
                for g in range(ntiles // 4):
                    def f(e, g=g):
                        last = None
                        for q in range(4):
                            t = g * 4 + q
                            last = self.mm_group(e, pV[:, q, :], [(hT[:, kt, t * P:(t + 1) * P], wb[ws][:, kt, col0:col0 + P]) for kt in range(KT)])
                        return last
                    S.op('pe', f, reads=[('whg', ws)], writes=['pV'])
                    S.op('act', lambda e, g=g: e.activation(dest[:, g * 4:(g + 1) * 4, :], pV[:], func), reads=['pV'], writes=[(key, g)])

            import os
            cut = int(os.environ.get("HG_CUT", "9"))

            def gate_pass(hh, di, sg, sgkeys, own, first_state):
                if cut < 2:
                    return
                col = di * 8 + hh
                fg, kk, lf, b, bb = sg, X2, X3, X4, X5
                S.op('dve', lambda e: e.tensor_scalar(fg[:], sg[:], oml[:, col:col + 1], lb[:, col:col + 1], ALU.mult, ALU.add),
                     reads=sgkeys + ['oml', 'lb'], writes=['fg'])
                S.op('dve', lambda e: e.tensor_scalar(kk[:], fg[:], -1.0, 1.0, ALU.mult, ALU.add), reads=['fg'], writes=['X2'])
                S.op('act', lambda e: e.activation(lf[:], fg[:], AF.Ln), reads=['fg'], writes=['X3'])
                S.op('dve', lambda e: e.tensor_tensor_scan(b[:], rmask[:], lf[:], 0.0, ALU.mult, ALU.add), reads=['X3', 'rmask'], writes=['X4'])
                b3 = b[:].rearrange("p (c i) -> p c i", i=64)
                S.op('act', lambda e: e.activation(etot[:].unsqueeze(2), b3[:, :, 63:64], AF.Exp), reads=['X4'], writes=['etot'])
                if di == 0:
                    bbt, bbk = b, 'X4'
                else:
                    S.op('dve', lambda e: e.tensor_tensor(bb[:], lf[:], b[:], ALU.subtract), reads=['X3', 'X4'], writes=['X5'])
                    bb3 = bb[:].rearrange("p (c i) -> p c i", i=64)
                    S.op('dve', lambda e: e.tensor_tensor(bb3, bb3, b3[:, :, 63:64].to_broadcast([P, NCH, 64]), ALU.add), reads=['X5', 'X4'], writes=['X5'])
                    bbt, bbk = bb, 'X5'
                en = lf
                S.op('act', lambda e: e.activation(en[:], bbt[:], AF.Exp, scale=-1.0), reads=[bbk, 'X3'], writes=['X3'])
                S.op('dve', lambda e: e.tensor_tensor(kk[:], kk[:], en[:], ALU.mult), reads=['X2', 'X3'], writes=['X2'])
                if own:
                    S.op('act', lambda e: e.activation(kdec[di][:], kk[:], AF.Copy), reads=['X2'], writes=[('kdec', di)])
                    S.op('act', lambda e: e.activation(en[:], bbt[:], AF.Exp), reads=[bbk, 'X2'], writes=['X3'])
                    S.op('dve', lambda e: e.tensor_tensor(qdec[di][:], qr[:], en[:], ALU.mult), reads=['X3', ('qr', 0), ('qr', 1)], writes=[('qdec', di)])
                kend32 = b if di == 1 else bb
                kkey = 'X4' if di == 1 else 'X5'
                S.op('dve', lambda e: e.tensor_tensor(kend32[:].rearrange("p (c i) -> p c i", i=64), kk[:].rearrange("p (c i) -> p c i", i=64),
                                                      etot[:].unsqueeze(2).to_broadcast([P, NCH, 64]), ALU.mult),
                     reads=['X2', 'etot', 'X4', 'X5'], writes=[kkey])
                if cut < 3:
                    return
                for g in range(2):
                    def ftr(e, g=g):
                        last = None
                        for q in range(4):
                            t = g * 4 + q
                            last = e.transpose(pTk[:, q, :], kend32[:, t * P:(t + 1) * P], self.ident[:])
                        return last
                    S.op('pe', ftr, reads=[kkey], writes=['pTk'])
                    for hf in range(2):
                        S.op('act', lambda e, g=g, hf=hf: e.activation(kend_tm[:, hf, g * 4:(g + 1) * 4, :], pTk[:], AF.Copy, scale=halfm[:, hf:hf + 1]),
                             reads=['pTk', 'halfm'], writes=[('kend_tm', g, hf)])
                if cut < 4:
                    return
                tile0 = 0 if own else NT_OWN
                order = list(range(NCH)) if di == 0 else list(range(NCH - 1, -1, -1))
                started = not first_state
                for gi in range(4):
                    cs = order[gi * 4:(gi + 1) * 4]

                    def fu(e, cs=cs):
                        last = None
                        for q, c in enumerate(cs):
                            t, hf = c // 2, c % 2
                            last = e.matmul(pU[:, q, :], kend_tm[:, hf, t, :], v_tm[:, tile0 + t, :], start=True, stop=True)
                        return last
                    S.op('pe', fu, reads=[('kend_tm', g, hf) for g in range(2) for hf in range(2)] + [('v_tm', g) for g in range(4)], writes=['pU'])
                    for q, c in enumerate(cs):
                        if own:
                            if started:
                                S.op('act', lambda e, c=c: e.activation(Sbf[di][:, c, :], S32[:], AF.Copy), reads=['S32'], writes=[('Sbf', di, c)])
                            else:
                                S.op('dve', lambda e, c=c: e.memset(Sbf[di][:, c, :], 0.0), writes=[('Sbf', di, c)])
                        if started:
                            S.op('dve', lambda e, q=q, c=c: e.scalar_tensor_tensor(S32[:], S32[:], etot[:, c:c + 1], pU[:, q, :], ALU.mult, ALU.add),
                                 reads=['S32', 'pU', 'etot'], writes=['S32'])
                        else:
                            S.op('dve', lambda e, q=q: e.tensor_copy(S32[:], pU[:, q, :]), reads=['pU'], writes=['S32'])
                            started = True

            for hh in range(8):
                hs = hh % 2
                S.dma('sp', hnw[hs][:], self.hnw[:, hh * P:(hh + 1) * P].partition_broadcast(P), writes=[('hnw', hs)])
                wsA = load_w(hh, 0)
                proj_fm(wsA, 0, 0, N, AF.Silu, qr, 'qr')
                proj_fm(wsA, P, 0, N, AF.Sigmoid, sgA, 'sgA')
                wsB = load_w(hh, 1)
                proj_tm(wsB, P, NT_ALL, AF.Copy, v_tm, 'v_tm')
                proj_fm(wsB, 0, N, N, AF.Sigmoid, sgB, 'sgB')
                gate_pass(hh, 0, sgA, [('sgA', 0), ('sgA', 1)], True, True)
                gate_pass(hh, 1, sgB, [('sgB', 0), ('sgB', 1)], False, True)
                proj_fm(wsB, 0, 0, N, AF.Sigmoid, sgB, 'sgB')
                wsG = load_w(hh, 2)
                for g in range(4):
                    def fg_(e, g=g, wsG=wsG):
                        last = None
                        for q in range(4):
                            c = g * 4 + q
                            last = self.mm_group(e, pV[0:64, q, :], [(hT[:, kt, c * 64:(c + 1) * 64], wb[wsG][:, kt, 0:P]) for kt in range(KT)])
                        return last
                    S.op('pe', fg_, reads=[('whg', wsG)], writes=['pV'])
                    S.op('act', lambda e, g=g: e.activation(sg_tm[:, g * 4:(g + 1) * 4, :], pV[0:64, :, :], AF.Silu), reads=['pV'], writes=[('sg_tm', g)])
                gate_pass(hh, 1, sgB, [('sgB', 0), ('sgB', 1)], True, False)
                for t in range(NT_OWN if cut >= 5 else 0):
                    tsl = slice(t * P, (t + 1) * P)

                    def fsc(e, tsl=tsl):
                        e.matmul(pSc[:, 0, :], kdec[0][:, tsl], qdec[0][:, tsl], start=True, stop=True)
                        return e.matmul(pSc[:, 1, :], kdec[1][:, tsl], qdec[1][:, tsl], start=True, stop=True)
                    S.op('pe', fsc, reads=[('kdec', 0), ('kdec', 1), ('qdec', 0), ('qdec', 1)], writes=['pSc'])
                    for di in range(2):
                        S.op('dve', lambda e, di=di: e.tensor_tensor(scm[di][:], pSc[:, di, :], msk[:, di, :], ALU.mult),
                             reads=['pSc', 'msk'], writes=[('scm', di)])

                    def fo(e, t=t, tsl=tsl):
                        last = None
                        for hf in range(2):
                            c = 2 * t + hf
                            csl = slice(hf * 64, (hf + 1) * 64)
                            tk = slice(t * P + hf * 64, t * P + (hf + 1) * 64)
                            e.matmul(pOh[:, hf, :], scm[0][:, csl], v_tm[:, t, :], start=True, stop=False)
                            e.matmul(pOh[:, hf, :], scm[1][:, csl], v_tm[:, t, :], start=False, stop=False)
                            e.matmul(pOh[:, hf, :], qdec[0][:, tk], Sbf[0][:, c, :], start=False, stop=False)
                            last = e.matmul(pOh[:, hf, :], qdec[1][:, tk], Sbf[1][:, c, :], start=False, stop=True)
                        return last
                    S.op('pe', fo, reads=[('scm', 0), ('scm', 1), ('qdec', 0), ('qdec', 1)] + [('Sbf', di, c) for di in range(2) for c in (2 * t, 2 * t + 1)]
                         + [('v_tm', g) for g in range(4)], writes=['pOh'])
                    S.op('act', lambda e: e.activation(oh[:], pOh[:], AF.Copy), reads=['pOh'], writes=['oh'])
                    for hf in range(2):
                        S.op('act', lambda e, hf=hf: e.activation(junk[:], oh[:, hf, :], AF.Square, accum_out=sc[:, hf:hf + 1]), reads=['oh'], writes=[('hss', hf), 'hjunk'])
                    S.op('act', lambda e: e.activation(sc[:, 2:4], sc[:, 0:2], AF.Sqrt, bias=self.eps_t[0:64, 0:1], scale=1.0 / 128), reads=[('hss', 0), ('hss', 1)], writes=['hsd'])
                    S.op('dve', lambda e: e.reciprocal(sc[:, 2:4], sc[:, 2:4]), reads=['hsd'], writes=['hrs'])
                    for hf in range(2):
                        S.op('dve', lambda e, hs=hs, hf=hf: e.scalar_tensor_tensor(oh2[:, hf, :], oh[:, hf, :], sc[:, 2 + hf:3 + hf], hnw[hs][0:64, :], ALU.mult, ALU.mult),
                             reads=['oh', 'hrs', ('hnw', hs)], writes=[('oh2', hf)])
                    S.op('dve', lambda e, t=t: e.tensor_tensor(oh2[:], oh2[:], sg_tm[:, 2 * t:2 * t + 2, :], ALU.mult),
                         reads=[('oh2', 0), ('oh2', 1)] + [('sg_tm', g) for g in range(4)], writes=[('oh2', 0), ('oh2', 1)])
                    if self.debug:
                        for hf in range(2):
                            rs_ = slice(t * P + hf * 64, t * P + (hf + 1) * 64)
                            S.dma('sp', self.dbg_os[rs_, hh * P:(hh + 1) * P], oh[:, hf, :], reads=['oh'], writes=[('dbgos', hh, t, hf)])
                            S.dma('sp', self.dbg_or[rs_, hh * P:(hh + 1) * P], oh2[:, hf, :], reads=[('oh2', hf)], writes=[('dbgor', hh, t, hf)])
                            dk += [('dbgos', hh, t, hf), ('dbgor', hh, t, hf)]

                    def ftx(e):
                        e.transpose(pX[:, 0, :], oh2[:, 0, :], self.ident[0:64, 0:64])
                        return e.transpose(pX[:, 1, :], oh2[:, 1, :], self.ident[0:64, 0:64])
                    S.op('pe', ftx, reads=[('oh2', 0), ('oh2', 1)], writes=['pXh'])
                    S.op('act', lambda e, hh=hh, tsl=tsl: e.activation(self.mixedT[:, 8 + hh, tsl], pX[:].rearrange("p a b -> p (a b)"), AF.Copy),
                         reads=['pXh'], writes=[('mixedT', 8 + hh, tsl.start)])
            S.emit(final_wait_keys=dk)

    def make_carep(self, S, st):
        nc = self.nc
        u = Sched._uid
        ccol = st.enter_context(nc.sbuf_tensor(f"ccol{u}", [P, KT], F32))
        cact = st.enter_context(nc.sbuf_tensor(f"cact{u}", [P, KT], F32))
        cabf = st.enter_context(nc.sbuf_tensor(f"cabf{u}", [P, KT], BF16))
        carep = st.enter_context(nc.sbuf_tensor(f"carep{u}", [P, KT, P], BF16))
        S.dma('sp', ccol[:], self.c_col, writes=['ccol'])
        S.op('act', lambda e: e.activation(cact[:], ccol[:], AF.Silu), reads=['ccol'], writes=['cact'])
        S.op('dve', lambda e: e.tensor_copy(carep[:], cact[:].unsqueeze(2).to_broadcast([P, KT, P])), reads=['cact'], writes=['carep'])
        S.op('dve', lambda e: e.tensor_copy(cabf[:], cact[:]), reads=['cact'], writes=['cabf'])
        return carep, cabf

    def phase_C0(self):
        nc = self.nc
        with contextlib.ExitStack() as st:
            S = self.S
            carep, cabf = self.make_carep(S, st)
            self.ada_part(S, st, self.w_ada2, self.b_ada2, 3 * D, self.mod2, carep, 2 * D)
            wg = [st.enter_context(nc.sbuf_tensor(f"wg2_{i}", [P, KT, 512], BF16)) for i in range(2)]
            bg = st.enter_context(nc.sbuf_tensor("bg2", [P, KT], F32))
            pg = st.enter_context(nc.psum_tensor("pg2", [P, KT], F32))
            S.dma('sp', bg[:], self.b_g2col, writes=['bg2'])
            for c in range(4):
                s = c % 2
                S.dma('pool', wg[s][:], self.w_ada3[:, c * 512:(c + 1) * 512].rearrange("(kt p) n -> p kt n", p=P), writes=[('wg2', s)])

                def f(e, s=s, c=c):
                    last = None
                    for q in range(4):
                        dt = c * 4 + q
                        last = self.mm_group(e, pg[:, dt:dt + 1], [(wg[s][:, kt, q * P:(q + 1) * P], cabf[:, kt:kt + 1]) for kt in range(KT)])
                    return last
                S.op('pe', f, reads=[('wg2', s), 'cabf'], writes=['pg2'])
            S.op('dve', lambda e: e.tensor_tensor(self.g2col[:], pg[:], bg[:], ALU.add), reads=['pg2', 'bg2'], writes=['g2col'])
            S.emit()

    def phase_C1(self):
        nc = self.nc
        with contextlib.ExitStack() as st:
            S = self.S
            wb = [st.enter_context(nc.sbuf_tensor(f"wo{i}", [P, KT, CW], BF16)) for i in range(3)]
            py = [st.enter_context(nc.psum_tensor(f"py{i}", [P, CW], F32)) for i in range(2)]
            it = 0
            for dc in range(8):
                ws = dc % 3
                S.dma('pool', wb[ws][:], self.w_out_t[dc], writes=[('wo', ws)], max_dma_last_dim=4096)
                for t in range(NT_OWN):
                    s = it % 2
                    it += 1
                    S.op('pe', lambda e, s=s, t=t, ws=ws: self.mm_group(
                        e, py[s][:], [(self.mixedT[:, mt, t * P:(t + 1) * P], wb[ws][:, mt, :]) for mt in range(16)]),
                        reads=[('wo', ws)], writes=[('py', s)])
                    S.op('act', lambda e, s=s, t=t, dc=dc: e.activation(self.acc[:, t, dc * CW:(dc + 1) * CW], py[s][:], AF.Copy),
                         reads=[('py', s)], writes=[('acc', t, dc)])
            S.emit()

    def phase_C2(self):
        nc = self.nc
        ALPHA = float(2.0 ** 0.25)
        with contextlib.ExitStack() as st:
            sb = lambda n, s, d=F32: st.enter_context(nc.sbuf_tensor(n, s, d))
            S = self.S
            mod2 = self.mod2
            g1r, sh2r, sc2r = mod2[:, 0:D], mod2[:, D:2 * D], mod2[:, 2 * D:3 * D]
            ln1 = sb("ln1r", [P, 2, D])
            S.dma('sp', ln1[:, 0, :], self.ln1[0:1, :].partition_broadcast(P), writes=['ln1g'])
            S.dma('sp', ln1[:, 1, :], self.ln1[1:2, :].partition_broadcast(P), writes=['ln1b'])
            wr = sb("wr", [P, KT, 32]); br = sb("br", [P, 32])
            S.dma('sp', wr[:], self.w_r, writes=['wr'])
            S.dma('sp', br[:], self.b_r.partition_broadcast(P), writes=['br'])
            xin = sb("xin_c", [P, D]); tt = sb("tt_c", [P, D]); h2 = sb("h2_c", [P, D])
            h2T32 = xin[:].rearrange("p (k j) -> p k j", k=KT)
            stt = sb("stt_c", [P, 4, 6]); mv = sb("mv_c", [P, 6]); mv2 = sb("mv2_c", [P, 6])
            lg = sb("lg", [P, 32]); top8 = sb("top8", [P, 8]); mask = sb("mask", [P, 32]); ex = sb("ex", [P, 32])
            gt = sb("gt", [P, 32]); sm = sb("sm", [P, 4])
            pT = [st.enter_context(nc.psum_tensor(f"pTc{i}", [P, 8, P], F32)) for i in range(3)]
            pL = st.enter_context(nc.psum_tensor("pL", [P, 32], F32))
            pG = st.enter_context(nc.psum_tensor("pG", [32, P], F32))
            npt = [0]
            dk = []

            def transposes(src, srckey, evac):
                for hf in range(2):
                    pi = npt[0] % 3
                    npt[0] += 1

                    def tr(e, hf=hf, pi=pi):
                        last = None
                        for k8 in range(8):
                            kt = hf * 8 + k8
                            last = e.transpose(pT[pi][:, k8, :], src[:, kt * P:(kt + 1) * P], self.ident[:])
                        return last
                    S.op('pe', tr, reads=[srckey], writes=[('pTc', pi)])
                    evac(hf, pi)

            import os
            ccut = int(os.environ.get("C_CUT", "9"))
            for t in range(NT_OWN):
                tsl = slice(t * P, (t + 1) * P)
                S.dma('sp', xin[:], self.x[tsl, :], writes=['xin'])
                S.op('dve', lambda e, t=t: e.tensor_tensor(tt[:], self.acc[:, t, :], g1r, ALU.mult), reads=[('acc', t)], writes=['tt'])
                S.op('dve', lambda e: e.scalar_tensor_tensor(tt[:], xin[:], ALPHA, tt[:], ALU.mult, ALU.add), reads=['tt', 'xin'], writes=['tt'])
                self.ln_stats(S, stt, mv, tt, 'tt', 'ln1')
                S.op('act', lambda e: e.activation(tt[:], tt[:], AF.Identity, bias=mv[:, 3:4], scale=mv[:, 2:3]),
                     reads=['tt', ('ln1', 'nb'), ('ln1', 'rs')], writes=['tt'])
                S.op('dve', lambda e: e.tensor_tensor(tt[:], tt[:], ln1[:, 0, :], ALU.mult), reads=['tt', 'ln1g'], writes=['tt'])
                S.op('dve', lambda e: e.tensor_tensor(tt[:], tt[:], ln1[:, 1, :], ALU.add), reads=['tt', 'ln1b'], writes=['tt'])
                if self.debug:
                    S.dma('sp', self.dbg_x1[tsl, :], tt[:], reads=['tt'], writes=[('dbgx1', t)]); dk.append(('dbgx1', t))
                if ccut < 5:
                    continue
                self.ln_stats(S, stt, mv2, tt, 'tt', 'ln2h')
                S.op('act', lambda e: e.activation(h2[:], tt[:], AF.Identity, bias=mv2[:, 3:4], scale=mv2[:, 2:3]),
                     reads=['tt', ('ln2h', 'nb'), ('ln2h', 'rs')], writes=['h2'])
                S.op('dve', lambda e: e.tensor_tensor(h2[:], h2[:], sc2r, ALU.mult), reads=['h2'], writes=['h2'])
                S.op('dve', lambda e: e.tensor_tensor(h2[:], h2[:], sh2r, ALU.add), reads=['h2'], writes=['h2'])
                if self.debug:
                    S.dma('sp', self.dbg_h2[tsl, :], h2[:], reads=['h2'], writes=[('dbgh2', t)]); dk.append(('dbgh2', t))

                def evac_h2(hf, pi, t=t, tsl=tsl):
                    S.op('act', lambda e: e.activation(self.h2T[:, hf * 8:(hf + 1) * 8, tsl], pT[pi][:], AF.Copy),
                         reads=[('pTc', pi)], writes=[('h2T', t, hf)])
                    S.op('act', lambda e: e.activation(h2T32[:, hf * 8:(hf + 1) * 8, :], pT[pi][:], AF.Copy),
                         reads=[('pTc', pi), 'xin'], writes=[('h2T32', hf), 'xin'])
                if ccut < 6:
                    continue
                transposes(h2, 'h2', evac_h2)
                if ccut < 7:
                    continue
                S.op('pe', lambda e: self.mm_group(e, pL[:], [(h2T32[:, kt, :], wr[:, kt, :]) for kt in range(KT)]),
                     reads=[('h2T32', 0), ('h2T32', 1), 'wr', 'xin'], writes=['pL'])
                S.op('dve', lambda e: e.tensor_tensor(lg[:], pL[:], br[:], ALU.add), reads=['pL', 'br'], writes=['lg'])
                if self.debug:
                    S.dma('sp', self.dbg_lg[tsl, :], lg[:], reads=['lg'], writes=[('dbglg', t)]); dk.append(('dbglg', t))
                if ccut < 8:
                    continue
                S.op('dve', lambda e: e.max(top8[:], lg[:]), reads=['lg'], writes=['top8'])
                S.op('dve', lambda e: e.tensor_scalar(mask[:], lg[:], top8[:, 3:4], None, ALU.is_ge), reads=['lg', 'top8'], writes=['mask'])
                S.op('dve', lambda e: e.tensor_scalar(sm[:, 0:1], top8[:, 0:1], -1.0, None, ALU.mult), reads=['top8'], writes=['negm'])
                S.op('act', lambda e: e.activation(ex[:], lg[:], AF.Exp, bias=sm[:, 0:1], scale=1.0), reads=['lg', 'negm'], writes=['ex'])
                S.op('dve', lambda e: e.tensor_tensor(ex[:], ex[:], mask[:], ALU.mult), reads=['ex', 'mask'], writes=['ex'])
                S.op('dve', lambda e: e.tensor_reduce(sm[:, 1:2], ex[:], AX.X, ALU.add), reads=['ex'], writes=['den'])
                S.op('dve', lambda e: e.reciprocal(sm[:, 2:3], sm[:, 1:2]), reads=['den'], writes=['rden'])
                S.op('dve', lambda e: e.tensor_scalar(gt[:], ex[:], sm[:, 2:3], None, ALU.mult), reads=['ex', 'rden'], writes=['gt'])
                S.op('pe', lambda e: e.transpose(pG[:], gt[:], self.ident[:]), reads=['gt'], writes=['pG'])
                S.op('act', lambda e, tsl=tsl: e.activation(self.gatesT[:, tsl], pG[:], AF.Copy), reads=['pG'], writes=[('gatesT', t)])
                S.op('dve', lambda e: e.tensor_scalar(tt[:], tt[:], ALPHA, None, ALU.mult), reads=['tt'], writes=['tt'])

                def evac_acc(hf, pi, t=t):
                    S.op('act', lambda e: e.activation(self.acc[:, t, hf * 1024:(hf + 1) * 1024].rearrange("p (k j) -> p k j", k=8), pT[pi][:], AF.Copy),
                         reads=[('pTc', pi)], writes=[('acc', t)])
                transposes(tt, 'tt', evac_acc)
            if self.debug:
                S.dma('sp', self.dbg_gt, self.gatesT[:], reads=[('gatesT', t) for t in range(NT_OWN)], writes=['dbggt']); dk.append('dbggt')
                for t in range(NT_OWN):
                    S.dma('sp', self.dbg_acc[:, t * D:(t + 1) * D], self.acc[:, t, :], reads=[('acc', t)], writes=[('dbgacc', t)]); dk.append(('dbgacc', t))
            S.emit(final_wait_keys=dk)

    def phase_MoE(self):
        nc = self.nc
        with contextlib.ExitStack() as st:
            sb = lambda n, s, d=F32: st.enter_context(nc.sbuf_tensor(n, s, d))
            ps = lambda n, s: st.enter_context(nc.psum_tensor(n, s, F32))
            S = self.S
            h2T, acc, gatesT, g2col = self.h2T, self.acc, self.gatesT, self.g2col
            wb = [sb(f"wmoe{i}", [P, KT, CW], BF16) for i in range(3)]
            actT = sb("actT", [P, 8, S_OWN], BF16)
            glu = [sb(f"glu{i}", [P, 512]) for i in range(2)]
            sig = [sb(f"sig{i}", [P, 512]) for i in range(2)]
            lin1 = [sb(f"lin1{i}", [P, 512]) for i in range(2)]
            tg = [sb(f"tg{i}", [P, 512]) for i in range(2)]
            grep = sb("grep", [P, S_OWN]); gsel = sb("gsel", [32, S_OWN])
            b1 = sb("b1s", [P, 32, 32]); b2s = sb("b2s", [32, D]); ones32 = sb("ones32", [32, P])
            pGL = [ps(f"pGL{i}", [P, 2, 512]) for i in range(2)]
            pY = [ps(f"pY{i}", [P, 512]) for i in range(2)]
            pGR = ps("pGR", [P, 512])
            S.dma('sp', b1[:], self.b1_r, writes=['b1'])
            S.dma('sp', b2s[:], self.b2, writes=['b2s'])
            S.op('dve', lambda e: e.memset(ones32[:], 1.0), writes=['ones32'])
            S.op('dve', lambda e: e.tensor_scalar(b1[:, :, 16:32], b1[:, :, 16:32], 1.0, None, ALU.add), reads=['b1'], writes=['b1'])
            nwb = [0]; ngl = [0]; ny = [0]

            def acc_add(s, dt, th):
                accv = acc[:, 4 * th:4 * th + 4, dt * P:(dt + 1) * P]
                S.op('dve', lambda e: e.scalar_tensor_tensor(accv, pY[s][:].rearrange("p (a b) -> p a b", a=4), g2col[:, dt:dt + 1], accv, ALU.mult, ALU.add),
                     reads=[('pY', s), 'g2col'], writes=[('accT', dt, th)])

            for dt in range(KT):
                for th in range(2):
                    s = ny[0] % 2
                    ny[0] += 1
                    S.op('pe', lambda e, s=s, dt=dt, th=th: e.matmul(pY[s][:], b2s[:, dt * P:(dt + 1) * P], gatesT[:, th * 512:(th + 1) * 512], start=True, stop=True),
                         reads=['b2s'], writes=[('pY', s)])
                    acc_add(s, dt, th)

            for ex in range(self.NE):
                S.op('dve', lambda e, ex=ex: e.tensor_scalar(gsel[:], gatesT[:], self.ident[0:32, ex:ex + 1], None, ALU.mult), reads=[], writes=['gsel'])
                for th in range(2):
                    S.op('pe', lambda e, th=th: e.matmul(pGR[:], ones32[:], gsel[:, th * 512:(th + 1) * 512], start=True, stop=True),
                         reads=['gsel', 'ones32'], writes=['pGR'])
                    S.op('dve', lambda e, th=th: e.tensor_copy(grep[:, th * 512:(th + 1) * 512], pGR[:]), reads=['pGR'], writes=[('grep', th)])
                for g in range(2):
                    for c8 in range(8):
                        c = g * 8 + c8
                        ws = nwb[0] % 3
                        nwb[0] += 1
                        S.dma('pool', wb[ws][:], self.w1_r[ex, c], writes=[('wmoe', ws)], max_dma_last_dim=4096)
                        for th in range(2):
                            s = ngl[0] % 2
                            ngl[0] += 1
                            tks = slice(th * 512, (th + 1) * 512)

                            def fgl(e, s=s, ws=ws, tks=tks):
                                self.mm_group(e, pGL[s][:, 0, :], [(wb[ws][:, kt, 0:P], h2T[:, kt, tks]) for kt in range(KT)])
                                return self.mm_group(e, pGL[s][:, 1, :], [(wb[ws][:, kt, P:2 * P], h2T[:, kt, tks]) for kt in range(KT)])
                            S.op('pe', fgl, reads=[('wmoe', ws)], writes=[('pGL', s)])
                            S.op('dve', lambda e, s=s, ex=ex, c=c: e.tensor_scalar(glu[s][:], pGL[s][:, 0, :], b1[:, ex, c:c + 1], 7.0, ALU.add, ALU.min),
                                 reads=[('pGL', s), 'b1'], writes=[('glu', s)])
                            S.op('act', lambda e, s=s: e.activation(sig[s][:], glu[s][:], AF.Sigmoid, scale=1.702), reads=[('glu', s)], writes=[('sig', s)])
                            S.op('dve', lambda e, s=s, ex=ex, c=c: e.tensor_scalar(lin1[s][:], pGL[s][:, 1, :], b1[:, ex, 16 + c:17 + c], 8.0, ALU.add, ALU.min),
                                 reads=[('pGL', s), 'b1'], writes=[('lin1', s)])
                            S.op('dve', lambda e, s=s: e.tensor_tensor(tg[s][:], glu[s][:], sig[s][:], ALU.mult), reads=[('glu', s), ('sig', s)], writes=[('tg', s)])
                            S.op('dve', lambda e, s=s: e.scalar_tensor_tensor(tg[s][:], lin1[s][:], -6.0, tg[s][:], ALU.max, ALU.mult),
                                 reads=[('lin1', s), ('tg', s)], writes=[('tg', s)])
                            S.op('dve', lambda e, s=s, c8=c8, tks=tks, th=th: e.tensor_tensor(actT[:, c8, tks], tg[s][:], grep[:, tks], ALU.mult),
                                 reads=[('tg', s), ('grep', th)], writes=[('actT', c8, th)])
                    for dc in range(4):
                        ws = nwb[0] % 3
                        nwb[0] += 1
                        wv = wb[ws][:].rearrange("p a b -> p (a b)").rearrange("p (f d) -> p f d", f=8)
                        S.dma('pool', wv, self.w2_r[ex, g, dc], writes=[('wmoe', ws)], max_dma_last_dim=4096)
                        for dsub in range(4):
                            dt = dc * 4 + dsub
                            for th in range(2):
                                s = ny[0] % 2
                                ny[0] += 1
                                S.op('pe', lambda e, s=s, wv=wv, dsub=dsub, th=th: self.mm_group(
                                    e, pY[s][:], [(wv[:, ft, dsub * P:(dsub + 1) * P], actT[:, ft, th * 512:(th + 1) * 512]) for ft in range(8)]),
                                    reads=[('wmoe', ws)] + [('actT', ft, th) for ft in range(8)], writes=[('pY', s)])
                                acc_add(s, dt, th)
            if self.debug:
                dk = []
                for t in range(NT_OWN):
                    S.dma('sp', self.dbg_acc2[:, t * D:(t + 1) * D], acc[:, t, :], reads=[('accT', dt, th) for dt in range(KT) for th in range(2)], writes=[('dbgacc2', t)])
                    dk.append(('dbgacc2', t))
                S.emit(final_wait_keys=dk)
            else:
                S.emit()

    def phase_final(self):
        nc = self.nc
        with contextlib.ExitStack() as st:
            sb = lambda n, s, d=F32: st.enter_context(nc.sbuf_tensor(n, s, d))
            S = self.S
            acc = self.acc
            ln2 = sb("ln2r", [P, 2, D])
            S.dma('sp', ln2[:, 0, :], self.ln2[0:1, :].partition_broadcast(P), writes=['ln2g'])
            S.dma('sp', ln2[:, 1, :], self.ln2[1:2, :].partition_broadcast(P), writes=['ln2b'])
            xo = [sb(f"xo{i}", [P, D]) for i in range(2)]
            stt = [sb(f"stt_f{i}", [P, 4, 6]) for i in range(2)]
            mv = [sb(f"mv_f{i}", [P, 6]) for i in range(2)]
            pT = [st.enter_context(nc.psum_tensor(f"pTf{i}", [P, 8, P], F32)) for i in range(3)]
            npt = 0
            dk = []
            for t in range(NT_OWN):
                s = t % 2
                for hf in range(2):
                    pi = npt % 3
                    npt += 1

                    def tr(e, hf=hf, pi=pi, t=t):
                        last = None
                        for k8 in range(8):
                            dt = hf * 8 + k8
                            last = e.transpose(pT[pi][:, k8, :], acc[:, t, dt * P:(dt + 1) * P], self.ident[:])
                        return last
                    S.op('pe', tr, reads=[], writes=[('pTf', pi)])
                    S.op('act', lambda e, s=s, hf=hf, pi=pi: e.activation(xo[s][:, hf * 1024:(hf + 1) * 1024].rearrange("p (k j) -> p k j", k=8), pT[pi][:], AF.Copy),
                         reads=[('pTf', pi)], writes=[('xo', s)])
                self.ln_stats(S, stt[s], mv[s], xo[s], ('xo', s), ('lnF', s))
                S.op('act', lambda e, s=s: e.activation(xo[s][:], xo[s][:], AF.Identity, bias=mv[s][:, 3:4], scale=mv[s][:, 2:3]),
                     reads=[('xo', s), (('lnF', s), 'nb'), (('lnF', s), 'rs')], writes=[('xo', s)])
                S.op('dve', lambda e, s=s: e.tensor_tensor(xo[s][:], xo[s][:], ln2[:, 0, :], ALU.mult), reads=[('xo', s), 'ln2g'], writes=[('xo', s)])
                S.op('dve', lambda e, s=s: e.tensor_tensor(xo[s][:], xo[s][:], ln2[:, 1, :], ALU.add), reads=[('xo', s), 'ln2b'], writes=[('xo', s)])
                S.dma('sp', self.y[t * P:(t + 1) * P, :], xo[s][:], reads=[('xo', s)], writes=[('y', t)])
                dk.append(('y', t))
            S.emit(final_wait_keys=dk)


def rope_tables():
    S = 2048
    t = np.arange(S)
    row = (t // 64 - (S // 64) // 2).astype(np.float32)
    col = (t % 64 - 32).astype(np.float32)
    inv = (10000.0 ** (-np.arange(0, 64, 2, dtype=np.float32) / 64.0)).astype(np.float32)
    ar = row[:, None] * inv[None, :]
    ac = col[:, None] * inv[None, :]
    cos = np.concatenate([np.cos(ar), np.cos(ar), np.cos(ac), np.cos(ac)], axis=1)
    sin = np.concatenate([-np.sin(ar), np.sin(ar), -np.sin(ac), np.sin(ac)], axis=1)
    return cos.astype(np.float32), sin.astype(np.float32)


def tile_w(w):
    K, N = w.shape
    return np.ascontiguousarray(w.reshape(KT, P, N // CW, CW).transpose(2, 1, 0, 3))


def prep_shared(inp, n_experts=32):
    l = 0
    NE = n_experts
    w1 = inp["w_exp_in"][l][:NE]
    w1 = np.ascontiguousarray(w1.reshape(NE, KT, P, 2, 16, P).transpose(0, 4, 2, 1, 3, 5)).reshape(NE, 16, P, KT, CW)
    w2 = inp["w_exp_out"][l][:NE]
    w2 = np.ascontiguousarray(w2.reshape(NE, 2, 8, P, 4, 512).transpose(0, 1, 4, 3, 2, 5))
    b1 = np.ascontiguousarray(inp["b_exp_in"][l].reshape(32, 32, P).transpose(2, 0, 1))
    return {"w1_r": w1, "w2_r": w2, "b1_r": b1, "b2": np.ascontiguousarray(inp["b_exp_out"][l]),
            "ln2": np.ascontiguousarray(np.stack([inp["ln2_g"][l], inp["ln2_b"][l]]))}


def prep_core(inp, core):
    b, half = core // 2, core % 2
    l = 0
    x = inp["x"][b]
    cos, sin = rope_tables()
    if half == 1:
        x = x[::-1]
        cos, sin = cos[::-1], sin[::-1]
    w_in = inp["w_in"][l]
    m = {
        "x_loc": np.ascontiguousarray(x),
        "c_col": np.ascontiguousarray(inp["c"][b].reshape(KT, P).T),
        "w_ada1": np.ascontiguousarray(inp["w_ada"][l][:, 0:2 * D]),
        "b_ada1": np.ascontiguousarray(inp["b_ada"][l][None, 0:2 * D]),
        "ident": np.eye(P, dtype=np.float32),
        "w_qkv": tile_w(w_in[:, 0:1536]),
        "qkw": np.stack([inp["q_norm_w"][l] * np.float32(1.0), inp["k_norm_w"][l]]).astype(np.float32),
        "cs": np.ascontiguousarray(np.stack([cos, sin])),
        "anw": np.ascontiguousarray(inp["attn_norm_w"][l][None, :]),
    }
    o_qr, o_ffw, o_fbw, o_i, o_g = 1536, 2560, 3584, 4608, 5632
    o_fa, o_fb = (o_ffw, o_fbw) if half == 0 else (o_fbw, o_ffw)
    cols = []
    for hh in range(8):
        sl = lambda o: w_in[:, o + hh * P:o + (hh + 1) * P]
        cols += [sl(o_qr), sl(o_fa), sl(o_fb), sl(o_i), sl(o_g), sl(o_g)]
    m["w_hg"] = tile_w(np.concatenate(cols, axis=1)).reshape(8, 3, P, KT, CW)
    lbr = inp["hgrn_lb"]
    dirs = (0, 1) if half == 0 else (1, 0)
    la = np.stack([np.concatenate([lbr[dirs[0], sl_].reshape(8, P).T, lbr[dirs[1], sl_].reshape(8, P).T], axis=1) for sl_ in range(2)])
    m["lb_a"] = np.ascontiguousarray(la.astype(np.float32))
    m["hnw"] = np.ascontiguousarray(inp["hgrn_norm_w"][l][None, :])
    jj, ii = np.meshgrid(np.arange(P), np.arange(P), indexing="ij")
    same = (jj // 64) == (ii // 64)
    m["hmask"] = np.stack([(same & (jj <= ii)), (same & (jj >= ii))]).astype(np.float32)
    m["rmask"] = (np.arange(S_OWN) % 64 != 0).astype(np.float32)[None, :]
    w_ada, b_ada = inp["w_ada"][l], inp["b_ada"][l]
    m["w_ada2"] = np.ascontiguousarray(w_ada[:, 2 * D:5 * D])
    m["b_ada2"] = np.ascontiguousarray(b_ada[None, 2 * D:5 * D])
    m["w_ada3"] = np.ascontiguousarray(w_ada[:, 5 * D:6 * D])
    m["b_g2col"] = np.ascontiguousarray(b_ada[5 * D:6 * D].reshape(KT, P).T)
    m["w_out_t"] = tile_w(inp["w_out"][l])
    m["ln1"] = np.ascontiguousarray(np.stack([inp["ln1_g"][l], inp["ln1_b"][l]]))
    m["w_r"] = np.ascontiguousarray(inp["w_router"][l].reshape(KT, P, 32).transpose(1, 0, 2))
    m["b_r"] = np.ascontiguousarray(inp["b_router"][l][None, :])
    m["halfm"] = np.stack([(np.arange(P) < 64), (np.arange(P) >= 64)], axis=1).astype(np.float32)
    return m


def kernel(**inputs):
    inp = {k: np.asarray(v) for k, v in inputs.items()}
    n = 8
    prog = Prog(['attn', 'hgrn', 'c', 'moe'], debug=False)
    shared = prep_shared(inp)
    in_maps = []
    for c in range(n):
        m = prep_core(inp, c)
        m.update(shared)
        in_maps.append(m)
    res = run_bass_kernel_spmd(prog.nc, in_maps, core_ids=list(range(n)))
    out = np.zeros((4, 2048, 2048), np.float32)
    for c in range(n):
        b, half = c // 2, c % 2
        y = np.asarray(res.results[c]["y_out"], dtype=np.float32)
        if half == 0:
            out[b, 0:1024] = y
        else:
            out[b, 1024:2048] = y[::-1]
    return out
```

```python
import contextlib
import numpy as np
import concourse.bass as bass
import concourse.mybir as mybir
from concourse.bass_utils import run_bass_kernel_spmd

F32 = mybir.dt.float32
BF16 = mybir.dt.bfloat16
AF = mybir.ActivationFunctionType
ALU = mybir.AluOpType
AX = mybir.AxisListType

COMPUTE = ('pe', 'act', 'dve')
DMAQ = ('sp', 'pool')
ALLENG = COMPUTE + DMAQ
NDS = 8
SAME_ENGINE_SYNC = True


class Sched:
    _uid = 0

    def __init__(self, nc):
        self.nc = nc
        Sched._uid += 1
        self.ops = {e: [] for e in ALLENG}
        self.n = {e: 0 for e in ALLENG}
        self.lastw = {}
        self.readers = {}
        self.seen = {e: {} for e in ALLENG}

    @staticmethod
    def _tgt(eng, idx):
        if eng in DMAQ:
            return (eng, (idx - 1) % NDS), 16 * ((idx - 1) // NDS + 1)
        return (eng, 0), idx

    def op(self, eng, fn, reads=(), writes=()):
        deps = {}

        def add(t, v):
            if deps.get(t, 0) < v:
                deps[t] = v

        for k in reads:
            if k in self.lastw:
                add(*self.lastw[k])
        for k in writes:
            if k in self.lastw:
                add(*self.lastw[k])
            for t, v in self.readers.get(k, {}).items():
                add(t, v)
        idx = None
        if fn is not None:
            self.n[eng] += 1
            idx = self.n[eng]
            if eng in DMAQ and idx > NDS:
                add(*self._tgt(eng, idx - NDS))
        waits = []
        for t, v in deps.items():
            if t[0] == eng and eng == 'pe':
                continue
            if t[0] == eng and eng in COMPUTE and not SAME_ENGINE_SYNC:
                continue
            if self.seen[eng].get(t, 0) >= v:
                continue
            self.seen[eng][t] = v
            waits.append((t, v))
        self.ops[eng].append((waits, fn, idx))
        if fn is None:
            return
        me = self._tgt(eng, idx)
        for k in writes:
            self.lastw[k] = me
            self.readers[k] = {}
        for k in reads:
            if k not in writes:
                r = self.readers.setdefault(k, {})
                if r.get(me[0], 0) < me[1]:
                    r[me[0]] = me[1]

    def dma(self, q, out, in_, reads=(), writes=(), **kw):
        self.op(q, lambda e: e.dma_start(out=out, in_=in_, **kw), reads=reads, writes=writes)

    def alloc_sems(self, stack):
        nc = self.nc
        self.sems = {}
        for e in COMPUTE:
            self.sems[(e, 0)] = stack.enter_context(nc.semaphore(f"s_{e}"))
        for e in DMAQ:
            for s in range(NDS):
                self.sems[(e, s)] = stack.enter_context(nc.semaphore(f"s_{e}{s}"))

    def emit(self, final_wait_keys=()):
        nc = self.nc
        Sched._uid += 1
        self.op('sp', None, reads=list(final_wait_keys))
        finals = []
        for e in ALLENG:
            if self.n[e] == 0:
                continue
            if e in DMAQ:
                for s in range(min(NDS, self.n[e])):
                    last = ((self.n[e] - 1 - s) // NDS) * NDS + s + 1
                    finals.append(self._tgt(e, last))
            else:
                finals.append(self._tgt(e, self.n[e]))
        for e in ALLENG:
            w = []
            for (t, v) in finals:
                if self.seen[e].get(t, 0) < v and not (t[0] == e and e == 'pe'):
                    w.append((t, v))
                    self.seen[e][t] = v
            self.ops[e].append((w, None, None))
        sems = self.sems
        ops = self.ops
        self.ops = {e: [] for e in ALLENG}
        with nc.Block() as blk:
            def run(eng, h):
                for waits, fn, idx in ops[eng]:
                    for t, v in waits:
                        h.wait_ge(sems[t], v)
                    if fn is not None:
                        ins = fn(h)
                        t, _ = self._tgt(eng, idx)
                        ins.then_inc(sems[t], 16 if eng in DMAQ else 1)

            blk.tensor(lambda h: run('pe', h))
            blk.scalar(lambda h: run('act', h))
            blk.vector(lambda h: run('dve', h))
            blk.gpsimd(lambda h: run('pool', h))
            blk.sync(lambda h: run('sp', h))


P = 128
D = 2048
KT = 16
S_ALL = 2048
S_OWN = 1024
NT_ALL = 16
NT_OWN = 8
CW = 256
EPS = 1e-6


class Prog:
    def __init__(self, stages, debug=False, n_experts=32):
        self.NE = n_experts
        self.stages = stages
        self.debug = debug
        nc = self.nc = bass.Bass("TRN2", target_bir_lowering=False)
        di = lambda n, s: nc.dram_tensor(n, s, F32, kind="ExternalInput").ap()
        self.x = di("x_loc", [S_ALL, D])
        self.c_col = di("c_col", [P, KT])
        self.w_ada1 = di("w_ada1", [D, 2 * D])
        self.b_ada1 = di("b_ada1", [1, 2 * D])
        self.ident_d = di("ident", [P, P])
        self.w_qkv = di("w_qkv", [6, P, KT, CW])
        self.qkw = di("qkw", [2, P])
        self.cs = di("cs", [2, S_ALL, P])
        self.anw = di("anw", [1, 1024])
        self.y = nc.dram_tensor("y_out", [S_OWN, D], F32, kind="ExternalOutput").ap()
        self.w_hg = di("w_hg", [8, 3, P, KT, CW])
        self.lb_a = di("lb_a", [2, P, 16])
        self.hnw = di("hnw", [1, 1024])
        self.hmask = di("hmask", [2, P, P])
        self.rmask_d = di("rmask", [1, S_OWN])
        self.halfm_d = di("halfm", [P, 2])
        self.w_ada2 = di("w_ada2", [D, 3 * D])
        self.b_ada2 = di("b_ada2", [1, 3 * D])
        self.w_ada3 = di("w_ada3", [D, D])
        self.b_g2col = di("b_g2col", [P, KT])
        self.w_out_t = di("w_out_t", [8, P, KT, CW])
        self.ln1 = di("ln1", [2, D])
        self.w_r = di("w_r", [P, KT, 32])
        self.b_r = di("b_r", [1, 32])
        self.w1_r = di("w1_r", [self.NE, 16, P, KT, CW])
        self.w2_r = di("w2_r", [self.NE, 2, 4, P, 8, 512])
        self.b1_r = di("b1_r", [P, 32, 32])
        self.b2 = di("b2", [32, D])
        self.ln2 = di("ln2", [2, D])
        if debug:
            do = lambda n, s: nc.dram_tensor(n, s, F32, kind="ExternalOutput").ap()
            self.dbg_h = do("dbg_h", [S_ALL, D])
            self.dbg_q = do("dbg_q", [S_OWN, 1024])
            self.dbg_k = do("dbg_k", [S_ALL, 256])
            self.dbg_oa = do("dbg_oa", [S_OWN, 1024])
            self.dbg_mod = do("dbg_mod", [P, 2 * D])
            self.dbg_or = do("dbg_or", [S_OWN, 1024])
            self.dbg_os = do("dbg_os", [S_OWN, 1024])
            self.dbg_x1 = do("dbg_x1", [S_OWN, D])
            self.dbg_h2 = do("dbg_h2", [S_OWN, D])
            self.dbg_lg = do("dbg_lg", [S_OWN, 32])
            self.dbg_gt = do("dbg_gt", [32, S_OWN])
            self.dbg_acc = do("dbg_acc", [P, 8 * D])
            self.dbg_acc2 = do("dbg_acc2", [P, 8 * D])
        self.build()

    def mm_group(self, e, out, pairs):
        last = None
        n = len(pairs)
        for i, (l, r) in enumerate(pairs):
            last = e.matmul(out, l, r, start=(i == 0), stop=(i == n - 1))
        return last

    def build(self):
        nc = self.nc
        with contextlib.ExitStack() as top:
            sb = lambda n, s, d=F32: top.enter_context(nc.sbuf_tensor(n, s, d))
            self.S = Sched(nc)
            self.S.alloc_sems(top)
            self.ident = sb("ident_s", [P, P])
            self.eps_t = sb("eps_t", [P, 1])
            self.gatesT = sb("gatesT", [32, S_OWN])
            self.g2col = sb("g2col", [P, KT])
            self.R1 = sb("R1", [P, KT, S_ALL], BF16)
            self.hT = self.R1
            self.acc = self.R1[:].rearrange("p a b -> p (a b)").bitcast(F32).rearrange("p (t d) -> p t d", t=NT_OWN)
            self.R2 = sb("R2", [P, 16, S_OWN], BF16)
            self.mixedT = self.R2
            self.h2T = self.R2
            self.phase_A()
            if 'attn' in self.stages:
                self.phase_B1()
            if 'hgrn' in self.stages:
                self.phase_B2()
            if 'c' in self.stages:
                import os
                ccut = int(os.environ.get("C_CUT", "9"))
                self.phase_C1()
                with contextlib.ExitStack() as cs:
                    self.mod2 = cs.enter_context(nc.sbuf_tensor("mod2", [P, 3 * D], F32))
                    if ccut >= 2:
                        self.phase_C0()
                    if ccut >= 3:
                        self.phase_C2()
            if 'moe' in self.stages:
                self.phase_MoE()
                self.phase_final()

    def ada_part(self, S, st, w_ap, b_ap, ncols, dest, carep, plus_one_from):
        nc = self.nc
        wbuf = [st.enter_context(nc.sbuf_tensor(f"adaw{i}_{Sched._uid}", [P, KT, 512], BF16)) for i in range(2)]
        bb = [st.enter_context(nc.sbuf_tensor(f"adab{i}_{Sched._uid}", [P, 512], F32)) for i in range(2)]
        ps = [st.enter_context(nc.psum_tensor(f"adap{i}_{Sched._uid}", [P, 512], F32)) for i in range(2)]
        for c in range(ncols // 512):
            s = c % 2
            S.dma('pool', wbuf[s][:], w_ap[:, c * 512:(c + 1) * 512].rearrange("(kt p) n -> p kt n", p=P),
                  writes=[('adaw', s)])
            S.dma('sp', bb[s][:], b_ap[:, c * 512:(c + 1) * 512].partition_broadcast(P), writes=[('adab', s)])
            S.op('pe', lambda e, s=s: self.mm_group(e, ps[s][:], [(carep[:, kt, :], wbuf[s][:, kt, :]) for kt in range(KT)]),
                 reads=[('adaw', s), 'carep'], writes=[('adap', s)])
            d = dest[:, c * 512:(c + 1) * 512]
            if c * 512 >= plus_one_from:
                S.op('dve', lambda e, s=s, d=d: e.scalar_tensor_tensor(d, ps[s][:], 1.0, bb[s][:], ALU.add, ALU.add),
                     reads=[('adap', s), ('adab', s)], writes=[('mod', c)])
            else:
                S.op('dve', lambda e, s=s, d=d: e.tensor_tensor(d, ps[s][:], bb[s][:], ALU.add),
                     reads=[('adap', s), ('adab', s)], writes=[('mod', c)])

    def ln_stats(self, S, st_t, mv_t, src, key_src, tag):
        def f(e):
            last = None
            for j in range(4):
                last = e.bn_stats(st_t[:, j, :], src[:, j * 512:(j + 1) * 512])
            return last
        S.op('dve', f, reads=[key_src], writes=[(tag, 'st')])
        S.op('dve', lambda e: e.bn_aggr(mv_t[:, 0:2], st_t[:].rearrange("p a b -> p (a b)")),
             reads=[(tag, 'st')], writes=[(tag, 'mv')])
        S.op('act', lambda e: e.activation(mv_t[:, 2:3], mv_t[:, 1:2], AF.Sqrt, bias=self.eps_t[:, 0:1], scale=1.0),
             reads=[(tag, 'mv')], writes=[(tag, 'sd')])
        r, v, t = mv_t[:, 2:3], mv_t[:, 4:5], mv_t[:, 5:6]
        S.op('dve', lambda e: e.reciprocal(r, r), reads=[(tag, 'sd')], writes=[(tag, 'rs')])
        S.op('dve', lambda e: e.tensor_scalar(v, mv_t[:, 1:2], EPS, None, ALU.add), reads=[(tag, 'mv')], writes=[(tag, 'v')])
        for _ in range(1):
            S.op('dve', lambda e: e.tensor_tensor(t, r, r, ALU.mult), reads=[(tag, 'rs')], writes=[(tag, 't')])
            S.op('dve', lambda e: e.tensor_scalar(t, t, v, -0.5, ALU.mult, ALU.mult), reads=[(tag, 't'), (tag, 'v')], writes=[(tag, 't')])
            S.op('dve', lambda e: e.scalar_tensor_tensor(r, t, 1.5, r, ALU.add, ALU.mult), reads=[(tag, 't'), (tag, 'rs')], writes=[(tag, 'rs')])
        S.op('dve', lambda e: e.tensor_scalar(mv_t[:, 3:4], mv_t[:, 0:1], mv_t[:, 2:3], -1.0, ALU.mult, ALU.mult),
             reads=[(tag, 'rs'), (tag, 'mv')], writes=[(tag, 'nb')])

    def phase_A(self):
        nc = self.nc
        with contextlib.ExitStack() as st:
            sb = lambda n, s, d=F32: st.enter_context(nc.sbuf_tensor(n, s, d))
            S = self.S
            S.op('dve', lambda e: e.memset(self.eps_t[:], EPS), writes=['eps'])
            S.dma('sp', self.ident[:], self.ident_d, writes=['ident'])
            carep = self.make_carep(S, st)[0]
            mod1 = sb("mod1", [P, 2 * D])
            self.ada_part(S, st, self.w_ada1, self.b_ada1, 2 * D, mod1, carep, D)
            modkeys = [('mod', c) for c in range(8)]
            if self.debug:
                S.dma('sp', self.dbg_mod, mod1[:], reads=modkeys, writes=['dbg_mod'])
            xin = [sb(f"xin{i}", [P, D]) for i in range(2)]
            hn = [sb(f"hn{i}", [P, D]) for i in range(2)]
            stt = [sb(f"stt{i}", [P, 4, 6]) for i in range(2)]
            mv = [sb(f"mv{i}", [P, 6]) for i in range(2)]
            pT = [st.enter_context(nc.psum_tensor(f"pT{i}", [P, 8, P], F32)) for i in range(3)]
            npt = 0
            for t in range(NT_ALL):
                s = t % 2
                S.dma('sp', xin[s][:], self.x[t * P:(t + 1) * P, :], writes=[('xin', s)])
                self.ln_stats(S, stt[s], mv[s], xin[s], ('xin', s), ('lnA', s))
                S.op('act', lambda e, s=s: e.activation(hn[s][:], xin[s][:], AF.Identity, bias=mv[s][:, 3:4], scale=mv[s][:, 2:3]),
                     reads=[('xin', s), (('lnA', s), 'nb'), (('lnA', s), 'rs')], writes=[('hn', s)])
                S.op('dve', lambda e, s=s: e.tensor_tensor(hn[s][:], hn[s][:], mod1[:, D:2 * D], ALU.mult),
                     reads=[('hn', s)] + modkeys, writes=[('hn', s)])
                S.op('dve', lambda e, s=s: e.tensor_tensor(hn[s][:], hn[s][:], mod1[:, 0:D], ALU.add),
                     reads=[('hn', s)] + modkeys, writes=[('hn', s)])
                if self.debug:
                    S.dma('sp', self.dbg_h[t * P:(t + 1) * P, :], hn[s][:], reads=[('hn', s)], writes=[('dbg_h', t)])
                for hf in range(2):
                    pi = npt % 3
                    npt += 1

                    def tr(e, s=s, hf=hf, pi=pi):
                        last = None
                        for k8 in range(8):
                            kt = hf * 8 + k8
                            last = e.transpose(pT[pi][:, k8, :], hn[s][:, kt * P:(kt + 1) * P], self.ident[:])
                        return last
                    S.op('pe', tr, reads=[('hn', s), 'ident'], writes=[('pT', pi)])
                    S.op('act', lambda e, t=t, hf=hf, pi=pi: e.activation(self.hT[:, hf * 8:(hf + 1) * 8, t * P:(t + 1) * P], pT[pi][:], AF.Copy),
                         reads=[('pT', pi)], writes=[('hT', t, hf)])
            S.emit(final_wait_keys=[('dbg_h', t) for t in range(NT_ALL)] if self.debug else [])

    def phase_B1(self):
        nc = self.nc
        with contextlib.ExitStack() as st:
            sb = lambda n, s, d=F32: st.enter_context(nc.sbuf_tensor(n, s, d))
            qT = sb("qT", [P, 8, S_OWN], BF16)
            kT = sb("kT", [P, 2, S_ALL], BF16)
            v_aug = sb("v_aug", [P, NT_ALL, 2, 132], BF16)
            self.B1a(qT, kT, v_aug)
            self.B1b(qT, kT, v_aug)

    def B1a(self, qT, kT, v_aug):
        nc = self.nc
        with contextlib.ExitStack() as st:
            sb = lambda n, s, d=F32: st.enter_context(nc.sbuf_tensor(n, s, d))
            S = self.S
            S.op('dve', lambda e: e.memset(v_aug[:, :, :, 128:129], 1.0), writes=['vones'])
            nw = sb("nw", [P, 2, P])
            S.dma('sp', nw[:, 0, :], self.qkw[0:1, :].partition_broadcast(P), writes=['nw0'])
            S.dma('sp', nw[:, 1, :], self.qkw[1:2, :].partition_broadcast(P), writes=['nw1'])
            wb = [sb(f"wqkv{i}", [P, KT, CW], BF16) for i in range(3)]
            cst = [sb(f"cst{i}", [P, 2, P]) for i in range(2)]
            sq = [sb(f"sq{i}", [P, 2, P]) for i in range(2)]
            qn = [sb(f"qn{i}", [P, 2, P]) for i in range(2)]
            t1 = [sb(f"t1{i}", [P, 2, P]) for i in range(2)]
            t2 = [sb(f"t2{i}", [P, 2, P]) for i in range(2)]
            ss = [sb(f"ss{i}", [P, 4]) for i in range(2)]
            pq = [st.enter_context(nc.psum_tensor(f"pq{i}", [P, CW], F32)) for i in range(2)]
            ptr = [st.enter_context(nc.psum_tensor(f"ptr{i}", [P, 2, P], F32)) for i in range(2)]
            it = 0
            for j in range(6):
                ws = j % 3
                S.dma('pool', wb[ws][:], self.w_qkv[j], writes=[('wb', ws)], max_dma_last_dim=4096)
                ntiles = NT_OWN if j < 4 else NT_ALL
                for t in range(ntiles):
                    s = it % 2
                    it += 1
                    S.op('pe', lambda e, s=s, t=t, ws=ws: self.mm_group(
                        e, pq[s][:], [(self.hT[:, kt, t * P:(t + 1) * P], wb[ws][:, kt, :]) for kt in range(KT)]),
                        reads=[('wb', ws), ('hT', t, 0), ('hT', t, 1)], writes=[('pq', s)])
                    pq3 = pq[s][:].rearrange("p (h d) -> p h d", h=2)
                    if j == 5:
                        S.op('act', lambda e, t=t, pq3=pq3: e.activation(v_aug[:, t, :, 0:128], pq3, AF.Copy),
                             reads=[('pq', s)], writes=[('v', t)])
                        continue
                    wi = 0 if j < 4 else 1
                    S.dma('sp', cst[s][:], self.cs[:, t * P:(t + 1) * P, :].rearrange("c t d -> t c d"), writes=[('cst', s)])
                    S.op('act', lambda e, s=s: e.activation(sq[s][:].rearrange("p h d -> p (h d)"), pq[s][:], AF.Square),
                         reads=[('pq', s)], writes=[('sq', s)])
                    S.op('dve', lambda e, s=s: e.tensor_reduce(ss[s][:, 0:2], sq[s][:], AX.X, ALU.add),
                         reads=[('sq', s)], writes=[('ss', s)])
                    S.op('act', lambda e, s=s: e.activation(ss[s][:, 2:4], ss[s][:, 0:2], AF.Sqrt, bias=self.eps_t[:, 0:1], scale=1.0 / 128),
                         reads=[('ss', s)], writes=[('sd', s)])
                    S.op('dve', lambda e, s=s: e.reciprocal(ss[s][:, 2:4], ss[s][:, 2:4]), reads=[('sd', s)], writes=[('rs', s)])
                    S.op('dve', lambda e, s=s, pq3=pq3: e.tensor_tensor(qn[s][:], pq3, ss[s][:, 2:4].unsqueeze(2).to_broadcast([P, 2, P]), ALU.mult),
                         reads=[('pq', s), ('rs', s)], writes=[('qn', s)])
                    S.op('dve', lambda e, s=s, wi=wi: e.tensor_tensor(qn[s][:], qn[s][:], nw[:, wi:wi + 1, :].to_broadcast([P, 2, P]), ALU.mult),
                         reads=[('qn', s), 'nw0', 'nw1'], writes=[('qn', s)])
                    S.op('dve', lambda e, s=s: e.tensor_tensor(t1[s][:], qn[s][:], cst[s][:, 0:1, :].to_broadcast([P, 2, P]), ALU.mult),
                         reads=[('qn', s), ('cst', s)], writes=[('t1', s)])
                    v5 = lambda a: a.rearrange("p h (a f d) -> p h a f d", a=2, f=2)
                    for f in range(2):
                        S.op('dve', lambda e, s=s, f=f: e.tensor_tensor(
                            v5(t2[s][:])[:, :, :, f, :], v5(qn[s][:])[:, :, :, 1 - f, :],
                            cst[s][:, 1, :].rearrange("p (a f d) -> p a f d", a=2, f=2)[:, :, f, :].unsqueeze(1).to_broadcast([P, 2, 2, 32]),
                            ALU.mult), reads=[('qn', s), ('cst', s)], writes=[('t2', s, f)])
                    S.op('dve', lambda e, s=s: e.tensor_tensor(t1[s][:], t1[s][:], t2[s][:], ALU.add),
                         reads=[('t1', s), ('t2', s, 0), ('t2', s, 1)], writes=[('t1', s)])
                    if self.debug:
                        dst = self.dbg_q[t * P:(t + 1) * P, j * CW:(j + 1) * CW] if j < 4 else self.dbg_k[t * P:(t + 1) * P, :]
                        S.dma('sp', dst, t1[s][:].rearrange("p h d -> p (h d)"), reads=[('t1', s)], writes=[('dbgqk', j, t)])

                    def tr(e, s=s):
                        e.transpose(ptr[s][:, 0, :], t1[s][:, 0, :], self.ident[:])
                        return e.transpose(ptr[s][:, 1, :], t1[s][:, 1, :], self.ident[:])
                    S.op('pe', tr, reads=[('t1', s)], writes=[('ptr', s)])
                    dstT = qT[:, 2 * j:2 * j + 2, t * P:(t + 1) * P] if j < 4 else kT[:, :, t * P:(t + 1) * P]
                    S.op('act', lambda e, s=s, dstT=dstT: e.activation(dstT, ptr[s][:], AF.Copy),
                         reads=[('ptr', s)], writes=[('qkT', j, t)])
            fk = [('dbgqk', j, t) for j in range(5) for t in range(NT_OWN if j < 4 else NT_ALL)] if self.debug else []
            S.emit(final_wait_keys=fk)

    def B1b(self, qT, kT, v_aug):
        nc = self.nc
        with contextlib.ExitStack() as st:
            sb = lambda n, s, d=F32: st.enter_context(nc.sbuf_tensor(n, s, d))
            S = self.S
            anw = sb("anw_rep", [P, 1024])
            S.dma('sp', anw[:], self.anw.partition_broadcast(P), writes=['anw'])
            zt = sb("zt", [P, 1024])
            S.op('dve', lambda e: e.memset(zt[:], 0.0), writes=['zt'])
            dk = []
            for t in range(NT_OWN if 'moe' not in self.stages else 0):
                S.dma('sp', self.y[t * P:(t + 1) * P, 1024:2048], zt[:], reads=['zt'], writes=[('yz', t)])
                dk.append(('yz', t))
            E = [sb(f"E{i}", [P, NT_ALL, 512], BF16) for i in range(2)]
            o = [sb(f"o{i}", [P, P]) for i in range(2)]
            on = [sb(f"on{i}", [P, P]) for i in range(2)]
            junk = [sb(f"junk{i}", [P, P]) for i in range(2)]
            sc = [sb(f"sc{i}", [P, 4]) for i in range(2)]
            pS = [st.enter_context(nc.psum_tensor(f"pS{i}", [P, 512], F32)) for i in range(3)]
            pO = [st.enter_context(nc.psum_tensor(f"pO{i}", [P, 132], F32)) for i in range(2)]
            pX = [st.enter_context(nc.psum_tensor(f"pX{i}", [P, P], F32)) for i in range(2)]
            ns = 0
            no = 0
            for h in range(8):
                kvh = h // 4
                for qc in range(2):
                    es = (h * 2 + qc) % 2
                    for j in range(NT_ALL):
                        s = ns % 3
                        ns += 1
                        S.op('pe', lambda e, s=s, j=j, h=h, qc=qc, kvh=kvh: e.matmul(
                            pS[s][:], kT[:, kvh, j * P:(j + 1) * P], qT[:, h, qc * 512:(qc + 1) * 512], start=True, stop=True),
                            reads=[], writes=[('pS', s)])
                        S.op('act', lambda e, s=s, j=j, es=es: e.activation(E[es][:, j, :], pS[s][:], AF.Exp, scale=float(1.0 / np.sqrt(128.0))),
                             reads=[('pS', s)], writes=[('E', es, j)])
                    for qt in range(4):
                        s = no % 2
                        no += 1
                        tq = qc * 4 + qt
                        S.op('pe', lambda e, s=s, es=es, qt=qt, kvh=kvh: self.mm_group(
                            e, pO[s][:, 0:129], [(E[es][:, j, qt * P:(qt + 1) * P], v_aug[:, j, kvh, 0:129]) for j in range(NT_ALL)]),
                            reads=[('E', es, j) for j in range(NT_ALL)], writes=[('pO', s)])
                        S.op('dve', lambda e, s=s: e.reciprocal(sc[s][:, 0:1], pO[s][:, 128:129]), reads=[('pO', s)], writes=[('rden', s)])
                        S.op('dve', lambda e, s=s: e.tensor_scalar(o[s][:], pO[s][:, 0:128], sc[s][:, 0:1], None, ALU.mult),
                             reads=[('pO', s), ('rden', s)], writes=[('o', s)])
                        S.op('act', lambda e, s=s: e.activation(junk[s][:], o[s][:], AF.Square, accum_out=sc[s][:, 1:2]),
                             reads=[('o', s)], writes=[('oss', s), ('junk', s)])
                        S.op('act', lambda e, s=s: e.activation(sc[s][:, 2:3], sc[s][:, 1:2], AF.Sqrt, bias=self.eps_t[:, 0:1], scale=1.0 / 128),
                             reads=[('oss', s)], writes=[('osd', s)])
                        S.op('dve', lambda e, s=s: e.reciprocal(sc[s][:, 2:3], sc[s][:, 2:3]), reads=[('osd', s)], writes=[('ors', s)])
                        S.op('dve', lambda e, s=s, h=h: e.scalar_tensor_tensor(on[s][:], o[s][:], sc[s][:, 2:3], anw[:, h * P:(h + 1) * P], ALU.mult, ALU.mult),
                             reads=[('o', s), ('ors', s), 'anw'], writes=[('on', s)])
                        if 'moe' not in self.stages:
                            S.dma('sp', self.y[tq * P:(tq + 1) * P, h * P:(h + 1) * P], on[s][:], reads=[('on', s)], writes=[('yo', h, tq)])
                            dk.append(('yo', h, tq))
                        if self.debug:
                            S.dma('sp', self.dbg_oa[tq * P:(tq + 1) * P, h * P:(h + 1) * P], on[s][:], reads=[('on', s)], writes=[('dbgoa', h, tq)])
                            dk.append(('dbgoa', h, tq))
                        S.op('pe', lambda e, s=s: e.transpose(pX[s][:], on[s][:], self.ident[:]), reads=[('on', s)], writes=[('pX', s)])
                        S.op('act', lambda e, s=s, h=h, tq=tq: e.activation(self.mixedT[:, h, tq * P:(tq + 1) * P], pX[s][:], AF.Copy),
                             reads=[('pX', s)], writes=[('mixedT', h, tq)])
            S.emit(final_wait_keys=dk)

    def phase_B2(self):
        nc = self.nc
        hT = self.hT
        with contextlib.ExitStack() as st:
            sb = lambda n, s, d=F32: st.enter_context(nc.sbuf_tensor(n, s, d))
            ps = lambda n, s: st.enter_context(nc.psum_tensor(n, s, F32))
            S = self.S
            N = S_OWN
            NCH = 16
            lba = sb("lba", [P, 2, 16]); lb = sb("lb", [P, 16]); oml = sb("oml", [P, 16])
            S.dma('sp', lba[:], self.lb_a.rearrange("s p c -> p s c"), writes=['lba'])
            S.op('dve', lambda e: e.tensor_tensor(oml[:], lba[:, 0, :], lba[:, 1, :], ALU.subtract), reads=['lba'], writes=['oml'])
            S.op('act', lambda e: e.activation(lb[:], oml[:], AF.Sigmoid), reads=['oml'], writes=['lb'])
            S.op('dve', lambda e: e.tensor_scalar(oml[:], lb[:], -1.0, 1.0, ALU.mult, ALU.add), reads=['lb'], writes=['oml'])
            msk = sb("hmask_s", [P, 2, P])
            S.dma('sp', msk[:], self.hmask.rearrange("m j i -> j m i"), writes=['msk'])
            rmask = sb("rmask_s", [P, N])
            S.dma('sp', rmask[:], self.rmask_d.partition_broadcast(P), writes=['rmask'])
            hnw = [sb(f"hnw{i}", [P, P]) for i in range(2)]
            wb = [sb(f"whg{i}", [P, KT, CW], BF16) for i in range(2)]
            qr = sb("qr", [P, N]); sgA = sb("sgA", [P, N]); sgB = sb("sgB", [P, N])
            v_tm = sb("v_tm", [P, NT_ALL, P], BF16); sg_tm = sb("sg_tm", [64, NCH, P], BF16)
            X2 = sb("X2", [P, N]); X3 = sb("X3", [P, N]); X4 = sb("X4", [P, N]); X5 = sb("X5", [P, N])
            kdec = [sb(f"kdec{i}", [P, N], BF16) for i in range(2)]
            qdec = [sb(f"qdec{i}", [P, N], BF16) for i in range(2)]
            kend_tm = sb("kend_tm", [P, 2, NT_OWN, P], BF16)
            halfm = sb("halfm_s", [P, 2])
            S.dma('sp', halfm[:], self.halfm_d, writes=['halfm'])
            Sbf = [sb(f"Sbf{i}", [P, NCH, P], BF16) for i in range(2)]
            S32 = sb("S32", [P, 2, P]); etot = sb("etot", [P, NCH]); scur = [0]
            scm_ = [[sb(f"scm{j}_{i}", [P, P], BF16) for i in range(2)] for j in range(2)]
            oh_ = [sb(f"oh{j}", [64, 2, P]) for j in range(2)]; oh2_ = [sb(f"oh2{j}", [64, 2, P]) for j in range(2)]
            junk_ = [sb(f"hjunk{j}", [64, P]) for j in range(2)]; sc_ = [sb(f"hsc{j}", [64, 8]) for j in range(2)]
            pP = [ps(f"pP{i}", [P, 512]) for i in range(2)]
            pV = ps("pV", [P, 4, P]); pTk = ps("pTk", [P, 4, P]); pU = ps("pU", [P, 4, P])
            pSc = ps("pSc", [P, 2, P]); pOh = ps("pOh", [64, 2, P]); pX = ps("pXh", [P, 2, 64])
            npp = [0]
            nwb = [0]
            dk = []

            def load_w(hh, c):
                s = nwb[0] % 2
                nwb[0] += 1
                S.dma('pool', wb[s][:], self.w_hg[hh, c], writes=[('whg', s)], max_dma_last_dim=4096)
                return s

            def proj_fm(ws, col0, tok0, ntok, func, dest, key):
                for c in range(ntok // 512):
                    s = npp[0] % 2
                    npp[0] += 1
                    S.op('pe', lambda e, s=s, c=c: self.mm_group(e, pP[s][:], [
                        (wb[ws][:, kt, col0:col0 + P], hT[:, kt, tok0 + c * 512:tok0 + (c + 1) * 512]) for kt in range(KT)]),
                        reads=[('whg', ws)], writes=[('pP', s)])
                    S.op('act', lambda e, s=s, c=c: e.activation(dest[:, c * 512:(c + 1) * 512], pP[s][:], func),
                         reads=[('pP', s)], writes=[(key, c)])

            def proj_tm(ws, col0, ntiles, func, dest, key):
                for g in range(ntiles // 4):
                    def f(e, g=g):
                        last = None
                        for q in range(4):
                            t = g * 4 + q
                            last = self.mm_group(e, pV[:, q, :], [(hT[:, kt, t * P:(t + 1) * P], wb[ws][:, kt, col0:col0 + P]) for kt in range(KT)])
                        return last
                    S.op('pe', f, reads=[('whg', ws)], writes=['pV'])
                    S.op('act', lambda e, g=g: e.activation(dest[:, g * 4:(g + 1) * 4, :], pV[:], func), reads=['pV'], writes=[(key, g)])

            import os
            cut = int(os.environ.get("HG_CUT", "9"))

            def gate_pass(hh, di, sg, sgkeys, own, first_state):
                if cut < 2:
                    return
                col = di * 8 + hh
                fg, kk, lf, b, bb = sg, X2, X3, X4, X5
                S.op('dve', lambda e: e.tensor_scalar(fg[:], sg[:], oml[:, col:col + 1], lb[:, col:col + 1], ALU.mult, ALU.add),
                     reads=sgkeys + ['oml', 'lb'], writes=['fg'])
                S.op('dve', lambda e: e.tensor_scalar(kk[:], fg[:], -1.0, 1.0, ALU.mult, ALU.add), reads=['fg'], writes=['X2'])
                S.op('act', lambda e: e.activation(lf[:], fg[:], AF.Ln), reads=['fg'], writes=['X3'])
                S.op('dve', lambda e: e.tensor_tensor_scan(b[:], rmask[:], lf[:], 0.0, ALU.mult, ALU.add), reads=['X3', 'rmask'], writes=['X4'])
                b3 = b[:].rearrange("p (c i) -> p c i", i=64)
                S.op('act', lambda e: e.activation(etot[:].unsqueeze(2), b3[:, :, 63:64], AF.Exp), reads=['X4'], writes=['etot'])
                if di == 0:
                    bbt, bbk = b, 'X4'
                else:
                    S.op('dve', lambda e: e.tensor_tensor(bb[:], lf[:], b[:], ALU.subtract), reads=['X3', 'X4'], writes=['X5'])
                    bb3 = bb[:].rearrange("p (c i) -> p c i", i=64)
                    S.op('dve', lambda e: e.tensor_tensor(bb3, bb3, b3[:, :, 63:64].to_broadcast([P, NCH, 64]), ALU.add), reads=['X5', 'X4'], writes=['X5'])
                    bbt, bbk = bb, 'X5'
                en = lf
                S.op('act', lambda e: e.activation(en[:], bbt[:], AF.Exp, scale=-1.0), reads=[bbk, 'X3'], writes=['X3'])
                S.op('dve', lambda e: e.tensor_tensor(kk[:], kk[:], en[:], ALU.mult), reads=['X2', 'X3'], writes=['X2'])
                if own:
                    S.op('act', lambda e: e.activation(kdec[di][:], kk[:], AF.Copy), reads=['X2'], writes=[('kdec', di)])
                    S.op('act', lambda e: e.activation(en[:], bbt[:], AF.Exp), reads=[bbk, 'X2'], writes=['X3'])
                    S.op('dve', lambda e: e.tensor_tensor(qdec[di][:], qr[:], en[:], ALU.mult), reads=['X3', ('qr', 0), ('qr', 1)], writes=[('qdec', di)])
                kend32 = b if di == 1 else bb
                kkey = 'X4' if di == 1 else 'X5'
                S.op('dve', lambda e: e.tensor_tensor(kend32[:].rearrange("p (c i) -> p c i", i=64), kk[:].rearrange("p (c i) -> p c i", i=64),
                                                      etot[:].unsqueeze(2).to_broadcast([P, NCH, 64]), ALU.mult),
                     reads=['X2', 'etot', 'X4', 'X5'], writes=[kkey])
                if cut < 3:
                    return
                for g in range(2):
                    def ftr(e, g=g):
                        last = None
                        for q in range(4):
                            t = g * 4 + q
                            last = e.transpose(pTk[:, q, :], kend32[:, t * P:(t + 1) * P], self.ident[:])
                        return last
                    S.op('pe', ftr, reads=[kkey], writes=['pTk'])
                    for hf in range(2):
                        S.op('act', lambda e, g=g, hf=hf: e.activation(kend_tm[:, hf, g * 4:(g + 1) * 4, :], pTk[:], AF.Copy, scale=halfm[:, hf:hf + 1]),
                             reads=['pTk', 'halfm'], writes=[('kend_tm', g, hf)])
                if cut < 4:
                    return
                tile0 = 0 if own else NT_OWN
                order = list(range(NCH)) if di == 0 else list(range(NCH - 1, -1, -1))
                started = not first_state
                for gi in range(4):
                    cs = order[gi * 4:(gi + 1) * 4]

                    def fu(e, cs=cs):
                        last = None
                        for q, c in enumerate(cs):
                            t, hf = c // 2, c % 2
                            last = e.matmul(pU[:, q, :], kend_tm[:, hf, t, :], v_tm[:, tile0 + t, :], start=True, stop=True)
                        return last
                    S.op('pe', fu, reads=[('kend_tm', g, hf) for g in range(2) for hf in range(2)] + [('v_tm', g) for g in range(4)], writes=['pU'])
                    for q, c in enumerate(cs):
                        if own:
                            if started:
                                S.op('act', lambda e, c=c, cu=scur[0]: e.activation(Sbf[di][:, c, :], S32[:, cu, :], AF.Copy), reads=[('S32', scur[0])], writes=[('Sbf', di, c)])
                            else:
                                S.op('dve', lambda e, c=c: e.memset(Sbf[di][:, c, :], 0.0), writes=[('Sbf', di, c)])
                        if started:
                            cu = scur[0]
                            S.op('dve', lambda e, q=q, c=c, cu=cu: e.scalar_tensor_tensor(S32[:, 1 - cu, :], S32[:, cu, :], etot[:, c:c + 1], pU[:, q, :], ALU.mult, ALU.add),
                                 reads=[('S32', cu), 'pU', 'etot'], writes=[('S32', 1 - cu)])
                            scur[0] = 1 - cu
                        else:
                            S.op('dve', lambda e, q=q, cu=scur[0]: e.tensor_copy(S32[:, cu, :], pU[:, q, :]), reads=['pU'], writes=[('S32', scur[0])])
                            started = True

            for hh in range(8):
                hs = hh % 2
                S.dma('sp', hnw[hs][:], self.hnw[:, hh * P:(hh + 1) * P].partition_broadcast(P), writes=[('hnw', hs)])
                wsA = load_w(hh, 0)
                proj_fm(wsA, 0, 0, N, AF.Silu, qr, 'qr')
                proj_fm(wsA, P, 0, N, AF.Sigmoid, sgA, 'sgA')
                wsB = load_w(hh, 1)
                proj_tm(wsB, P, NT_ALL, AF.Copy, v_tm, 'v_tm')
                proj_fm(wsB, 0, N, N, AF.Sigmoid, sgB, 'sgB')
                gate_pass(hh, 0, sgA, [('sgA', 0), ('sgA', 1)], True, True)
                gate_pass(hh, 1, sgB, [('sgB', 0), ('sgB', 1)], False, True)
                proj_fm(wsB, 0, 0, N, AF.Sigmoid, sgB, 'sgB')
                wsG = load_w(hh, 2)
                for g in range(4):
                    def fg_(e, g=g, wsG=wsG):
                        last = None
                        for q in range(4):
                            c = g * 4 + q
                            last = self.mm_group(e, pV[0:64, q, :], [(hT[:, kt, c * 64:(c + 1) * 64], wb[wsG][:, kt, 0:P]) for kt in range(KT)])
                        return last
                    S.op('pe', fg_, reads=[('whg', wsG)], writes=['pV'])
                    S.op('act', lambda e, g=g: e.activation(sg_tm[:, g * 4:(g + 1) * 4, :], pV[0:64, :, :], AF.Silu), reads=['pV'], writes=[('sg_tm', g)])
                gate_pass(hh, 1, sgB, [('sgB', 0), ('sgB', 1)], True, False)
                def out_tile(t, pb, hh=hh, hs=hs):
                    tsl = slice(t * P, (t + 1) * P)
                    scm, oh, oh2, junk, sc = scm_[pb], oh_[pb], oh2_[pb], junk_[pb], sc_[pb]

                    def fsc(e, tsl=tsl):
                        e.matmul(pSc[:, 0, :], kdec[0][:, tsl], qdec[0][:, tsl], start=True, stop=True)
                        return e.matmul(pSc[:, 1, :], kdec[1][:, tsl], qdec[1][:, tsl], start=True, stop=True)
                    S.op('pe', fsc, reads=[('kdec', 0), ('kdec', 1), ('qdec', 0), ('qdec', 1)], writes=['pSc'])
                    for di in range(2):
                        S.op('dve', lambda e, di=di: e.tensor_tensor(scm[di][:], pSc[:, di, :], msk[:, di, :], ALU.mult),
                             reads=['pSc', 'msk'], writes=[('scm', pb, di)])

                    def fo(e, t=t, tsl=tsl):
                        last = None
                        for hf in range(2):
                            c = 2 * t + hf
                            csl = slice(hf * 64, (hf + 1) * 64)
                            tk = slice(t * P + hf * 64, t * P + (hf + 1) * 64)
                            e.matmul(pOh[:, hf, :], scm[0][:, csl], v_tm[:, t, :], start=True, stop=False)
                            e.matmul(pOh[:, hf, :], scm[1][:, csl], v_tm[:, t, :], start=False, stop=False)
                            e.matmul(pOh[:, hf, :], qdec[0][:, tk], Sbf[0][:, c, :], start=False, stop=False)
                            last = e.matmul(pOh[:, hf, :], qdec[1][:, tk], Sbf[1][:, c, :], start=False, stop=True)
                        return last
                    S.op('pe', fo, reads=[('scm', pb, 0), ('scm', pb, 1), ('qdec', 0), ('qdec', 1)] + [('Sbf', di, c) for di in range(2) for c in (2 * t, 2 * t + 1)]
                         + [('v_tm', g) for g in range(4)], writes=['pOh'])
                    S.op('act', lambda e: e.activation(oh[:], pOh[:], AF.Copy), reads=['pOh'], writes=[('oh', pb)])
                    for hf in range(2):
                        S.op('act', lambda e, hf=hf: e.activation(junk[:], oh[:, hf, :], AF.Square, accum_out=sc[:, hf:hf + 1]), reads=[('oh', pb)], writes=[('hss', pb, hf), ('hjunk', pb)])
                    S.op('act', lambda e: e.activation(sc[:, 2:4], sc[:, 0:2], AF.Sqrt, bias=self.eps_t[0:64, 0:1], scale=1.0 / 128), reads=[('hss', pb, 0), ('hss', pb, 1)], writes=[('hsd', pb)])
                    S.op('dve', lambda e: e.reciprocal(sc[:, 2:4], sc[:, 2:4]), reads=[('hsd', pb)], writes=[('hrs', pb)])
                    for hf in range(2):
                        S.op('dve', lambda e, hs=hs, hf=hf: e.scalar_tensor_tensor(oh2[:, hf, :], oh[:, hf, :], sc[:, 2 + hf:3 + hf], hnw[hs][0:64, :], ALU.mult, ALU.mult),
                             reads=[('oh', pb), ('hrs', pb), ('hnw', hs)], writes=[('oh2', pb, hf)])
                    S.op('dve', lambda e, t=t: e.tensor_tensor(oh2[:], oh2[:], sg_tm[:, 2 * t:2 * t + 2, :], ALU.mult),
                         reads=[('oh2', pb, 0), ('oh2', pb, 1)] + [('sg_tm', g) for g in range(4)], writes=[('oh2', pb, 0), ('oh2', pb, 1)])
                    if self.debug:
                        for hf in range(2):
                            rs_ = slice(t * P + hf * 64, t * P + (hf + 1) * 64)
                            S.dma('sp', self.dbg_os[rs_, hh * P:(hh + 1) * P], oh[:, hf, :], reads=[('oh', pb)], writes=[('dbgos', hh, t, hf)])
                            S.dma('sp', self.dbg_or[rs_, hh * P:(hh + 1) * P], oh2[:, hf, :], reads=[('oh2', pb, hf)], writes=[('dbgor', hh, t, hf)])
                            dk.extend([('dbgos', hh, t, hf), ('dbgor', hh, t, hf)])

                    def ftx(e):
                        e.transpose(pX[:, 0, :], oh2[:, 0, :], self.ident[0:64, 0:64])
                        return e.transpose(pX[:, 1, :], oh2[:, 1, :], self.ident[0:64, 0:64])
                    S.op('pe', ftx, reads=[('oh2', pb, 0), ('oh2', pb, 1)], writes=['pXh'])
                    S.op('act', lambda e, hh=hh, tsl=tsl: e.activation(self.mixedT[:, 8 + hh, tsl], pX[:].rearrange("p a b -> p (a b)"), AF.Copy),
                         reads=['pXh'], writes=[('mixedT', 8 + hh, tsl.start)])
                for t in range(NT_OWN if cut >= 5 else 0):
                    out_tile(t, t % 2)
            S.emit(final_wait_keys=dk)

    def make_carep(self, S, st):
        nc = self.nc
        u = Sched._uid
        ccol = st.enter_context(nc.sbuf_tensor(f"ccol{u}", [P, KT], F32))
        cact = st.enter_context(nc.sbuf_tensor(f"cact{u}", [P, KT], F32))
        cabf = st.enter_context(nc.sbuf_tensor(f"cabf{u}", [P, KT], BF16))
        carep = st.enter_context(nc.sbuf_tensor(f"carep{u}", [P, KT, P], BF16))
        S.dma('sp', ccol[:], self.c_col, writes=['ccol'])
        S.op('act', lambda e: e.activation(cact[:], ccol[:], AF.Silu), reads=['ccol'], writes=['cact'])
        S.op('dve', lambda e: e.tensor_copy(carep[:], cact[:].unsqueeze(2).to_broadcast([P, KT, P])), reads=['cact'], writes=['carep'])
        S.op('dve', lambda e: e.tensor_copy(cabf[:], cact[:]), reads=['cact'], writes=['cabf'])
        return carep, cabf

    def phase_C0(self):
        nc = self.nc
        with contextlib.ExitStack() as st:
            S = self.S
            carep, cabf = self.make_carep(S, st)
            self.ada_part(S, st, self.w_ada2, self.b_ada2, 3 * D, self.mod2, carep, 2 * D)
            wg = [st.enter_context(nc.sbuf_tensor(f"wg2_{i}", [P, KT, 512], BF16)) for i in range(2)]
            bg = st.enter_context(nc.sbuf_tensor("bg2", [P, KT], F32))
            pg = st.enter_context(nc.psum_tensor("pg2", [P, KT], F32))
            S.dma('sp', bg[:], self.b_g2col, writes=['bg2'])
            for c in range(4):
                s = c % 2
                S.dma('pool', wg[s][:], self.w_ada3[:, c * 512:(c + 1) * 512].rearrange("(kt p) n -> p kt n", p=P), writes=[('wg2', s)])

                def f(e, s=s, c=c):
                    last = None
                    for q in range(4):
                        dt = c * 4 + q
                        last = self.mm_group(e, pg[:, dt:dt + 1], [(wg[s][:, kt, q * P:(q + 1) * P], cabf[:, kt:kt + 1]) for kt in range(KT)])
                    return last
                S.op('pe', f, reads=[('wg2', s), 'cabf'], writes=['pg2'])
            S.op('dve', lambda e: e.tensor_tensor(self.g2col[:], pg[:], bg[:], ALU.add), reads=['pg2', 'bg2'], writes=['g2col'])
            S.emit()

    def phase_C1(self):
        nc = self.nc
        with contextlib.ExitStack() as st:
            S = self.S
            wb = [st.enter_context(nc.sbuf_tensor(f"wo{i}", [P, KT, CW], BF16)) for i in range(3)]
            py = [st.enter_context(nc.psum_tensor(f"py{i}", [P, CW], F32)) for i in range(2)]
            it = 0
            for dc in range(8):
                ws = dc % 3
                S.dma('pool', wb[ws][:], self.w_out_t[dc], writes=[('wo', ws)], max_dma_last_dim=4096)
                for t in range(NT_OWN):
                    s = it % 2
                    it += 1
                    S.op('pe', lambda e, s=s, t=t, ws=ws: self.mm_group(
                        e, py[s][:], [(self.mixedT[:, mt, t * P:(t + 1) * P], wb[ws][:, mt, :]) for mt in range(16)]),
                        reads=[('wo', ws)], writes=[('py', s)])
                    S.op('act', lambda e, s=s, t=t, dc=dc: e.activation(self.acc[:, t, dc * CW:(dc + 1) * CW], py[s][:], AF.Copy),
                         reads=[('py', s)], writes=[('acc', t, dc)])
            S.emit()

    def phase_C2(self):
        nc = self.nc
        ALPHA = float(2.0 ** 0.25)
        with contextlib.ExitStack() as st:
            sb = lambda n, s, d=F32: st.enter_context(nc.sbuf_tensor(n, s, d))
            S = self.S
            mod2 = self.mod2
            g1r, sh2r, sc2r = mod2[:, 0:D], mod2[:, D:2 * D], mod2[:, 2 * D:3 * D]
            ln1 = sb("ln1r", [P, 2, D])
            S.dma('sp', ln1[:, 0, :], self.ln1[0:1, :].partition_broadcast(P), writes=['ln1g'])
            S.dma('sp', ln1[:, 1, :], self.ln1[1:2, :].partition_broadcast(P), writes=['ln1b'])
            wr = sb("wr", [P, KT, 32]); br = sb("br", [P, 32])
            S.dma('sp', wr[:], self.w_r, writes=['wr'])
            S.dma('sp', br[:], self.b_r.partition_broadcast(P), writes=['br'])
            xin = sb("xin_c", [P, D]); tt = sb("tt_c", [P, D]); h2 = sb("h2_c", [P, D])
            h2T32 = xin[:].rearrange("p (k j) -> p k j", k=KT)
            stt = sb("stt_c", [P, 4, 6]); mv = sb("mv_c", [P, 6]); mv2 = sb("mv2_c", [P, 6])
            lg = sb("lg", [P, 32]); top8 = sb("top8", [P, 8]); mask = sb("mask", [P, 32]); ex = sb("ex", [P, 32])
            gt = sb("gt", [P, 32]); sm = sb("sm", [P, 4])
            pT = [st.enter_context(nc.psum_tensor(f"pTc{i}", [P, 8, P], F32)) for i in range(3)]
            pL = st.enter_context(nc.psum_tensor("pL", [P, 32], F32))
            pG = st.enter_context(nc.psum_tensor("pG", [32, P], F32))
            npt = [0]
            dk = []

            def transposes(src, srckey, evac):
                for hf in range(2):
                    pi = npt[0] % 3
                    npt[0] += 1

                    def tr(e, hf=hf, pi=pi):
                        last = None
                        for k8 in range(8):
                            kt = hf * 8 + k8
                            last = e.transpose(pT[pi][:, k8, :], src[:, kt * P:(kt + 1) * P], self.ident[:])
                        return last
                    S.op('pe', tr, reads=[srckey], writes=[('pTc', pi)])
                    evac(hf, pi)

            import os
            ccut = int(os.environ.get("C_CUT", "9"))
            for t in range(NT_OWN):
                tsl = slice(t * P, (t + 1) * P)
                S.dma('sp', xin[:], self.x[tsl, :], writes=['xin'])
                S.op('dve', lambda e, t=t: e.tensor_tensor(tt[:], self.acc[:, t, :], g1r, ALU.mult), reads=[('acc', t)], writes=['tt'])
                S.op('dve', lambda e: e.scalar_tensor_tensor(tt[:], xin[:], ALPHA, tt[:], ALU.mult, ALU.add), reads=['tt', 'xin'], writes=['tt'])
                self.ln_stats(S, stt, mv, tt, 'tt', 'ln1')
                S.op('act', lambda e: e.activation(tt[:], tt[:], AF.Identity, bias=mv[:, 3:4], scale=mv[:, 2:3]),
                     reads=['tt', ('ln1', 'nb'), ('ln1', 'rs')], writes=['tt'])
                S.op('dve', lambda e: e.tensor_tensor(tt[:], tt[:], ln1[:, 0, :], ALU.mult), reads=['tt', 'ln1g'], writes=['tt'])
                S.op('dve', lambda e: e.tensor_tensor(tt[:], tt[:], ln1[:, 1, :], ALU.add), reads=['tt', 'ln1b'], writes=['tt'])
                if self.debug:
                    S.dma('sp', self.dbg_x1[tsl, :], tt[:], reads=['tt'], writes=[('dbgx1', t)]); dk.append(('dbgx1', t))
                if ccut < 5:
                    continue
                self.ln_stats(S, stt, mv2, tt, 'tt', 'ln2h')
                S.op('act', lambda e: e.activation(h2[:], tt[:], AF.Identity, bias=mv2[:, 3:4], scale=mv2[:, 2:3]),
                     reads=['tt', ('ln2h', 'nb'), ('ln2h', 'rs')], writes=['h2'])
                S.op('dve', lambda e: e.tensor_tensor(h2[:], h2[:], sc2r, ALU.mult), reads=['h2'], writes=['h2'])
                S.op('dve', lambda e: e.tensor_tensor(h2[:], h2[:], sh2r, ALU.add), reads=['h2'], writes=['h2'])
                if self.debug:
                    S.dma('sp', self.dbg_h2[tsl, :], h2[:], reads=['h2'], writes=[('dbgh2', t)]); dk.append(('dbgh2', t))

                def evac_h2(hf, pi, t=t, tsl=tsl):
                    S.op('act', lambda e: e.activation(self.h2T[:, hf * 8:(hf + 1) * 8, tsl], pT[pi][:], AF.Copy),
                         reads=[('pTc', pi)], writes=[('h2T', t, hf)])
                    S.op('act', lambda e: e.activation(h2T32[:, hf * 8:(hf + 1) * 8, :], pT[pi][:], AF.Copy),
                         reads=[('pTc', pi), 'xin'], writes=[('h2T32', hf), 'xin'])
                if ccut < 6:
                    continue
                transposes(h2, 'h2', evac_h2)
                if ccut < 7:
                    continue
                S.op('pe', lambda e: self.mm_group(e, pL[:], [(h2T32[:, kt, :], wr[:, kt, :]) for kt in range(KT)]),
                     reads=[('h2T32', 0), ('h2T32', 1), 'wr', 'xin'], writes=['pL'])
                S.op('dve', lambda e: e.tensor_tensor(lg[:], pL[:], br[:], ALU.add), reads=['pL', 'br'], writes=['lg'])
                if self.debug:
                    S.dma('sp', self.dbg_lg[tsl, :], lg[:], reads=['lg'], writes=[('dbglg', t)]); dk.append(('dbglg', t))
                if ccut < 8:
                    continue
                S.op('dve', lambda e: e.max(top8[:], lg[:]), reads=['lg'], writes=['top8'])
                S.op('dve', lambda e: e.tensor_scalar(mask[:], lg[:], top8[:, 3:4], None, ALU.is_ge), reads=['lg', 'top8'], writes=['mask'])
                S.op('dve', lambda e: e.tensor_scalar(sm[:, 0:1], top8[:, 0:1], -1.0, None, ALU.mult), reads=['top8'], writes=['negm'])
                S.op('act', lambda e: e.activation(ex[:], lg[:], AF.Exp, bias=sm[:, 0:1], scale=1.0), reads=['lg', 'negm'], writes=['ex'])
                S.op('dve', lambda e: e.tensor_tensor(ex[:], ex[:], mask[:], ALU.mult), reads=['ex', 'mask'], writes=['ex'])
                S.op('dve', lambda e: e.tensor_reduce(sm[:, 1:2], ex[:], AX.X, ALU.add), reads=['ex'], writes=['den'])
                S.op('dve', lambda e: e.reciprocal(sm[:, 2:3], sm[:, 1:2]), reads=['den'], writes=['rden'])
                S.op('dve', lambda e: e.tensor_scalar(gt[:], ex[:], sm[:, 2:3], None, ALU.mult), reads=['ex', 'rden'], writes=['gt'])
                S.op('pe', lambda e: e.transpose(pG[:], gt[:], self.ident[:]), reads=['gt'], writes=['pG'])
                S.op('act', lambda e, tsl=tsl: e.activation(self.gatesT[:, tsl], pG[:], AF.Copy), reads=['pG'], writes=[('gatesT', t)])
                S.op('dve', lambda e: e.tensor_scalar(tt[:], tt[:], ALPHA, None, ALU.mult), reads=['tt'], writes=['tt'])

                def evac_acc(hf, pi, t=t):
                    S.op('act', lambda e: e.activation(self.acc[:, t, hf * 1024:(hf + 1) * 1024].rearrange("p (k j) -> p k j", k=8), pT[pi][:], AF.Copy),
                         reads=[('pTc', pi)], writes=[('acc', t)])
                transposes(tt, 'tt', evac_acc)
            if self.debug:
                S.dma('sp', self.dbg_gt, self.gatesT[:], reads=[('gatesT', t) for t in range(NT_OWN)], writes=['dbggt']); dk.append('dbggt')
                for t in range(NT_OWN):
                    S.dma('sp', self.dbg_acc[:, t * D:(t + 1) * D], self.acc[:, t, :], reads=[('acc', t)], writes=[('dbgacc', t)]); dk.append(('dbgacc', t))
            S.emit(final_wait_keys=dk)

    def phase_MoE(self):
        nc = self.nc
        with contextlib.ExitStack() as st:
            sb = lambda n, s, d=F32: st.enter_context(nc.sbuf_tensor(n, s, d))
            ps = lambda n, s: st.enter_context(nc.psum_tensor(n, s, F32))
            S = self.S
            h2T, acc, gatesT, g2col = self.h2T, self.acc, self.gatesT, self.g2col
            wb = [sb(f"wmoe{i}", [P, KT, CW], BF16) for i in range(3)]
            actT = sb("actT", [P, 8, S_OWN], BF16)
            glu = [sb(f"glu{i}", [P, 512]) for i in range(2)]
            sig = [sb(f"sig{i}", [P, 512]) for i in range(2)]
            lin1 = [sb(f"lin1{i}", [P, 512]) for i in range(2)]
            tg = [sb(f"tg{i}", [P, 512]) for i in range(2)]
            grep = sb("grep", [P, S_OWN]); gsel = sb("gsel", [32, S_OWN])
            b1 = sb("b1s", [P, 32, 32]); b2s = sb("b2s", [32, D]); ones32 = sb("ones32", [32, P])
            pGL = [ps(f"pGL{i}", [P, 2, 512]) for i in range(2)]
            pY = [ps(f"pY{i}", [P, 512]) for i in range(2)]
            pGR = ps("pGR", [P, 512])
            S.dma('sp', b1[:], self.b1_r, writes=['b1'])
            S.dma('sp', b2s[:], self.b2, writes=['b2s'])
            S.op('dve', lambda e: e.memset(ones32[:], 1.0), writes=['ones32'])
            S.op('dve', lambda e: e.tensor_scalar(b1[:, :, 16:32], b1[:, :, 16:32], 1.0, None, ALU.add), reads=['b1'], writes=['b1'])
            nwb = [0]; ngl = [0]; ny = [0]

            def acc_add(s, dt, th):
                accv = acc[:, 4 * th:4 * th + 4, dt * P:(dt + 1) * P]
                S.op('dve', lambda e: e.scalar_tensor_tensor(accv, pY[s][:].rearrange("p (a b) -> p a b", a=4), g2col[:, dt:dt + 1], accv, ALU.mult, ALU.add),
                     reads=[('pY', s), 'g2col'], writes=[('accT', dt, th)])

            for dt in range(KT):
                for th in range(2):
                    s = ny[0] % 2
                    ny[0] += 1
                    S.op('pe', lambda e, s=s, dt=dt, th=th: e.matmul(pY[s][:], b2s[:, dt * P:(dt + 1) * P], gatesT[:, th * 512:(th + 1) * 512], start=True, stop=True),
                         reads=['b2s'], writes=[('pY', s)])
                    acc_add(s, dt, th)

            for ex in range(self.NE):
                S.op('dve', lambda e, ex=ex: e.tensor_scalar(gsel[:], gatesT[:], self.ident[0:32, ex:ex + 1], None, ALU.mult), reads=[], writes=['gsel'])
                for th in range(2):
                    S.op('pe', lambda e, th=th: e.matmul(pGR[:], ones32[:], gsel[:, th * 512:(th + 1) * 512], start=True, stop=True),
                         reads=['gsel', 'ones32'], writes=['pGR'])
                    S.op('dve', lambda e, th=th: e.tensor_copy(grep[:, th * 512:(th + 1) * 512], pGR[:]), reads=['pGR'], writes=[('grep', th)])
                for g in range(2):
                    for c8 in range(8):
                        c = g * 8 + c8
                        ws = nwb[0] % 3
                        nwb[0] += 1
                        S.dma('pool', wb[ws][:], self.w1_r[ex, c], writes=[('wmoe', ws)], max_dma_last_dim=4096)
                        for th in range(2):
                            s = ngl[0] % 2
                            ngl[0] += 1
                            tks = slice(th * 512, (th + 1) * 512)

                            def fgl(e, s=s, ws=ws, tks=tks):
                                self.mm_group(e, pGL[s][:, 0, :], [(wb[ws][:, kt, 0:P], h2T[:, kt, tks]) for kt in range(KT)])
                                return self.mm_group(e, pGL[s][:, 1, :], [(wb[ws][:, kt, P:2 * P], h2T[:, kt, tks]) for kt in range(KT)])
                            S.op('pe', fgl, reads=[('wmoe', ws)], writes=[('pGL', s)])
                            S.op('dve', lambda e, s=s, ex=ex, c=c: e.tensor_scalar(glu[s][:], pGL[s][:, 0, :], b1[:, ex, c:c + 1], 7.0, ALU.add, ALU.min),
                                 reads=[('pGL', s), 'b1'], writes=[('glu', s)])
                            S.op('act', lambda e, s=s: e.activation(sig[s][:], glu[s][:], AF.Sigmoid, scale=1.702), reads=[('glu', s)], writes=[('sig', s)])
                            S.op('dve', lambda e, s=s, ex=ex, c=c: e.tensor_scalar(lin1[s][:], pGL[s][:, 1, :], b1[:, ex, 16 + c:17 + c], 8.0, ALU.add, ALU.min),
                                 reads=[('pGL', s), 'b1'], writes=[('lin1', s)])
                            S.op('dve', lambda e, s=s: e.tensor_tensor(tg[s][:], glu[s][:], sig[s][:], ALU.mult), reads=[('glu', s), ('sig', s)], writes=[('tg', s)])
                            S.op('dve', lambda e, s=s: e.scalar_tensor_tensor(tg[s][:], lin1[s][:], -6.0, tg[s][:], ALU.max, ALU.mult),
                                 reads=[('lin1', s), ('tg', s)], writes=[('tg', s)])
                            S.op('dve', lambda e, s=s, c8=c8, tks=tks, th=th: e.tensor_tensor(actT[:, c8, tks], tg[s][:], grep[:, tks], ALU.mult),
                                 reads=[('tg', s), ('grep', th)], writes=[('actT', c8, th)])
                    for dc in range(4):
                        ws = nwb[0] % 3
                        nwb[0] += 1
                        wv = wb[ws][:].rearrange("p a b -> p (a b)").rearrange("p (f d) -> p f d", f=8)
                        S.dma('pool', wv, self.w2_r[ex, g, dc], writes=[('wmoe', ws)], max_dma_last_dim=4096)
                        for dsub in range(4):
                            dt = dc * 4 + dsub
                            for th in range(2):
                                s = ny[0] % 2
                                ny[0] += 1
                                S.op('pe', lambda e, s=s, wv=wv, dsub=dsub, th=th: self.mm_group(
                                    e, pY[s][:], [(wv[:, ft, dsub * P:(dsub + 1) * P], actT[:, ft, th * 512:(th + 1) * 512]) for ft in range(8)]),
                                    reads=[('wmoe', ws)] + [('actT', ft, th) for ft in range(8)], writes=[('pY', s)])
                                acc_add(s, dt, th)
            if self.debug:
                dk = []
                for t in range(NT_OWN):
                    S.dma('sp', self.dbg_acc2[:, t * D:(t + 1) * D], acc[:, t, :], reads=[('accT', dt, th) for dt in range(KT) for th in range(2)], writes=[('dbgacc2', t)])
                    dk.append(('dbgacc2', t))
                S.emit(final_wait_keys=dk)
            else:
                S.emit()

    def phase_final(self):
        nc = self.nc
        with contextlib.ExitStack() as st:
            sb = lambda n, s, d=F32: st.enter_context(nc.sbuf_tensor(n, s, d))
            S = self.S
            acc = self.acc
            ln2 = sb("ln2r", [P, 2, D])
            S.dma('sp', ln2[:, 0, :], self.ln2[0:1, :].partition_broadcast(P), writes=['ln2g'])
            S.dma('sp', ln2[:, 1, :], self.ln2[1:2, :].partition_broadcast(P), writes=['ln2b'])
            xo = [sb(f"xo{i}", [P, D]) for i in range(2)]
            stt = [sb(f"stt_f{i}", [P, 4, 6]) for i in range(2)]
            mv = [sb(f"mv_f{i}", [P, 6]) for i in range(2)]
            pT = [st.enter_context(nc.psum_tensor(f"pTf{i}", [P, 8, P], F32)) for i in range(3)]
            npt = 0
            dk = []
            for t in range(NT_OWN):
                s = t % 2
                for hf in range(2):
                    pi = npt % 3
                    npt += 1

                    def tr(e, hf=hf, pi=pi, t=t):
                        last = None
                        for k8 in range(8):
                            dt = hf * 8 + k8
                            last = e.transpose(pT[pi][:, k8, :], acc[:, t, dt * P:(dt + 1) * P], self.ident[:])
                        return last
                    S.op('pe', tr, reads=[], writes=[('pTf', pi)])
                    S.op('act', lambda e, s=s, hf=hf, pi=pi: e.activation(xo[s][:, hf * 1024:(hf + 1) * 1024].rearrange("p (k j) -> p k j", k=8), pT[pi][:], AF.Copy),
                         reads=[('pTf', pi)], writes=[('xo', s)])
                self.ln_stats(S, stt[s], mv[s], xo[s], ('xo', s), ('lnF', s))
                S.op('act', lambda e, s=s: e.activation(xo[s][:], xo[s][:], AF.Identity, bias=mv[s][:, 3:4], scale=mv[s][:, 2:3]),
                     reads=[('xo', s), (('lnF', s), 'nb'), (('lnF', s), 'rs')], writes=[('xo', s)])
                S.op('dve', lambda e, s=s: e.tensor_tensor(xo[s][:], xo[s][:], ln2[:, 0, :], ALU.mult), reads=[('xo', s), 'ln2g'], writes=[('xo', s)])
                S.op('dve', lambda e, s=s: e.tensor_tensor(xo[s][:], xo[s][:], ln2[:, 1, :], ALU.add), reads=[('xo', s), 'ln2b'], writes=[('xo', s)])
                S.dma('sp', self.y[t * P:(t + 1) * P, :], xo[s][:], reads=[('xo', s)], writes=[('y', t)])
                dk.append(('y', t))
            S.emit(final_wait_keys=dk)


def rope_tables():
    S = 2048
    t = np.arange(S)
    row = (t // 64 - (S // 64) // 2).astype(np.float32)
    col = (t % 64 - 32).astype(np.float32)
    inv = (10000.0 ** (-np.arange(0, 64, 2, dtype=np.float32) / 64.0)).astype(np.float32)
    ar = row[:, None] * inv[None, :]
    ac = col[:, None] * inv[None, :]
    cos = np.concatenate([np.cos(ar), np.cos(ar), np.cos(ac), np.cos(ac)], axis=1)
    sin = np.concatenate([-np.sin(ar), np.sin(ar), -np.sin(ac), np.sin(ac)], axis=1)
    return cos.astype(np.float32), sin.astype(np.float32)


def tile_w(w):
    K, N = w.shape
    return np.ascontiguousarray(w.reshape(KT, P, N // CW, CW).transpose(2, 1, 0, 3))


def prep_shared(inp, n_experts=32):
    l = 0
    NE = n_experts
    w1 = inp["w_exp_in"][l][:NE]
    w1 = np.ascontiguousarray(w1.reshape(NE, KT, P, 2, 16, P).transpose(0, 4, 2, 1, 3, 5)).reshape(NE, 16, P, KT, CW)
    w2 = inp["w_exp_out"][l][:NE]
    w2 = np.ascontiguousarray(w2.reshape(NE, 2, 8, P, 4, 512).transpose(0, 1, 4, 3, 2, 5))
    b1 = np.ascontiguousarray(inp["b_exp_in"][l].reshape(32, 32, P).transpose(2, 0, 1))
    return {"w1_r": w1, "w2_r": w2, "b1_r": b1, "b2": np.ascontiguousarray(inp["b_exp_out"][l]),
            "ln2": np.ascontiguousarray(np.stack([inp["ln2_g"][l], inp["ln2_b"][l]]))}


def prep_core(inp, core):
    b, half = core // 2, core % 2
    l = 0
    x = inp["x"][b]
    cos, sin = rope_tables()
    if half == 1:
        x = x[::-1]
        cos, sin = cos[::-1], sin[::-1]
    w_in = inp["w_in"][l]
    m = {
        "x_loc": np.ascontiguousarray(x),
        "c_col": np.ascontiguousarray(inp["c"][b].reshape(KT, P).T),
        "w_ada1": np.ascontiguousarray(inp["w_ada"][l][:, 0:2 * D]),
        "b_ada1": np.ascontiguousarray(inp["b_ada"][l][None, 0:2 * D]),
        "ident": np.eye(P, dtype=np.float32),
        "w_qkv": tile_w(w_in[:, 0:1536]),
        "qkw": np.stack([inp["q_norm_w"][l] * np.float32(1.0), inp["k_norm_w"][l]]).astype(np.float32),
        "cs": np.ascontiguousarray(np.stack([cos, sin])),
        "anw": np.ascontiguousarray(inp["attn_norm_w"][l][None, :]),
    }
    o_qr, o_ffw, o_fbw, o_i, o_g = 1536, 2560, 3584, 4608, 5632
    o_fa, o_fb = (o_ffw, o_fbw) if half == 0 else (o_fbw, o_ffw)
    cols = []
    for hh in range(8):
        sl = lambda o: w_in[:, o + hh * P:o + (hh + 1) * P]
        cols += [sl(o_qr), sl(o_fa), sl(o_fb), sl(o_i), sl(o_g), sl(o_g)]
    m["w_hg"] = tile_w(np.concatenate(cols, axis=1)).reshape(8, 3, P, KT, CW)
    lbr = inp["hgrn_lb"]
    dirs = (0, 1) if half == 0 else (1, 0)
    la = np.stack([np.concatenate([lbr[dirs[0], sl_].reshape(8, P).T, lbr[dirs[1], sl_].reshape(8, P).T], axis=1) for sl_ in range(2)])
    m["lb_a"] = np.ascontiguousarray(la.astype(np.float32))
    m["hnw"] = np.ascontiguousarray(inp["hgrn_norm_w"][l][None, :])
    jj, ii = np.meshgrid(np.arange(P), np.arange(P), indexing="ij")
    same = (jj // 64) == (ii // 64)
    m["hmask"] = np.stack([(same & (jj <= ii)), (same & (jj >= ii))]).astype(np.float32)
    m["rmask"] = (np.arange(S_OWN) % 64 != 0).astype(np.float32)[None, :]
    w_ada, b_ada = inp["w_ada"][l], inp["b_ada"][l]
    m["w_ada2"] = np.ascontiguousarray(w_ada[:, 2 * D:5 * D])
    m["b_ada2"] = np.ascontiguousarray(b_ada[None, 2 * D:5 * D])
    m["w_ada3"] = np.ascontiguousarray(w_ada[:, 5 * D:6 * D])
    m["b_g2col"] = np.ascontiguousarray(b_ada[5 * D:6 * D].reshape(KT, P).T)
    m["w_out_t"] = tile_w(inp["w_out"][l])
    m["ln1"] = np.ascontiguousarray(np.stack([inp["ln1_g"][l], inp["ln1_b"][l]]))
    m["w_r"] = np.ascontiguousarray(inp["w_router"][l].reshape(KT, P, 32).transpose(1, 0, 2))
    m["b_r"] = np.ascontiguousarray(inp["b_router"][l][None, :])
    m["halfm"] = np.stack([(np.arange(P) < 64), (np.arange(P) >= 64)], axis=1).astype(np.float32)
    return m


def kernel(**inputs):
    inp = {k: np.asarray(v) for k, v in inputs.items()}
    n = 8
    prog = Prog(['attn', 'hgrn', 'c', 'moe'], debug=False)
    shared = prep_shared(inp)
    in_maps = []
    for c in range(n):
        m = prep_core(inp, c)
        m.update(shared)
        in_maps.append(m)
    res = run_bass_kernel_spmd(prog.nc, in_maps, core_ids=list(range(n)))
    out = np.zeros((4, 2048, 2048), np.float32)
    for c in range(n):
        b, half = c // 2, c % 2
        y = np.asarray(res.results[c]["y_out"], dtype=np.float32)
        if half == 0:
            out[b, 0:1024] = y
        else:
            out[b, 1024:2048] = y[::-1]
    return out
```

```python
import contextlib
import numpy as np
import concourse.bass as bass
import concourse.mybir as mybir
from concourse.bass_utils import run_bass_kernel_spmd

F32 = mybir.dt.float32
BF16 = mybir.dt.bfloat16
AF = mybir.ActivationFunctionType
ALU = mybir.AluOpType
AX = mybir.AxisListType

COMPUTE = ('pe', 'act', 'dve')
DMAQ = ('sp', 'pool')
ALLENG = COMPUTE + DMAQ
NDS = 8
SAME_ENGINE_SYNC = True


class Sched:
    _uid = 0

    def __init__(self, nc):
        self.nc = nc
        Sched._uid += 1
        self.ops = {e: [] for e in ALLENG}
        self.n = {e: 0 for e in ALLENG}
        self.lastw = {}
        self.readers = {}
        self.seen = {e: {} for e in ALLENG}

    @staticmethod
    def _tgt(eng, idx):
        if eng in DMAQ:
            return (eng, (idx - 1) % NDS), 16 * ((idx - 1) // NDS + 1)
        return (eng, 0), idx

    def op(self, eng, fn, reads=(), writes=()):
        deps = {}

        def add(t, v):
            if deps.get(t, 0) < v:
                deps[t] = v

        for k in reads:
            if k in self.lastw:
                add(*self.lastw[k])
        for k in writes:
            if k in self.lastw:
                add(*self.lastw[k])
            for t, v in self.readers.get(k, {}).items():
                add(t, v)
        idx = None
        if fn is not None:
            self.n[eng] += 1
            idx = self.n[eng]
            if eng in DMAQ and idx > NDS:
                add(*self._tgt(eng, idx - NDS))
        waits = []
        for t, v in deps.items():
            if t[0] == eng and eng == 'pe':
                continue
            if t[0] == eng and eng in COMPUTE and not SAME_ENGINE_SYNC:
                continue
            if self.seen[eng].get(t, 0) >= v:
                continue
            self.seen[eng][t] = v
            waits.append((t, v))
        self.ops[eng].append((waits, fn, idx))
        if fn is None:
            return
        me = self._tgt(eng, idx)
        for k in writes:
            self.lastw[k] = me
            self.readers[k] = {}
        for k in reads:
            if k not in writes:
                r = self.readers.setdefault(k, {})
                if r.get(me[0], 0) < me[1]:
                    r[me[0]] = me[1]

    def dma(self, q, out, in_, reads=(), writes=(), **kw):
        self.op(q, lambda e: e.dma_start(out=out, in_=in_, **kw), reads=reads, writes=writes)

    def alloc_sems(self, stack):
        nc = self.nc
        self.sems = {}
        for e in COMPUTE:
            self.sems[(e, 0)] = stack.enter_context(nc.semaphore(f"s_{e}"))
        for e in DMAQ:
            for s in range(NDS):
                self.sems[(e, s)] = stack.enter_context(nc.semaphore(f"s_{e}{s}"))

    def emit(self, final_wait_keys=()):
        nc = self.nc
        Sched._uid += 1
        self.op('sp', None, reads=list(final_wait_keys))
        finals = []
        for e in ALLENG:
            if self.n[e] == 0:
                continue
            if e in DMAQ:
                for s in range(min(NDS, self.n[e])):
                    last = ((self.n[e] - 1 - s) // NDS) * NDS + s + 1
                    finals.append(self._tgt(e, last))
            else:
                finals.append(self._tgt(e, self.n[e]))
        for e in ALLENG:
            w = []
            for (t, v) in finals:
                if self.seen[e].get(t, 0) < v and not (t[0] == e and e == 'pe'):
                    w.append((t, v))
                    self.seen[e][t] = v
            self.ops[e].append((w, None, None))
        sems = self.sems
        ops = self.ops
        self.ops = {e: [] for e in ALLENG}
        with nc.Block() as blk:
            def run(eng, h):
                for waits, fn, idx in ops[eng]:
                    for t, v in waits:
                        h.wait_ge(sems[t], v)
                    if fn is not None:
                        ins = fn(h)
                        t, _ = self._tgt(eng, idx)
                        ins.then_inc(sems[t], 16 if eng in DMAQ else 1)

            blk.tensor(lambda h: run('pe', h))
            blk.scalar(lambda h: run('act', h))
            blk.vector(lambda h: run('dve', h))
            blk.gpsimd(lambda h: run('pool', h))
            blk.sync(lambda h: run('sp', h))


P = 128
D = 2048
KT = 16
S_ALL = 2048
S_OWN = 1024
NT_ALL = 16
NT_OWN = 8
CW = 256
EPS = 1e-6


class Prog:
    def __init__(self, stages, debug=False, n_experts=32):
        self.NE = n_experts
        self.stages = stages
        self.debug = debug
        nc = self.nc = bass.Bass("TRN2", target_bir_lowering=False)
        di = lambda n, s: nc.dram_tensor(n, s, F32, kind="ExternalInput").ap()
        self.x = di("x_loc", [S_ALL, D])
        self.c_col = di("c_col", [P, KT])
        self.w_ada1 = di("w_ada1", [D, 2 * D])
        self.b_ada1 = di("b_ada1", [1, 2 * D])
        self.ident_d = di("ident", [P, P])
        self.w_qkv = di("w_qkv", [6, P, KT, CW])
        self.qkw = di("qkw", [2, P])
        self.cs = di("cs", [2, S_ALL, P])
        self.anw = di("anw", [1, 1024])
        self.y = nc.dram_tensor("y_out", [S_OWN, D], F32, kind="ExternalOutput").ap()
        self.w_hg = di("w_hg", [8, 3, P, KT, CW])
        self.lb_a = di("lb_a", [2, P, 16])
        self.hnw = di("hnw", [1, 1024])
        self.hmask = di("hmask", [2, P, P])
        self.rmask_d = di("rmask", [1, S_OWN])
        self.halfm_d = di("halfm", [P, 2])
        self.w_ada2 = di("w_ada2", [D, 3 * D])
        self.b_ada2 = di("b_ada2", [1, 3 * D])
        self.w_ada3 = di("w_ada3", [D, D])
        self.b_g2col = di("b_g2col", [P, KT])
        self.w_out_t = di("w_out_t", [8, P, KT, CW])
        self.ln1 = di("ln1", [2, D])
        self.w_r = di("w_r", [P, KT, 32])
        self.b_r = di("b_r", [1, 32])
        self.w1_r = di("w1_r", [self.NE, 16, P, KT, CW])
        self.w2_r = di("w2_r", [self.NE, 2, 4, P, 8, 512])
        self.b1_r = di("b1_r", [P, 32, 32])
        self.b2 = di("b2", [32, D])
        self.ln2 = di("ln2", [2, D])
        if debug:
            do = lambda n, s: nc.dram_tensor(n, s, F32, kind="ExternalOutput").ap()
            self.dbg_h = do("dbg_h", [S_ALL, D])
            self.dbg_q = do("dbg_q", [S_OWN, 1024])
            self.dbg_k = do("dbg_k", [S_ALL, 256])
            self.dbg_oa = do("dbg_oa", [S_OWN, 1024])
            self.dbg_mod = do("dbg_mod", [P, 2 * D])
            self.dbg_or = do("dbg_or", [S_OWN, 1024])
            self.dbg_os = do("dbg_os", [S_OWN, 1024])
            self.dbg_x1 = do("dbg_x1", [S_OWN, D])
            self.dbg_h2 = do("dbg_h2", [S_OWN, D])
            self.dbg_lg = do("dbg_lg", [S_OWN, 32])
            self.dbg_gt = do("dbg_gt", [32, S_OWN])
            self.dbg_acc = do("dbg_acc", [P, 8 * D])
            self.dbg_acc2 = do("dbg_acc2", [P, 8 * D])
        self.build()

    def mm_group(self, e, out, pairs):
        last = None
        n = len(pairs)
        for i, (l, r) in enumerate(pairs):
            last = e.matmul(out, l, r, start=(i == 0), stop=(i == n - 1))
        return last

    def build(self):
        nc = self.nc
        with contextlib.ExitStack() as top:
            sb = lambda n, s, d=F32: top.enter_context(nc.sbuf_tensor(n, s, d))
            self.S = Sched(nc)
            self.S.alloc_sems(top)
            self.ident = sb("ident_s", [P, P])
            self.eps_t = sb("eps_t", [P, 1])
            self.gatesT = sb("gatesT", [32, S_OWN])
            self.g2col = sb("g2col", [P, KT])
            self.R1 = sb("R1", [P, KT, S_ALL], BF16)
            self.hT = self.R1
            self.acc = self.R1[:].rearrange("p a b -> p (a b)").bitcast(F32).rearrange("p (t d) -> p t d", t=NT_OWN)
            self.R2 = sb("R2", [P, 16, S_OWN], BF16)
            self.mixedT = self.R2
            self.h2T = self.R2
            self.phase_A()
            if 'attn' in self.stages:
                self.phase_B1()
            if 'hgrn' in self.stages:
                self.phase_B2()
            if 'c' in self.stages:
                import os
                ccut = int(os.environ.get("C_CUT", "9"))
                self.phase_C1()
                with contextlib.ExitStack() as cs:
                    self.mod2 = cs.enter_context(nc.sbuf_tensor("mod2", [P, 3 * D], F32))
                    if ccut >= 2:
                        self.phase_C0()
                    if ccut >= 3:
                        self.phase_C2()
            if 'moe' in self.stages:
                self.phase_MoE()
                self.phase_final()

    def ada_part(self, S, st, w_ap, b_ap, ncols, dest, carep, plus_one_from):
        nc = self.nc
        wbuf = [st.enter_context(nc.sbuf_tensor(f"adaw{i}_{Sched._uid}", [P, KT, 512], BF16)) for i in range(2)]
        bb = [st.enter_context(nc.sbuf_tensor(f"adab{i}_{Sched._uid}", [P, 512], F32)) for i in range(2)]
        ps = [st.enter_context(nc.psum_tensor(f"adap{i}_{Sched._uid}", [P, 512], F32)) for i in range(2)]
        for c in range(ncols // 512):
            s = c % 2
            S.dma('pool', wbuf[s][:], w_ap[:, c * 512:(c + 1) * 512].rearrange("(kt p) n -> p kt n", p=P),
                  writes=[('adaw', s)])
            S.dma('sp', bb[s][:], b_ap[:, c * 512:(c + 1) * 512].partition_broadcast(P), writes=[('adab', s)])
            S.op('pe', lambda e, s=s: self.mm_group(e, ps[s][:], [(carep[:, kt, :], wbuf[s][:, kt, :]) for kt in range(KT)]),
                 reads=[('adaw', s), 'carep'], writes=[('adap', s)])
            d = dest[:, c * 512:(c + 1) * 512]
            if c * 512 >= plus_one_from:
                S.op('dve', lambda e, s=s, d=d: e.scalar_tensor_tensor(d, ps[s][:], 1.0, bb[s][:], ALU.add, ALU.add),
                     reads=[('adap', s), ('adab', s)], writes=[('mod', c)])
            else:
                S.op('dve', lambda e, s=s, d=d: e.tensor_tensor(d, ps[s][:], bb[s][:], ALU.add),
                     reads=[('adap', s), ('adab', s)], writes=[('mod', c)])

    def ln_stats(self, S, st_t, mv_t, src, key_src, tag):
        def f(e):
            last = None
            for j in range(4):
                last = e.bn_stats(st_t[:, j, :], src[:, j * 512:(j + 1) * 512])
            return last
        S.op('dve', f, reads=[key_src], writes=[(tag, 'st')])
        S.op('dve', lambda e: e.bn_aggr(mv_t[:, 0:2], st_t[:].rearrange("p a b -> p (a b)")),
             reads=[(tag, 'st')], writes=[(tag, 'mv')])
        S.op('act', lambda e: e.activation(mv_t[:, 2:3], mv_t[:, 1:2], AF.Sqrt, bias=self.eps_t[:, 0:1], scale=1.0),
             reads=[(tag, 'mv')], writes=[(tag, 'sd')])
        r, v, t = mv_t[:, 2:3], mv_t[:, 4:5], mv_t[:, 5:6]
        S.op('dve', lambda e: e.reciprocal(r, r), reads=[(tag, 'sd')], writes=[(tag, 'rs')])
        S.op('dve', lambda e: e.tensor_scalar(v, mv_t[:, 1:2], EPS, None, ALU.add), reads=[(tag, 'mv')], writes=[(tag, 'v')])
        for _ in range(1):
            S.op('dve', lambda e: e.tensor_tensor(t, r, r, ALU.mult), reads=[(tag, 'rs')], writes=[(tag, 't')])
            S.op('dve', lambda e: e.tensor_scalar(t, t, v, -0.5, ALU.mult, ALU.mult), reads=[(tag, 't'), (tag, 'v')], writes=[(tag, 't')])
            S.op('dve', lambda e: e.scalar_tensor_tensor(r, t, 1.5, r, ALU.add, ALU.mult), reads=[(tag, 't'), (tag, 'rs')], writes=[(tag, 'rs')])
        S.op('dve', lambda e: e.tensor_scalar(mv_t[:, 3:4], mv_t[:, 0:1], mv_t[:, 2:3], -1.0, ALU.mult, ALU.mult),
             reads=[(tag, 'rs'), (tag, 'mv')], writes=[(tag, 'nb')])

    def phase_A(self):
        nc = self.nc
        with contextlib.ExitStack() as st:
            sb = lambda n, s, d=F32: st.enter_context(nc.sbuf_tensor(n, s, d))
            S = self.S
            S.op('dve', lambda e: e.memset(self.eps_t[:], EPS), writes=['eps'])
            S.dma('sp', self.ident[:], self.ident_d, writes=['ident'])
            carep = self.make_carep(S, st)[0]
            mod1 = sb("mod1", [P, 2 * D])
            self.ada_part(S, st, self.w_ada1, self.b_ada1, 2 * D, mod1, carep, D)
            modkeys = [('mod', c) for c in range(8)]
            if self.debug:
                S.dma('sp', self.dbg_mod, mod1[:], reads=modkeys, writes=['dbg_mod'])
            xin = [sb(f"xin{i}", [P, D]) for i in range(2)]
            hn = [sb(f"hn{i}", [P, D]) for i in range(2)]
            stt = [sb(f"stt{i}", [P, 4, 6]) for i in range(2)]
            mv = [sb(f"mv{i}", [P, 6]) for i in range(2)]
            pT = [st.enter_context(nc.psum_tensor(f"pT{i}", [P, 8, P], F32)) for i in range(3)]
            npt = 0
            for t in range(NT_ALL):
                s = t % 2
                S.dma('sp', xin[s][:], self.x[t * P:(t + 1) * P, :], writes=[('xin', s)])
                self.ln_stats(S, stt[s], mv[s], xin[s], ('xin', s), ('lnA', s))
                S.op('act', lambda e, s=s: e.activation(hn[s][:], xin[s][:], AF.Identity, bias=mv[s][:, 3:4], scale=mv[s][:, 2:3]),
                     reads=[('xin', s), (('lnA', s), 'nb'), (('lnA', s), 'rs')], writes=[('hn', s)])
                S.op('dve', lambda e, s=s: e.tensor_tensor(hn[s][:], hn[s][:], mod1[:, D:2 * D], ALU.mult),
                     reads=[('hn', s)] + modkeys, writes=[('hn', s)])
                S.op('dve', lambda e, s=s: e.tensor_tensor(hn[s][:], hn[s][:], mod1[:, 0:D], ALU.add),
                     reads=[('hn', s)] + modkeys, writes=[('hn', s)])
                if self.debug:
                    S.dma('sp', self.dbg_h[t * P:(t + 1) * P, :], hn[s][:], reads=[('hn', s)], writes=[('dbg_h', t)])
                for hf in range(2):
                    pi = npt % 3
                    npt += 1

                    def tr(e, s=s, hf=hf, pi=pi):
                        last = None
                        for k8 in range(8):
                            kt = hf * 8 + k8
                            last = e.transpose(pT[pi][:, k8, :], hn[s][:, kt * P:(kt + 1) * P], self.ident[:])
                        return last
                    S.op('pe', tr, reads=[('hn', s), 'ident'], writes=[('pT', pi)])
                    S.op('act', lambda e, t=t, hf=hf, pi=pi: e.activation(self.hT[:, hf * 8:(hf + 1) * 8, t * P:(t + 1) * P], pT[pi][:], AF.Copy),
                         reads=[('pT', pi)], writes=[('hT', t, hf)])
            S.emit(final_wait_keys=[('dbg_h', t) for t in range(NT_ALL)] if self.debug else [])

    def phase_B1(self):
        nc = self.nc
        with contextlib.ExitStack() as st:
            sb = lambda n, s, d=F32: st.enter_context(nc.sbuf_tensor(n, s, d))
            qT = sb("qT", [P, 8, S_OWN], BF16)
            kT = sb("kT", [P, 2, S_ALL], BF16)
            v_aug = sb("v_aug", [P, NT_ALL, 2, 132], BF16)
            self.B1a(qT, kT, v_aug)
            self.B1b(qT, kT, v_aug)

    def B1a(self, qT, kT, v_aug):
        nc = self.nc
        with contextlib.ExitStack() as st:
            sb = lambda n, s, d=F32: st.enter_context(nc.sbuf_tensor(n, s, d))
            S = self.S
            S.op('dve', lambda e: e.memset(v_aug[:, :, :, 128:129], 1.0), writes=['vones'])
            nw = sb("nw", [P, 2, P])
            S.dma('sp', nw[:, 0, :], self.qkw[0:1, :].partition_broadcast(P), writes=['nw0'])
            S.dma('sp', nw[:, 1, :], self.qkw[1:2, :].partition_broadcast(P), writes=['nw1'])
            wb = [sb(f"wqkv{i}", [P, KT, CW], BF16) for i in range(3)]
            cst = [sb(f"cst{i}", [P, 2, P]) for i in range(2)]
            sq = [sb(f"sq{i}", [P, 2, P]) for i in range(2)]
            qn = [sb(f"qn{i}", [P, 2, P]) for i in range(2)]
            t1 = [sb(f"t1{i}", [P, 2, P]) for i in range(2)]
            t2 = [sb(f"t2{i}", [P, 2, P]) for i in range(2)]
            ss = [sb(f"ss{i}", [P, 4]) for i in range(2)]
            pq = [st.enter_context(nc.psum_tensor(f"pq{i}", [P, CW], F32)) for i in range(2)]
            ptr = [st.enter_context(nc.psum_tensor(f"ptr{i}", [P, 2, P], F32)) for i in range(2)]
            it = 0
            for j in range(6):
                ws = j % 3
                S.dma('pool', wb[ws][:], self.w_qkv[j], writes=[('wb', ws)], max_dma_last_dim=4096)
                ntiles = NT_OWN if j < 4 else NT_ALL
                for t in range(ntiles):
                    s = it % 2
                    it += 1
                    S.op('pe', lambda e, s=s, t=t, ws=ws: self.mm_group(
                        e, pq[s][:], [(self.hT[:, kt, t * P:(t + 1) * P], wb[ws][:, kt, :]) for kt in range(KT)]),
                        reads=[('wb', ws), ('hT', t, 0), ('hT', t, 1)], writes=[('pq', s)])
                    pq3 = pq[s][:].rearrange("p (h d) -> p h d", h=2)
                    if j == 5:
                        S.op('act', lambda e, t=t, pq3=pq3: e.activation(v_aug[:, t, :, 0:128], pq3, AF.Copy),
                             reads=[('pq', s)], writes=[('v', t)])
                        continue
                    wi = 0 if j < 4 else 1
                    S.dma('sp', cst[s][:], self.cs[:, t * P:(t + 1) * P, :].rearrange("c t d -> t c d"), writes=[('cst', s)])
                    S.op('act', lambda e, s=s: e.activation(sq[s][:].rearrange("p h d -> p (h d)"), pq[s][:], AF.Square),
                         reads=[('pq', s)], writes=[('sq', s)])
                    S.op('dve', lambda e, s=s: e.tensor_reduce(ss[s][:, 0:2], sq[s][:], AX.X, ALU.add),
                         reads=[('sq', s)], writes=[('ss', s)])
                    S.op('act', lambda e, s=s: e.activation(ss[s][:, 2:4], ss[s][:, 0:2], AF.Sqrt, bias=self.eps_t[:, 0:1], scale=1.0 / 128),
                         reads=[('ss', s)], writes=[('sd', s)])
                    S.op('dve', lambda e, s=s: e.reciprocal(ss[s][:, 2:4], ss[s][:, 2:4]), reads=[('sd', s)], writes=[('rs', s)])
                    S.op('dve', lambda e, s=s, pq3=pq3: e.tensor_tensor(qn[s][:], pq3, ss[s][:, 2:4].unsqueeze(2).to_broadcast([P, 2, P]), ALU.mult),
                         reads=[('pq', s), ('rs', s)], writes=[('qn', s)])
                    S.op('dve', lambda e, s=s, wi=wi: e.tensor_tensor(qn[s][:], qn[s][:], nw[:, wi:wi + 1, :].to_broadcast([P, 2, P]), ALU.mult),
                         reads=[('qn', s), 'nw0', 'nw1'], writes=[('qn', s)])
                    S.op('dve', lambda e, s=s: e.tensor_tensor(t1[s][:], qn[s][:], cst[s][:, 0:1, :].to_broadcast([P, 2, P]), ALU.mult),
                         reads=[('qn', s), ('cst', s)], writes=[('t1', s)])
                    v5 = lambda a: a.rearrange("p h (a f d) -> p h a f d", a=2, f=2)
                    for f in range(2):
                        S.op('dve', lambda e, s=s, f=f: e.tensor_tensor(
                            v5(t2[s][:])[:, :, :, f, :], v5(qn[s][:])[:, :, :, 1 - f, :],
                            cst[s][:, 1, :].rearrange("p (a f d) -> p a f d", a=2, f=2)[:, :, f, :].unsqueeze(1).to_broadcast([P, 2, 2, 32]),
                            ALU.mult), reads=[('qn', s), ('cst', s)], writes=[('t2', s, f)])
                    S.op('dve', lambda e, s=s: e.tensor_tensor(t1[s][:], t1[s][:], t2[s][:], ALU.add),
                         reads=[('t1', s), ('t2', s, 0), ('t2', s, 1)], writes=[('t1', s)])
                    if self.debug:
                        dst = self.dbg_q[t * P:(t + 1) * P, j * CW:(j + 1) * CW] if j < 4 else self.dbg_k[t * P:(t + 1) * P, :]
                        S.dma('sp', dst, t1[s][:].rearrange("p h d -> p (h d)"), reads=[('t1', s)], writes=[('dbgqk', j, t)])

                    def tr(e, s=s):
                        e.transpose(ptr[s][:, 0, :], t1[s][:, 0, :], self.ident[:])
                        return e.transpose(ptr[s][:, 1, :], t1[s][:, 1, :], self.ident[:])
                    S.op('pe', tr, reads=[('t1', s)], writes=[('ptr', s)])
                    dstT = qT[:, 2 * j:2 * j + 2, t * P:(t + 1) * P] if j < 4 else kT[:, :, t * P:(t + 1) * P]
                    S.op('act', lambda e, s=s, dstT=dstT: e.activation(dstT, ptr[s][:], AF.Copy),
                         reads=[('ptr', s)], writes=[('qkT', j, t)])
            fk = [('dbgqk', j, t) for j in range(5) for t in range(NT_OWN if j < 4 else NT_ALL)] if self.debug else []
            S.emit(final_wait_keys=fk)

    def B1b(self, qT, kT, v_aug):
        nc = self.nc
        with contextlib.ExitStack() as st:
            sb = lambda n, s, d=F32: st.enter_context(nc.sbuf_tensor(n, s, d))
            S = self.S
            anw = sb("anw_rep", [P, 1024])
            S.dma('sp', anw[:], self.anw.partition_broadcast(P), writes=['anw'])
            zt = sb("zt", [P, 1024])
            S.op('dve', lambda e: e.memset(zt[:], 0.0), writes=['zt'])
            dk = []
            for t in range(NT_OWN if 'moe' not in self.stages else 0):
                S.dma('sp', self.y[t * P:(t + 1) * P, 1024:2048], zt[:], reads=['zt'], writes=[('yz', t)])
                dk.append(('yz', t))
            E = [sb(f"E{i}", [P, NT_ALL, 512], BF16) for i in range(2)]
            o = [sb(f"o{i}", [P, P]) for i in range(2)]
            on = [sb(f"on{i}", [P, P]) for i in range(2)]
            junk = [sb(f"junk{i}", [P, P]) for i in range(2)]
            sc = [sb(f"sc{i}", [P, 4]) for i in range(2)]
            pS = [st.enter_context(nc.psum_tensor(f"pS{i}", [P, 512], F32)) for i in range(3)]
            pO = [st.enter_context(nc.psum_tensor(f"pO{i}", [P, 132], F32)) for i in range(2)]
            pX = [st.enter_context(nc.psum_tensor(f"pX{i}", [P, P], F32)) for i in range(2)]
            ns = 0
            no = 0
            for h in range(8):
                kvh = h // 4
                for qc in range(2):
                    es = (h * 2 + qc) % 2
                    for j in range(NT_ALL):
                        s = ns % 3
                        ns += 1
                        S.op('pe', lambda e, s=s, j=j, h=h, qc=qc, kvh=kvh: e.matmul(
                            pS[s][:], kT[:, kvh, j * P:(j + 1) * P], qT[:, h, qc * 512:(qc + 1) * 512], start=True, stop=True),
                            reads=[], writes=[('pS', s)])
                        S.op('act', lambda e, s=s, j=j, es=es: e.activation(E[es][:, j, :], pS[s][:], AF.Exp, scale=float(1.0 / np.sqrt(128.0))),
                             reads=[('pS', s)], writes=[('E', es, j)])
                    for qt in range(4):
                        s = no % 2
                        no += 1
                        tq = qc * 4 + qt
                        S.op('pe', lambda e, s=s, es=es, qt=qt, kvh=kvh: self.mm_group(
                            e, pO[s][:, 0:129], [(E[es][:, j, qt * P:(qt + 1) * P], v_aug[:, j, kvh, 0:129]) for j in range(NT_ALL)]),
                            reads=[('E', es, j) for j in range(NT_ALL)], writes=[('pO', s)])
                        S.op('dve', lambda e, s=s: e.reciprocal(sc[s][:, 0:1], pO[s][:, 128:129]), reads=[('pO', s)], writes=[('rden', s)])
                        S.op('dve', lambda e, s=s: e.tensor_scalar(o[s][:], pO[s][:, 0:128], sc[s][:, 0:1], None, ALU.mult),
                             reads=[('pO', s), ('rden', s)], writes=[('o', s)])
                        S.op('act', lambda e, s=s: e.activation(junk[s][:], o[s][:], AF.Square, accum_out=sc[s][:, 1:2]),
                             reads=[('o', s)], writes=[('oss', s), ('junk', s)])
                        S.op('act', lambda e, s=s: e.activation(sc[s][:, 2:3], sc[s][:, 1:2], AF.Sqrt, bias=self.eps_t[:, 0:1], scale=1.0 / 128),
                             reads=[('oss', s)], writes=[('osd', s)])
                        S.op('dve', lambda e, s=s: e.reciprocal(sc[s][:, 2:3], sc[s][:, 2:3]), reads=[('osd', s)], writes=[('ors', s)])
                        S.op('dve', lambda e, s=s, h=h: e.scalar_tensor_tensor(on[s][:], o[s][:], sc[s][:, 2:3], anw[:, h * P:(h + 1) * P], ALU.mult, ALU.mult),
                             reads=[('o', s), ('ors', s), 'anw'], writes=[('on', s)])
                        if 'moe' not in self.stages:
                            S.dma('sp', self.y[tq * P:(tq + 1) * P, h * P:(h + 1) * P], on[s][:], reads=[('on', s)], writes=[('yo', h, tq)])
                            dk.append(('yo', h, tq))
                        if self.debug:
                            S.dma('sp', self.dbg_oa[tq * P:(tq + 1) * P, h * P:(h + 1) * P], on[s][:], reads=[('on', s)], writes=[('dbgoa', h, tq)])
                            dk.append(('dbgoa', h, tq))
                        S.op('pe', lambda e, s=s: e.transpose(pX[s][:], on[s][:], self.ident[:]), reads=[('on', s)], writes=[('pX', s)])
                        S.op('act', lambda e, s=s, h=h, tq=tq: e.activation(self.mixedT[:, h, tq * P:(tq + 1) * P], pX[s][:], AF.Copy),
                             reads=[('pX', s)], writes=[('mixedT', h, tq)])
            S.emit(final_wait_keys=dk)

    def phase_B2(self):
        nc = self.nc
        hT = self.hT
        with contextlib.ExitStack() as st:
            sb = lambda n, s, d=F32: st.enter_context(nc.sbuf_tensor(n, s, d))
            ps = lambda n, s: st.enter_context(nc.psum_tensor(n, s, F32))
            S = self.S
            N = S_OWN
            NCH = 16
            lba = sb("lba", [P, 2, 16]); lb = sb("lb", [P, 16]); oml = sb("oml", [P, 16])
            S.dma('sp', lba[:], self.lb_a.rearrange("s p c -> p s c"), writes=['lba'])
            S.op('dve', lambda e: e.tensor_tensor(oml[:], lba[:, 0, :], lba[:, 1, :], ALU.subtract), reads=['lba'], writes=['oml'])
            S.op('act', lambda e: e.activation(lb[:], oml[:], AF.Sigmoid), reads=['oml'], writes=['lb'])
            S.op('dve', lambda e: e.tensor_scalar(oml[:], lb[:], -1.0, 1.0, ALU.mult, ALU.add), reads=['lb'], writes=['oml'])
            msk = sb("hmask_s", [P, 2, P])
            S.dma('sp', msk[:], self.hmask.rearrange("m j i -> j m i"), writes=['msk'])
            rmask = sb("rmask_s", [P, N])
            S.dma('sp', rmask[:], self.rmask_d.partition_broadcast(P), writes=['rmask'])
            hnw = [sb(f"hnw{i}", [P, P]) for i in range(2)]
            wb = [sb(f"whg{i}", [P, KT, CW], BF16) for i in range(2)]
            qr = sb("qr", [P, N]); sgA = sb("sgA", [P, N]); sgB = sb("sgB", [P, N])
            v_tm = sb("v_tm", [P, NT_ALL, P], BF16); sg_tm = sb("sg_tm", [64, NCH, P], BF16)
            X2 = sb("X2", [P, N]); X3 = sb("X3", [P, N]); X4 = sb("X4", [P, N]); X5 = sb("X5", [P, N])
            kdec = [sb(f"kdec{i}", [P, N], BF16) for i in range(2)]
            qdec = [sb(f"qdec{i}", [P, N], BF16) for i in range(2)]
            kend_tm = sb("kend_tm", [P, 2, NT_OWN, P], BF16)
            halfm = sb("halfm_s", [P, 2])
            S.dma('sp', halfm[:], self.halfm_d, writes=['halfm'])
            Sbf = [sb(f"Sbf{i}", [P, NCH, P], BF16) for i in range(2)]
            S32 = sb("S32", [P, 2, P]); etot = sb("etot", [P, NCH]); scur = [0]
            scm_ = [[sb(f"scm{j}_{i}", [P, P], BF16) for i in range(2)] for j in range(2)]
            oh_ = [sb(f"oh{j}", [64, 2, P]) for j in range(2)]; oh2_ = [sb(f"oh2{j}", [64, 2, P]) for j in range(2)]
            junk_ = [sb(f"hjunk{j}", [64, P]) for j in range(2)]; sc_ = [sb(f"hsc{j}", [64, 8]) for j in range(2)]
            pP = [ps(f"pP{i}", [P, 512]) for i in range(2)]
            pV = ps("pV", [P, 4, P]); pTk = ps("pTk", [P, 4, P]); pU = ps("pU", [P, 4, P])
            pSc = ps("pSc", [P, 2, P]); pOh = ps("pOh", [64, 2, P]); pX = ps("pXh", [P, 2, 64])
            npp = [0]
            nwb = [0]
            dk = []

            def load_w(hh, c):
                s = nwb[0] % 2
                nwb[0] += 1
                S.dma('pool', wb[s][:], self.w_hg[hh, c], writes=[('whg', s)], max_dma_last_dim=4096)
                return s

            def proj_fm(ws, col0, tok0, ntok, func, dest, key):
                for c in range(ntok // 512):
                    s = npp[0] % 2
                    npp[0] += 1
                    S.op('pe', lambda e, s=s, c=c: self.mm_group(e, pP[s][:], [
                        (wb[ws][:, kt, col0:col0 + P], hT[:, kt, tok0 + c * 512:tok0 + (c + 1) * 512]) for kt in range(KT)]),
                        reads=[('whg', ws)], writes=[('pP', s)])
                    S.op('act', lambda e, s=s, c=c: e.activation(dest[:, c * 512:(c + 1) * 512], pP[s][:], func),
                         reads=[('pP', s)], writes=[(key, c)])

            def proj_tm(ws, col0, ntiles, func, dest, key):
                for g in range(ntiles // 4):
                    def f(e, g=g):
                        last = None
                        for q in range(4):
                            t = g * 4 + q
                            last = self.mm_group(e, pV[:, q, :], [(hT[:, kt, t * P:(t + 1) * P], wb[ws][:, kt, col0:col0 + P]) for kt in range(KT)])
                        return last
                    S.op('pe', f, reads=[('whg', ws)], writes=['pV'])
                    S.op('act', lambda e, g=g: e.activation(dest[:, g * 4:(g + 1) * 4, :], pV[:], func), reads=['pV'], writes=[(key, g)])

            import os
            cut = int(os.environ.get("HG_CUT", "9"))

            def gate_pass(hh, di, sg, sgkeys, own, first_state):
                if cut < 2:
                    return
                col = di * 8 + hh
                fg, kk, lf, b, bb = sg, X2, X3, X4, X5
                S.op('dve', lambda e: e.tensor_scalar(fg[:], sg[:], oml[:, col:col + 1], lb[:, col:col + 1], ALU.mult, ALU.add),
                     reads=sgkeys + ['oml', 'lb'], writes=['fg'])
                S.op('dve', lambda e: e.tensor_scalar(kk[:], fg[:], -1.0, 1.0, ALU.mult, ALU.add), reads=['fg'], writes=['X2'])
                S.op('act', lambda e: e.activation(lf[:], fg[:], AF.Ln), reads=['fg'], writes=['X3'])
                S.op('dve', lambda e: e.tensor_tensor_scan(b[:], rmask[:], lf[:], 0.0, ALU.mult, ALU.add), reads=['X3', 'rmask'], writes=['X4'])
                b3 = b[:].rearrange("p (c i) -> p c i", i=64)
                S.op('act', lambda e: e.activation(etot[:].unsqueeze(2), b3[:, :, 63:64], AF.Exp), reads=['X4'], writes=['etot'])
                if di == 0:
                    bbt, bbk = b, 'X4'
                else:
                    S.op('dve', lambda e: e.tensor_tensor(bb[:], lf[:], b[:], ALU.subtract), reads=['X3', 'X4'], writes=['X5'])
                    bb3 = bb[:].rearrange("p (c i) -> p c i", i=64)
                    S.op('dve', lambda e: e.tensor_tensor(bb3, bb3, b3[:, :, 63:64].to_broadcast([P, NCH, 64]), ALU.add), reads=['X5', 'X4'], writes=['X5'])
                    bbt, bbk = bb, 'X5'
                en = lf
                S.op('act', lambda e: e.activation(en[:], bbt[:], AF.Exp, scale=-1.0), reads=[bbk, 'X3'], writes=['X3'])
                S.op('dve', lambda e: e.tensor_tensor(kk[:], kk[:], en[:], ALU.mult), reads=['X2', 'X3'], writes=['X2'])
                if own:
                    S.op('act', lambda e: e.activation(kdec[di][:], kk[:], AF.Copy), reads=['X2'], writes=[('kdec', di)])
                    S.op('act', lambda e: e.activation(en[:], bbt[:], AF.Exp), reads=[bbk, 'X2'], writes=['X3'])
                    S.op('dve', lambda e: e.tensor_tensor(qdec[di][:], qr[:], en[:], ALU.mult), reads=['X3', ('qr', 0), ('qr', 1)], writes=[('qdec', di)])
                kend32 = b if di == 1 else bb
                kkey = 'X4' if di == 1 else 'X5'
                S.op('dve', lambda e: e.tensor_tensor(kend32[:].rearrange("p (c i) -> p c i", i=64), kk[:].rearrange("p (c i) -> p c i", i=64),
                                                      etot[:].unsqueeze(2).to_broadcast([P, NCH, 64]), ALU.mult),
                     reads=['X2', 'etot', 'X4', 'X5'], writes=[kkey])
                if cut < 3:
                    return
                for g in range(2):
                    def ftr(e, g=g):
                        last = None
                        for q in range(4):
                            t = g * 4 + q
                            last = e.transpose(pTk[:, q, :], kend32[:, t * P:(t + 1) * P], self.ident[:])
                        return last
                    S.op('pe', ftr, reads=[kkey], writes=['pTk'])
                    for hf in range(2):
                        S.op('act', lambda e, g=g, hf=hf: e.activation(kend_tm[:, hf, g * 4:(g + 1) * 4, :], pTk[:], AF.Copy, scale=halfm[:, hf:hf + 1]),
                             reads=['pTk', 'halfm'], writes=[('kend_tm', g, hf)])
                if cut < 4:
                    return
                tile0 = 0 if own else NT_OWN
                order = list(range(NCH)) if di == 0 else list(range(NCH - 1, -1, -1))
                started = not first_state
                for gi in range(4):
                    cs = order[gi * 4:(gi + 1) * 4]

                    def fu(e, cs=cs):
                        last = None
                        for q, c in enumerate(cs):
                            t, hf = c // 2, c % 2
                            last = e.matmul(pU[:, q, :], kend_tm[:, hf, t, :], v_tm[:, tile0 + t, :], start=True, stop=True)
                        return last
                    S.op('pe', fu, reads=[('kend_tm', g, hf) for g in range(2) for hf in range(2)] + [('v_tm', g) for g in range(4)], writes=['pU'])
                    for q, c in enumerate(cs):
                        if own:
                            if started:
                                S.op('act', lambda e, c=c, cu=scur[0]: e.activation(Sbf[di][:, c, :], S32[:, cu, :], AF.Copy), reads=[('S32', scur[0])], writes=[('Sbf', di, c)])
                            else:
                                S.op('dve', lambda e, c=c: e.memset(Sbf[di][:, c, :], 0.0), writes=[('Sbf', di, c)])
                        if started:
                            cu = scur[0]
                            S.op('dve', lambda e, q=q, c=c, cu=cu: e.scalar_tensor_tensor(S32[:, 1 - cu, :], S32[:, cu, :], etot[:, c:c + 1], pU[:, q, :], ALU.mult, ALU.add),
                                 reads=[('S32', cu), 'pU', 'etot'], writes=[('S32', 1 - cu)])
                            scur[0] = 1 - cu
                        else:
                            S.op('dve', lambda e, q=q, cu=scur[0]: e.tensor_copy(S32[:, cu, :], pU[:, q, :]), reads=['pU'], writes=[('S32', scur[0])])
                            started = True

            for hh in range(8):
                hs = hh % 2
                S.dma('sp', hnw[hs][:], self.hnw[:, hh * P:(hh + 1) * P].partition_broadcast(P), writes=[('hnw', hs)])
                wsA = load_w(hh, 0)
                proj_fm(wsA, 0, 0, N, AF.Silu, qr, 'qr')
                proj_fm(wsA, P, 0, N, AF.Sigmoid, sgA, 'sgA')
                wsB = load_w(hh, 1)
                proj_tm(wsB, P, NT_ALL, AF.Copy, v_tm, 'v_tm')
                proj_fm(wsB, 0, N, N, AF.Sigmoid, sgB, 'sgB')
                gate_pass(hh, 0, sgA, [('sgA', 0), ('sgA', 1)], True, True)
                gate_pass(hh, 1, sgB, [('sgB', 0), ('sgB', 1)], False, True)
                proj_fm(wsB, 0, 0, N, AF.Sigmoid, sgB, 'sgB')
                wsG = load_w(hh, 2)
                for g in range(4):
                    def fg_(e, g=g, wsG=wsG):
                        last = None
                        for q in range(4):
                            c = g * 4 + q
                            last = self.mm_group(e, pV[0:64, q, :], [(hT[:, kt, c * 64:(c + 1) * 64], wb[wsG][:, kt, 0:P]) for kt in range(KT)])
                        return last
                    S.op('pe', fg_, reads=[('whg', wsG)], writes=['pV'])
                    S.op('act', lambda e, g=g: e.activation(sg_tm[:, g * 4:(g + 1) * 4, :], pV[0:64, :, :], AF.Silu), reads=['pV'], writes=[('sg_tm', g)])
                gate_pass(hh, 1, sgB, [('sgB', 0), ('sgB', 1)], True, False)
                def out_tile(t, pb, hh=hh, hs=hs):
                    tsl = slice(t * P, (t + 1) * P)
                    scm, oh, oh2, junk, sc = scm_[pb], oh_[pb], oh2_[pb], junk_[pb], sc_[pb]

                    def fsc(e, tsl=tsl):
                        e.matmul(pSc[:, 0, :], kdec[0][:, tsl], qdec[0][:, tsl], start=True, stop=True)
                        return e.matmul(pSc[:, 1, :], kdec[1][:, tsl], qdec[1][:, tsl], start=True, stop=True)
                    S.op('pe', fsc, reads=[('kdec', 0), ('kdec', 1), ('qdec', 0), ('qdec', 1)], writes=['pSc'])
                    for di in range(2):
                        S.op('dve', lambda e, di=di: e.tensor_tensor(scm[di][:], pSc[:, di, :], msk[:, di, :], ALU.mult),
                             reads=['pSc', 'msk'], writes=[('scm', pb, di)])

                    def fo(e, t=t, tsl=tsl):
                        last = None
                        for hf in range(2):
                            c = 2 * t + hf
                            csl = slice(hf * 64, (hf + 1) * 64)
                            tk = slice(t * P + hf * 64, t * P + (hf + 1) * 64)
                            e.matmul(pOh[:, hf, :], scm[0][:, csl], v_tm[:, t, :], start=True, stop=False)
                            e.matmul(pOh[:, hf, :], scm[1][:, csl], v_tm[:, t, :], start=False, stop=False)
                            e.matmul(pOh[:, hf, :], qdec[0][:, tk], Sbf[0][:, c, :], start=False, stop=False)
                            last = e.matmul(pOh[:, hf, :], qdec[1][:, tk], Sbf[1][:, c, :], start=False, stop=True)
                        return last
                    S.op('pe', fo, reads=[('scm', pb, 0), ('scm', pb, 1), ('qdec', 0), ('qdec', 1)] + [('Sbf', di, c) for di in range(2) for c in (2 * t, 2 * t + 1)]
                         + [('v_tm', g) for g in range(4)], writes=['pOh'])
                    S.op('act', lambda e: e.activation(oh[:], pOh[:], AF.Copy), reads=['pOh'], writes=[('oh', pb)])
                    for hf in range(2):
                        S.op('act', lambda e, hf=hf: e.activation(junk[:], oh[:, hf, :], AF.Square, accum_out=sc[:, hf:hf + 1]), reads=[('oh', pb)], writes=[('hss', pb, hf), ('hjunk', pb)])
                    S.op('act', lambda e: e.activation(sc[:, 2:4], sc[:, 0:2], AF.Sqrt, bias=self.eps_t[0:64, 0:1], scale=1.0 / 128), reads=[('hss', pb, 0), ('hss', pb, 1)], writes=[('hsd', pb)])
                    S.op('dve', lambda e: e.reciprocal(sc[:, 2:4], sc[:, 2:4]), reads=[('hsd', pb)], writes=[('hrs', pb)])
                    for hf in range(2):
                        S.op('dve', lambda e, hs=hs, hf=hf: e.scalar_tensor_tensor(oh2[:, hf, :], oh[:, hf, :], sc[:, 2 + hf:3 + hf], hnw[hs][0:64, :], ALU.mult, ALU.mult),
                             reads=[('oh', pb), ('hrs', pb), ('hnw', hs)], writes=[('oh2', pb, hf)])
                    S.op('dve', lambda e, t=t: e.tensor_tensor(oh2[:], oh2[:], sg_tm[:, 2 * t:2 * t + 2, :], ALU.mult),
                         reads=[('oh2', pb, 0), ('oh2', pb, 1)] + [('sg_tm', g) for g in range(4)], writes=[('oh2', pb, 0), ('oh2', pb, 1)])
                    if self.debug:
                        for hf in range(2):
                            rs_ = slice(t * P + hf * 64, t * P + (hf + 1) * 64)
                            S.dma('sp', self.dbg_os[rs_, hh * P:(hh + 1) * P], oh[:, hf, :], reads=[('oh', pb)], writes=[('dbgos', hh, t, hf)])
                            S.dma('sp', self.dbg_or[rs_, hh * P:(hh + 1) * P], oh2[:, hf, :], reads=[('oh2', pb, hf)], writes=[('dbgor', hh, t, hf)])
                            dk.extend([('dbgos', hh, t, hf), ('dbgor', hh, t, hf)])

                    def ftx(e):
                        e.transpose(pX[:, 0, :], oh2[:, 0, :], self.ident[0:64, 0:64])
                        return e.transpose(pX[:, 1, :], oh2[:, 1, :], self.ident[0:64, 0:64])
                    S.op('pe', ftx, reads=[('oh2', pb, 0), ('oh2', pb, 1)], writes=['pXh'])
                    S.op('act', lambda e, hh=hh, tsl=tsl: e.activation(self.mixedT[:, 8 + hh, tsl], pX[:].rearrange("p a b -> p (a b)"), AF.Copy),
                         reads=['pXh'], writes=[('mixedT', 8 + hh, tsl.start)])
                for t in range(NT_OWN if cut >= 5 else 0):
                    out_tile(t, t % 2)
            S.emit(final_wait_keys=dk)

    def make_carep(self, S, st):
        nc = self.nc
        u = Sched._uid
        ccol = st.enter_context(nc.sbuf_tensor(f"ccol{u}", [P, KT], F32))
        cact = st.enter_context(nc.sbuf_tensor(f"cact{u}", [P, KT], F32))
        cabf = st.enter_context(nc.sbuf_tensor(f"cabf{u}", [P, KT], BF16))
        carep = st.enter_context(nc.sbuf_tensor(f"carep{u}", [P, KT, P], BF16))
        S.dma('sp', ccol[:], self.c_col, writes=['ccol'])
        S.op('act', lambda e: e.activation(cact[:], ccol[:], AF.Silu), reads=['ccol'], writes=['cact'])
        S.op('dve', lambda e: e.tensor_copy(carep[:], cact[:].unsqueeze(2).to_broadcast([P, KT, P])), reads=['cact'], writes=['carep'])
        S.op('dve', lambda e: e.tensor_copy(cabf[:], cact[:]), reads=['cact'], writes=['cabf'])
        return carep, cabf

    def phase_C0(self):
        nc = self.nc
        with contextlib.ExitStack() as st:
            S = self.S
            carep, cabf = self.make_carep(S, st)
            self.ada_part(S, st, self.w_ada2, self.b_ada2, 3 * D, self.mod2, carep, 2 * D)
            wg = [st.enter_context(nc.sbuf_tensor(f"wg2_{i}", [P, KT, 512], BF16)) for i in range(2)]
            bg = st.enter_context(nc.sbuf_tensor("bg2", [P, KT], F32))
            pg = st.enter_context(nc.psum_tensor("pg2", [P, KT], F32))
            S.dma('sp', bg[:], self.b_g2col, writes=['bg2'])
            for c in range(4):
                s = c % 2
                S.dma('pool', wg[s][:], self.w_ada3[:, c * 512:(c + 1) * 512].rearrange("(kt p) n -> p kt n", p=P), writes=[('wg2', s)])

                def f(e, s=s, c=c):
                    last = None
                    for q in range(4):
                        dt = c * 4 + q
                        last = self.mm_group(e, pg[:, dt:dt + 1], [(wg[s][:, kt, q * P:(q + 1) * P], cabf[:, kt:kt + 1]) for kt in range(KT)])
                    return last
                S.op('pe', f, reads=[('wg2', s), 'cabf'], writes=['pg2'])
            S.op('dve', lambda e: e.tensor_tensor(self.g2col[:], pg[:], bg[:], ALU.add), reads=['pg2', 'bg2'], writes=['g2col'])
            S.emit()

    def phase_C1(self):
        nc = self.nc
        with contextlib.ExitStack() as st:
            S = self.S
            wb = [st.enter_context(nc.sbuf_tensor(f"wo{i}", [P, KT, CW], BF16)) for i in range(3)]
            py = [st.enter_context(nc.psum_tensor(f"py{i}", [P, CW], F32)) for i in range(2)]
            it = 0
            for dc in range(8):
                ws = dc % 3
                S.dma('pool', wb[ws][:], self.w_out_t[dc], writes=[('wo', ws)], max_dma_last_dim=4096)
                for t in range(NT_OWN):
                    s = it % 2
                    it += 1
                    S.op('pe', lambda e, s=s, t=t, ws=ws: self.mm_group(
                        e, py[s][:], [(self.mixedT[:, mt, t * P:(t + 1) * P], wb[ws][:, mt, :]) for mt in range(16)]),
                        reads=[('wo', ws)], writes=[('py', s)])
                    S.op('act', lambda e, s=s, t=t, dc=dc: e.activation(self.acc[:, t, dc * CW:(dc + 1) * CW], py[s][:], AF.Copy),
                         reads=[('py', s)], writes=[('acc', t, dc)])
            S.emit()

    def phase_C2(self):
        nc = self.nc
        ALPHA = float(2.0 ** 0.25)
        with contextlib.ExitStack() as st:
            sb = lambda n, s, d=F32: st.enter_context(nc.sbuf_tensor(n, s, d))
            S = self.S
            mod2 = self.mod2
            g1r, sh2r, sc2r = mod2[:, 0:D], mod2[:, D:2 * D], mod2[:, 2 * D:3 * D]
            ln1 = sb("ln1r", [P, 2, D])
            S.dma('sp', ln1[:, 0, :], self.ln1[0:1, :].partition_broadcast(P), writes=['ln1g'])
            S.dma('sp', ln1[:, 1, :], self.ln1[1:2, :].partition_broadcast(P), writes=['ln1b'])
            wr = sb("wr", [P, KT, 32]); br = sb("br", [P, 32])
            S.dma('sp', wr[:], self.w_r, writes=['wr'])
            S.dma('sp', br[:], self.b_r.partition_broadcast(P), writes=['br'])
            xin_ = [sb(f"xin_c{i}", [P, D]) for i in range(2)]; tt_ = [sb(f"tt_c{i}", [P, D]) for i in range(2)]
            h2_ = [sb(f"h2_c{i}", [P, D]) for i in range(2)]
            stt_ = [sb(f"stt_c{i}", [P, 4, 6]) for i in range(2)]; mv_ = [sb(f"mv_c{i}", [P, 6]) for i in range(2)]
            mv2_ = [sb(f"mv2_c{i}", [P, 6]) for i in range(2)]
            lg_ = [sb(f"lg{i}", [P, 32]) for i in range(2)]; top8_ = [sb(f"top8{i}", [P, 8]) for i in range(2)]
            mask_ = [sb(f"mask{i}", [P, 32]) for i in range(2)]; ex_ = [sb(f"ex{i}", [P, 32]) for i in range(2)]
            gt_ = [sb(f"gt{i}", [P, 32]) for i in range(2)]; sm_ = [sb(f"sm{i}", [P, 4]) for i in range(2)]
            pT = [st.enter_context(nc.psum_tensor(f"pTc{i}", [P, 8, P], F32)) for i in range(3)]
            pL = st.enter_context(nc.psum_tensor("pL", [P, 32], F32))
            pG = st.enter_context(nc.psum_tensor("pG", [32, P], F32))
            npt = [0]
            dk = []

            def transposes(src, srckey, evac):
                for hf in range(2):
                    pi = npt[0] % 3
                    npt[0] += 1

                    def tr(e, hf=hf, pi=pi):
                        last = None
                        for k8 in range(8):
                            kt = hf * 8 + k8
                            last = e.transpose(pT[pi][:, k8, :], src[:, kt * P:(kt + 1) * P], self.ident[:])
                        return last
                    S.op('pe', tr, reads=[srckey], writes=[('pTc', pi)])
                    evac(hf, pi)

            import os
            ccut = int(os.environ.get("C_CUT", "9"))
            def tile_body(t, pb):
                xin, tt, h2, stt, mv, mv2 = xin_[pb], tt_[pb], h2_[pb], stt_[pb], mv_[pb], mv2_[pb]
                lg, top8, mask, ex, gt, sm = lg_[pb], top8_[pb], mask_[pb], ex_[pb], gt_[pb], sm_[pb]
                h2T32 = xin[:].rearrange("p (k j) -> p k j", k=KT)
                tsl = slice(t * P, (t + 1) * P)
                S.dma('sp', xin[:], self.x[tsl, :], writes=[('xin', pb)])
                S.op('dve', lambda e, t=t: e.tensor_tensor(tt[:], self.acc[:, t, :], g1r, ALU.mult), reads=[('acc', t)], writes=[('tt', pb)])
                S.op('dve', lambda e: e.scalar_tensor_tensor(tt[:], xin[:], ALPHA, tt[:], ALU.mult, ALU.add), reads=[('tt', pb), ('xin', pb)], writes=[('tt', pb)])
                self.ln_stats(S, stt, mv, tt, ('tt', pb), ('ln1', pb))
                S.op('act', lambda e: e.activation(tt[:], tt[:], AF.Identity, bias=mv[:, 3:4], scale=mv[:, 2:3]),
                     reads=[('tt', pb), (('ln1', pb), 'nb'), (('ln1', pb), 'rs')], writes=[('tt', pb)])
                S.op('dve', lambda e: e.tensor_tensor(tt[:], tt[:], ln1[:, 0, :], ALU.mult), reads=[('tt', pb), 'ln1g'], writes=[('tt', pb)])
                S.op('dve', lambda e: e.tensor_tensor(tt[:], tt[:], ln1[:, 1, :], ALU.add), reads=[('tt', pb), 'ln1b'], writes=[('tt', pb)])
                if self.debug:
                    S.dma('sp', self.dbg_x1[tsl, :], tt[:], reads=[('tt', pb)], writes=[('dbgx1', t)]); dk.append(('dbgx1', t))
                if ccut < 5:
                    return
                self.ln_stats(S, stt, mv2, tt, ('tt', pb), ('ln2h', pb))
                S.op('act', lambda e: e.activation(h2[:], tt[:], AF.Identity, bias=mv2[:, 3:4], scale=mv2[:, 2:3]),
                     reads=[('tt', pb), (('ln2h', pb), 'nb'), (('ln2h', pb), 'rs')], writes=[('h2', pb)])
                S.op('dve', lambda e: e.tensor_tensor(h2[:], h2[:], sc2r, ALU.mult), reads=[('h2', pb)], writes=[('h2', pb)])
                S.op('dve', lambda e: e.tensor_tensor(h2[:], h2[:], sh2r, ALU.add), reads=[('h2', pb)], writes=[('h2', pb)])
                if self.debug:
                    S.dma('sp', self.dbg_h2[tsl, :], h2[:], reads=[('h2', pb)], writes=[('dbgh2', t)]); dk.append(('dbgh2', t))

                def evac_h2(hf, pi, t=t, tsl=tsl):
                    S.op('act', lambda e: e.activation(self.h2T[:, hf * 8:(hf + 1) * 8, tsl], pT[pi][:], AF.Copy),
                         reads=[('pTc', pi)], writes=[('h2T', t, hf)])
                    S.op('act', lambda e: e.activation(h2T32[:, hf * 8:(hf + 1) * 8, :], pT[pi][:], AF.Copy),
                         reads=[('pTc', pi), ('xin', pb)], writes=[('h2T32', pb, hf), ('xin', pb)])
                if ccut < 6:
                    return
                transposes(h2, ('h2', pb), evac_h2)
                if ccut < 7:
                    return
                S.op('pe', lambda e: self.mm_group(e, pL[:], [(h2T32[:, kt, :], wr[:, kt, :]) for kt in range(KT)]),
                     reads=[('h2T32', pb, 0), ('h2T32', pb, 1), 'wr', ('xin', pb)], writes=['pL'])
                S.op('dve', lambda e: e.tensor_tensor(lg[:], pL[:], br[:], ALU.add), reads=['pL', 'br'], writes=[('lg', pb)])
                if self.debug:
                    S.dma('sp', self.dbg_lg[tsl, :], lg[:], reads=[('lg', pb)], writes=[('dbglg', t)]); dk.append(('dbglg', t))
                if ccut < 8:
                    return
                S.op('dve', lambda e: e.max(top8[:], lg[:]), reads=[('lg', pb)], writes=[('top8', pb)])
                S.op('dve', lambda e: e.tensor_scalar(mask[:], lg[:], top8[:, 3:4], None, ALU.is_ge), reads=[('lg', pb), ('top8', pb)], writes=[('mask', pb)])
                S.op('dve', lambda e: e.tensor_scalar(sm[:, 0:1], top8[:, 0:1], -1.0, None, ALU.mult), reads=[('top8', pb)], writes=[('negm', pb)])
                S.op('act', lambda e: e.activation(ex[:], lg[:], AF.Exp, bias=sm[:, 0:1], scale=1.0), reads=[('lg', pb), ('negm', pb)], writes=[('ex', pb)])
                S.op('dve', lambda e: e.tensor_tensor(ex[:], ex[:], mask[:], ALU.mult), reads=[('ex', pb), ('mask', pb)], writes=[('ex', pb)])
                S.op('dve', lambda e: e.tensor_reduce(sm[:, 1:2], ex[:], AX.X, ALU.add), reads=[('ex', pb)], writes=[('den', pb)])
                S.op('dve', lambda e: e.reciprocal(sm[:, 2:3], sm[:, 1:2]), reads=[('den', pb)], writes=[('rden', pb)])
                S.op('dve', lambda e: e.tensor_scalar(gt[:], ex[:], sm[:, 2:3], None, ALU.mult), reads=[('ex', pb), ('rden', pb)], writes=[('gt', pb)])
                S.op('pe', lambda e: e.transpose(pG[:], gt[:], self.ident[:]), reads=[('gt', pb)], writes=['pG'])
                S.op('act', lambda e, tsl=tsl: e.activation(self.gatesT[:, tsl], pG[:], AF.Copy), reads=['pG'], writes=[('gatesT', t)])
                S.op('dve', lambda e: e.tensor_scalar(tt[:], tt[:], ALPHA, None, ALU.mult), reads=[('tt', pb)], writes=[('tt', pb)])

                def evac_acc(hf, pi, t=t):
                    S.op('act', lambda e: e.activation(self.acc[:, t, hf * 1024:(hf + 1) * 1024].rearrange("p (k j) -> p k j", k=8), pT[pi][:], AF.Copy),
                         reads=[('pTc', pi)], writes=[('acc', t)])
                transposes(tt, ('tt', pb), evac_acc)
            for t in range(NT_OWN):
                tile_body(t, t % 2)
            if self.debug:
                S.dma('sp', self.dbg_gt, self.gatesT[:], reads=[('gatesT', t) for t in range(NT_OWN)], writes=['dbggt']); dk.append('dbggt')
                for t in range(NT_OWN):
                    S.dma('sp', self.dbg_acc[:, t * D:(t + 1) * D], self.acc[:, t, :], reads=[('acc', t)], writes=[('dbgacc', t)]); dk.append(('dbgacc', t))
            S.emit(final_wait_keys=dk)

    def phase_MoE(self):
        nc = self.nc
        with contextlib.ExitStack() as st:
            sb = lambda n, s, d=F32: st.enter_context(nc.sbuf_tensor(n, s, d))
            ps = lambda n, s: st.enter_context(nc.psum_tensor(n, s, F32))
            S = self.S
            h2T, acc, gatesT, g2col = self.h2T, self.acc, self.gatesT, self.g2col
            wb = [sb(f"wmoe{i}", [P, KT, CW], BF16) for i in range(3)]
            actT = sb("actT", [P, 8, S_OWN], BF16)
            glu = [sb(f"glu{i}", [P, 512]) for i in range(2)]
            sig = [sb(f"sig{i}", [P, 512]) for i in range(2)]
            lin1 = [sb(f"lin1{i}", [P, 512]) for i in range(2)]
            tg = [sb(f"tg{i}", [P, 512]) for i in range(2)]
            grep = sb("grep", [P, S_OWN]); gsel = sb("gsel", [32, S_OWN])
            b1 = sb("b1s", [P, 32, 32]); b2s = sb("b2s", [32, D]); ones32 = sb("ones32", [32, P])
            pGL = [ps(f"pGL{i}", [P, 2, 512]) for i in range(2)]
            pY = [ps(f"pY{i}", [P, 512]) for i in range(2)]
            pGR = ps("pGR", [P, 512])
            S.dma('sp', b1[:], self.b1_r, writes=['b1'])
            S.dma('sp', b2s[:], self.b2, writes=['b2s'])
            S.op('dve', lambda e: e.memset(ones32[:], 1.0), writes=['ones32'])
            S.op('dve', lambda e: e.tensor_scalar(b1[:, :, 16:32], b1[:, :, 16:32], 1.0, None, ALU.add), reads=['b1'], writes=['b1'])
            nwb = [0]; ngl = [0]; ny = [0]

            def acc_add(s, dt, th):
                accv = acc[:, 4 * th:4 * th + 4, dt * P:(dt + 1) * P]
                S.op('dve', lambda e: e.scalar_tensor_tensor(accv, pY[s][:].rearrange("p (a b) -> p a b", a=4), g2col[:, dt:dt + 1], accv, ALU.mult, ALU.add),
                     reads=[('pY', s), 'g2col'], writes=[('accT', dt, th)])

            for dt in range(KT):
                for th in range(2):
                    s = ny[0] % 2
                    ny[0] += 1
                    S.op('pe', lambda e, s=s, dt=dt, th=th: e.matmul(pY[s][:], b2s[:, dt * P:(dt + 1) * P], gatesT[:, th * 512:(th + 1) * 512], start=True, stop=True),
                         reads=['b2s'], writes=[('pY', s)])
                    acc_add(s, dt, th)

            for ex in range(self.NE):
                S.op('dve', lambda e, ex=ex: e.tensor_scalar(gsel[:], gatesT[:], self.ident[0:32, ex:ex + 1], None, ALU.mult), reads=[], writes=['gsel'])
                for th in range(2):
                    S.op('pe', lambda e, th=th: e.matmul(pGR[:], ones32[:], gsel[:, th * 512:(th + 1) * 512], start=True, stop=True),
                         reads=['gsel', 'ones32'], writes=['pGR'])
                    S.op('dve', lambda e, th=th: e.tensor_copy(grep[:, th * 512:(th + 1) * 512], pGR[:]), reads=['pGR'], writes=[('grep', th)])
                for g in range(2):
                    for c8 in range(8):
                        c = g * 8 + c8
                        ws = nwb[0] % 3
                        nwb[0] += 1
                        S.dma('pool', wb[ws][:], self.w1_r[ex, c], writes=[('wmoe', ws)], max_dma_last_dim=4096)
                        for th in range(2):
                            s = ngl[0] % 2
                            ngl[0] += 1
                            tks = slice(th * 512, (th + 1) * 512)

                            def fgl(e, s=s, ws=ws, tks=tks):
                                self.mm_group(e, pGL[s][:, 0, :], [(wb[ws][:, kt, 0:P], h2T[:, kt, tks]) for kt in range(KT)])
                                return self.mm_group(e, pGL[s][:, 1, :], [(wb[ws][:, kt, P:2 * P], h2T[:, kt, tks]) for kt in range(KT)])
                            S.op('pe', fgl, reads=[('wmoe', ws)], writes=[('pGL', s)])
                            S.op('dve', lambda e, s=s, ex=ex, c=c: e.tensor_scalar(glu[s][:], pGL[s][:, 0, :], b1[:, ex, c:c + 1], 7.0, ALU.add, ALU.min),
                                 reads=[('pGL', s), 'b1'], writes=[('glu', s)])
                            S.op('act', lambda e, s=s: e.activation(sig[s][:], glu[s][:], AF.Sigmoid, scale=1.702), reads=[('glu', s)], writes=[('sig', s)])
                            S.op('dve', lambda e, s=s, ex=ex, c=c: e.tensor_scalar(lin1[s][:], pGL[s][:, 1, :], b1[:, ex, 16 + c:17 + c], 8.0, ALU.add, ALU.min),
                                 reads=[('pGL', s), 'b1'], writes=[('lin1', s)])
                            S.op('dve', lambda e, s=s: e.tensor_tensor(tg[s][:], glu[s][:], sig[s][:], ALU.mult), reads=[('glu', s), ('sig', s)], writes=[('tg', s)])
                            S.op('dve', lambda e, s=s: e.scalar_tensor_tensor(tg[s][:], lin1[s][:], -6.0, tg[s][:], ALU.max, ALU.mult),
                                 reads=[('lin1', s), ('tg', s)], writes=[('tg', s)])
                            S.op('dve', lambda e, s=s, c8=c8, tks=tks, th=th: e.tensor_tensor(actT[:, c8, tks], tg[s][:], grep[:, tks], ALU.mult),
                                 reads=[('tg', s), ('grep', th)], writes=[('actT', c8, th)])
                    for dc in range(4):
                        ws = nwb[0] % 3
                        nwb[0] += 1
                        wv = wb[ws][:].rearrange("p a b -> p (a b)").rearrange("p (f d) -> p f d", f=8)
                        S.dma('pool', wv, self.w2_r[ex, g, dc], writes=[('wmoe', ws)], max_dma_last_dim=4096)
                        for dsub in range(4):
                            dt = dc * 4 + dsub
                            for th in range(2):
                                s = ny[0] % 2
                                ny[0] += 1
                                S.op('pe', lambda e, s=s, wv=wv, dsub=dsub, th=th: self.mm_group(
                                    e, pY[s][:], [(wv[:, ft, dsub * P:(dsub + 1) * P], actT[:, ft, th * 512:(th + 1) * 512]) for ft in range(8)]),
                                    reads=[('wmoe', ws)] + [('actT', ft, th) for ft in range(8)], writes=[('pY', s)])
                                acc_add(s, dt, th)
            if self.debug:
                dk = []
                for t in range(NT_OWN):
                    S.dma('sp', self.dbg_acc2[:, t * D:(t + 1) * D], acc[:, t, :], reads=[('accT', dt, th) for dt in range(KT) for th in range(2)], writes=[('dbgacc2', t)])
                    dk.append(('dbgacc2', t))
                S.emit(final_wait_keys=dk)
            else:
                S.emit()

    def phase_final(self):
        nc = self.nc
        with contextlib.ExitStack() as st:
            sb = lambda n, s, d=F32: st.enter_context(nc.sbuf_tensor(n, s, d))
            S = self.S
            acc = self.acc
            ln2 = sb("ln2r", [P, 2, D])
            S.dma('sp', ln2[:, 0, :], self.ln2[0:1, :].partition_broadcast(P), writes=['ln2g'])
            S.dma('sp', ln2[:, 1, :], self.ln2[1:2, :].partition_broadcast(P), writes=['ln2b'])
            xo = [sb(f"xo{i}", [P, D]) for i in range(2)]
            stt = [sb(f"stt_f{i}", [P, 4, 6]) for i in range(2)]
            mv = [sb(f"mv_f{i}", [P, 6]) for i in range(2)]
            pT = [st.enter_context(nc.psum_tensor(f"pTf{i}", [P, 8, P], F32)) for i in range(3)]
            npt = 0
            dk = []
            for t in range(NT_OWN):
                s = t % 2
                for hf in range(2):
                    pi = npt % 3
                    npt += 1

                    def tr(e, hf=hf, pi=pi, t=t):
                        last = None
                        for k8 in range(8):
                            dt = hf * 8 + k8
                            last = e.transpose(pT[pi][:, k8, :], acc[:, t, dt * P:(dt + 1) * P], self.ident[:])
                        return last
                    S.op('pe', tr, reads=[], writes=[('pTf', pi)])
                    S.op('act', lambda e, s=s, hf=hf, pi=pi: e.activation(xo[s][:, hf * 1024:(hf + 1) * 1024].rearrange("p (k j) -> p k j", k=8), pT[pi][:], AF.Copy),
                         reads=[('pTf', pi)], writes=[('xo', s)])
                self.ln_stats(S, stt[s], mv[s], xo[s], ('xo', s), ('lnF', s))
                S.op('act', lambda e, s=s: e.activation(xo[s][:], xo[s][:], AF.Identity, bias=mv[s][:, 3:4], scale=mv[s][:, 2:3]),
                     reads=[('xo', s), (('lnF', s), 'nb'), (('lnF', s), 'rs')], writes=[('xo', s)])
                S.op('dve', lambda e, s=s: e.tensor_tensor(xo[s][:], xo[s][:], ln2[:, 0, :], ALU.mult), reads=[('xo', s), 'ln2g'], writes=[('xo', s)])
                S.op('dve', lambda e, s=s: e.tensor_tensor(xo[s][:], xo[s][:], ln2[:, 1, :], ALU.add), reads=[('xo', s), 'ln2b'], writes=[('xo', s)])
                S.dma('sp', self.y[t * P:(t + 1) * P, :], xo[s][:], reads=[('xo', s)], writes=[('y', t)])
                dk.append(('y', t))
            S.emit(final_wait_keys=dk)


def rope_tables():
    S = 2048
    t = np.arange(S)
    row = (t // 64 - (S // 64) // 2).astype(np.float32)
    col = (t % 64 - 32).astype(np.float32)
    inv = (10000.0 ** (-np.arange(0, 64, 2, dtype=np.float32) / 64.0)).astype(np.float32)
    ar = row[:, None] * inv[None, :]
    ac = col[:, None] * inv[None, :]
    cos = np.concatenate([np.cos(ar), np.cos(ar), np.cos(ac), np.cos(ac)], axis=1)
    sin = np.concatenate([-np.sin(ar), np.sin(ar), -np.sin(ac), np.sin(ac)], axis=1)
    return cos.astype(np.float32), sin.astype(np.float32)


def tile_w(w):
    K, N = w.shape
    return np.ascontiguousarray(w.reshape(KT, P, N // CW, CW).transpose(2, 1, 0, 3))


def prep_shared(inp, n_experts=32):
    l = 0
    NE = n_experts
    w1 = inp["w_exp_in"][l][:NE]
    w1 = np.ascontiguousarray(w1.reshape(NE, KT, P, 2, 16, P).transpose(0, 4, 2, 1, 3, 5)).reshape(NE, 16, P, KT, CW)
    w2 = inp["w_exp_out"][l][:NE]
    w2 = np.ascontiguousarray(w2.reshape(NE, 2, 8, P, 4, 512).transpose(0, 1, 4, 3, 2, 5))
    b1 = np.ascontiguousarray(inp["b_exp_in"][l].reshape(32, 32, P).transpose(2, 0, 1))
    return {"w1_r": w1, "w2_r": w2, "b1_r": b1, "b2": np.ascontiguousarray(inp["b_exp_out"][l]),
            "ln2": np.ascontiguousarray(np.stack([inp["ln2_g"][l], inp["ln2_b"][l]]))}


def prep_core(inp, core):
    b, half = core // 2, core % 2
    l = 0
    x = inp["x"][b]
    cos, sin = rope_tables()
    if half == 1:
        x = x[::-1]
        cos, sin = cos[::-1], sin[::-1]
    w_in = inp["w_in"][l]
    m = {
        "x_loc": np.ascontiguousarray(x),
        "c_col": np.ascontiguousarray(inp["c"][b].reshape(KT, P).T),
        "w_ada1": np.ascontiguousarray(inp["w_ada"][l][:, 0:2 * D]),
        "b_ada1": np.ascontiguousarray(inp["b_ada"][l][None, 0:2 * D]),
        "ident": np.eye(P, dtype=np.float32),
        "w_qkv": tile_w(w_in[:, 0:1536]),
        "qkw": np.stack([inp["q_norm_w"][l] * np.float32(1.0), inp["k_norm_w"][l]]).astype(np.float32),
        "cs": np.ascontiguousarray(np.stack([cos, sin])),
        "anw": np.ascontiguousarray(inp["attn_norm_w"][l][None, :]),
    }
    o_qr, o_ffw, o_fbw, o_i, o_g = 1536, 2560, 3584, 4608, 5632
    o_fa, o_fb = (o_ffw, o_fbw) if half == 0 else (o_fbw, o_ffw)
    cols = []
    for hh in range(8):
        sl = lambda o: w_in[:, o + hh * P:o + (hh + 1) * P]
        cols += [sl(o_qr), sl(o_fa), sl(o_fb), sl(o_i), sl(o_g), sl(o_g)]
    m["w_hg"] = tile_w(np.concatenate(cols, axis=1)).reshape(8, 3, P, KT, CW)
    lbr = inp["hgrn_lb"]
    dirs = (0, 1) if half == 0 else (1, 0)
    la = np.stack([np.concatenate([lbr[dirs[0], sl_].reshape(8, P).T, lbr[dirs[1], sl_].reshape(8, P).T], axis=1) for sl_ in range(2)])
    m["lb_a"] = np.ascontiguousarray(la.astype(np.float32))
    m["hnw"] = np.ascontiguousarray(inp["hgrn_norm_w"][l][None, :])
    jj, ii = np.meshgrid(np.arange(P), np.arange(P), indexing="ij")
    same = (jj // 64) == (ii // 64)
    m["hmask"] = np.stack([(same & (jj <= ii)), (same & (jj >= ii))]).astype(np.float32)
    m["rmask"] = (np.arange(S_OWN) % 64 != 0).astype(np.float32)[None, :]
    w_ada, b_ada = inp["w_ada"][l], inp["b_ada"][l]
    m["w_ada2"] = np.ascontiguousarray(w_ada[:, 2 * D:5 * D])
    m["b_ada2"] = np.ascontiguousarray(b_ada[None, 2 * D:5 * D])
    m["w_ada3"] = np.ascontiguousarray(w_ada[:, 5 * D:6 * D])
    m["b_g2col"] = np.ascontiguousarray(b_ada[5 * D:6 * D].reshape(KT, P).T)
    m["w_out_t"] = tile_w(inp["w_out"][l])
    m["ln1"] = np.ascontiguousarray(np.stack([inp["ln1_g"][l], inp["ln1_b"][l]]))
    m["w_r"] = np.ascontiguousarray(inp["w_router"][l].reshape(KT, P, 32).transpose(1, 0, 2))
    m["b_r"] = np.ascontiguousarray(inp["b_router"][l][None, :])
    m["halfm"] = np.stack([(np.arange(P) < 64), (np.arange(P) >= 64)], axis=1).astype(np.float32)
    return m


def kernel(**inputs):
    inp = {k: np.asarray(v) for k, v in inputs.items()}
    n = 8
    prog = Prog(['attn', 'hgrn', 'c', 'moe'], debug=False)
    shared = prep_shared(inp)
    in_maps = []
    for c in range(n):
        m = prep_core(inp, c)
        m.update(shared)
        in_maps.append(m)
    res = run_bass_kernel_spmd(prog.nc, in_maps, core_ids=list(range(n)))
    out = np.zeros((4, 2048, 2048), np.float32)
    for c in range(n):
        b, half = c // 2, c % 2
        y = np.asarray(res.results[c]["y_out"], dtype=np.float32)
        if half == 0:
            out[b, 0:1024] = y
        else:
            out[b, 1024:2048] = y[::-1]
    return out
```

```python
import contextlib
import os
import numpy as np
import concourse.bass as bass
import concourse.mybir as mybir
from concourse.bass_utils import run_bass_kernel_spmd

F32 = mybir.dt.float32
BF16 = mybir.dt.bfloat16
AF = mybir.ActivationFunctionType
ALU = mybir.AluOpType
AX = mybir.AxisListType

COMPUTE = ('pe', 'act', 'dve')
DMAQ = ('sp', 'pool')
ALLENG = COMPUTE + DMAQ
NDS = 8
SAME_ENGINE_SYNC = True
SEMCAP = int(os.environ.get("SEMCAP", "8000"))
NBANK = 6


class Sched:
    _uid = 0

    def __init__(self, nc):
        self.nc = nc
        Sched._uid += 1
        self.ops = {e: [] for e in ALLENG}
        self.n = {e: 0 for e in ALLENG}
        self.lastw = {}
        self.readers = {}
        self.seen = {e: {} for e in ALLENG}
        self.cond = None
        self._seen_backup = None
        self.regcache = {}
        self._nreg = 0

    def cond_begin(self, val_ap, thr, dep_key, cache_key, dma_uncond=False):
        assert self.cond is None
        self.cond = dict(ap=val_ap, thr=thr, dep=self.lastw[dep_key], key=cache_key, dma_uncond=dma_uncond)
        self._seen_backup = {e: dict(v) for e, v in self.seen.items()}

    def _cond_reg(self, eng, h, c):
        ent = self.regcache.get(eng)
        if ent is not None and ent[1] == c['key']:
            return ent[0]
        if ent is not None and ent[2] is not None:
            try:
                self.nc.free_register(ent[2])
            except Exception:
                pass
        t, v = c['dep']
        h.wait_ge(self.sems[t], v)
        reg = h.alloc_register(f"cr_{eng}_{self._nreg}")
        self._nreg += 1
        h.reg_load(reg, c['ap'])
        val = h.snap(reg, donate=True)
        self.regcache[eng] = [val, c['key'], reg]
        return val

    def cond_end(self):
        self.cond = None
        self.seen = self._seen_backup
        self._seen_backup = None

    @staticmethod
    def _tgt(eng, idx):
        if eng in DMAQ:
            return (eng, (idx - 1) % NDS), 16 * ((idx - 1) // NDS + 1)
        b = (idx - 1) // SEMCAP
        return (eng, b), idx - b * SEMCAP

    def op(self, eng, fn, reads=(), writes=()):
        deps = {}

        def add(t, v):
            if deps.get(t, 0) < v:
                deps[t] = v

        for k in reads:
            if k in self.lastw:
                add(*self.lastw[k])
        for k in writes:
            if k in self.lastw:
                add(*self.lastw[k])
            for t, v in self.readers.get(k, {}).items():
                add(t, v)
        idx = None
        if fn is not None:
            self.n[eng] += 1
            idx = self.n[eng]
            if eng in DMAQ and idx > NDS:
                add(*self._tgt(eng, idx - NDS))
        waits = []
        for t, v in deps.items():
            if t[0] == eng and eng == 'pe':
                continue
            if t[0] == eng and eng in COMPUTE and not SAME_ENGINE_SYNC:
                continue
            if self.seen[eng].get(t, 0) >= v:
                continue
            self.seen[eng][t] = v
            waits.append((t, v))
        self.ops[eng].append((waits, fn, idx, self.cond))
        if fn is None:
            return
        me = self._tgt(eng, idx)
        for k in writes:
            self.lastw[k] = me
            self.readers[k] = {}
        for k in reads:
            if k not in writes:
                r = self.readers.setdefault(k, {})
                if r.get(me[0], 0) < me[1]:
                    r[me[0]] = me[1]

    def dma(self, q, out, in_, reads=(), writes=(), **kw):
        c = self.cond
        if c is not None and c.get('dma_uncond'):
            saved, self.cond = self.cond, None
            self.op(q, lambda e: e.dma_start(out=out, in_=in_, **kw), reads=reads, writes=writes)
            self.cond = saved
            return
        self.op(q, lambda e: e.dma_start(out=out, in_=in_, **kw), reads=reads, writes=writes)

    def alloc_sems(self, stack):
        nc = self.nc
        self.sems = {}
        for e in COMPUTE:
            for b in range(NBANK):
                self.sems[(e, b)] = stack.enter_context(nc.semaphore(f"s_{e}_{b}"))
        for e in DMAQ:
            for s in range(NDS):
                self.sems[(e, s)] = stack.enter_context(nc.semaphore(f"s_{e}{s}"))

    def emit(self, final_wait_keys=()):
        nc = self.nc
        Sched._uid += 1
        self.op('sp', None, reads=list(final_wait_keys))
        finals = []
        for e in ALLENG:
            if self.n[e] == 0:
                continue
            if e in DMAQ:
                for s in range(min(NDS, self.n[e])):
                    last = ((self.n[e] - 1 - s) // NDS) * NDS + s + 1
                    finals.append(self._tgt(e, last))
            else:
                finals.append(self._tgt(e, self.n[e]))
        for e in ALLENG:
            w = []
            for (t, v) in finals:
                if self.seen[e].get(t, 0) < v and not (t[0] == e and e == 'pe'):
                    w.append((t, v))
                    self.seen[e][t] = v
            self.ops[e].append((w, None, None, None))
        sems = self.sems
        ops = self.ops
        self.ops = {e: [] for e in ALLENG}
        with nc.Block() as blk:
            def emit_one(eng, h, rec):
                waits, fn, idx, cnd = rec
                if False:
                    for t, v in waits[:-1]:
                        h.wait_ge(sems[t], v)
                    ins = fn(h)
                    ins._wait_ge(sems[waits[-1][0]], waits[-1][1])
                    t, _ = self._tgt(eng, idx)
                    ins.then_inc(sems[t], 1)
                    return
                for t, v in waits:
                    h.wait_ge(sems[t], v)
                if fn is not None:
                    ins = fn(h)
                    t, _ = self._tgt(eng, idx)
                    ins.then_inc(sems[t], 16 if eng in DMAQ else 1)

            def run(eng, h):
                lst = ops[eng]
                i = 0
                while i < len(lst):
                    c = lst[i][3]
                    if c is None:
                        emit_one(eng, h, lst[i])
                        i += 1
                        continue
                    j = i
                    while j < len(lst) and lst[j][3] is c:
                        j += 1
                    thr = c['thr']
                    if True:
                        val = self._cond_reg(eng, h, c)
                    else:
                        ent = self.regcache.get(eng)
                        if ent is None or ent[1] != c['key']:
                            t, v = c['dep']
                            h.wait_ge(sems[t], v)
                            ent = [h.value_load(c['ap'], min_val=0, max_val=1024), c['key']]
                            self.regcache[eng] = ent
                        val = ent[0]
                    with h.If(val > thr):
                        for r in lst[i:j]:
                            emit_one(eng, h, r)
                    if os.environ.get("NO_SKIPINC") is not None and eng not in COMPUTE:
                        i = j
                        continue
                    with h.If(val <= thr):
                        if eng in COMPUTE:
                            per = {}
                            for r in lst[i:j]:
                                if r[1] is not None:
                                    tb = self._tgt(eng, r[2])[0]
                                    per[tb] = per.get(tb, 0) + 1
                            for tb, n_ in per.items():
                                h.sem_inc(sems[tb], n_)
                        else:
                            for r in lst[i:j]:
                                if r[1] is not None:
                                    h.sem_inc(sems[self._tgt(eng, r[2])[0]], 16)
                    i = j

            blk.tensor(lambda h: run('pe', h))
            blk.scalar(lambda h: run('act', h))
            blk.vector(lambda h: run('dve', h))
            blk.gpsimd(lambda h: run('pool', h))
            blk.sync(lambda h: run('sp', h))


P = 128
D = 2048
KT = 16
S_ALL = 2048
S_OWN = 1024
NT_ALL = 16
NT_OWN = 8
CW = 256
EPS = 1e-6


class Prog:
    def __init__(self, stages, debug=False, n_experts=32):
        self.NE = n_experts
        self.stages = stages
        self.debug = debug
        nc = self.nc = bass.Bass("TRN2", target_bir_lowering=False)
        di = lambda n, s: nc.dram_tensor(n, s, F32, kind="ExternalInput").ap()
        self.x = di("x_loc", [S_ALL, D])
        self.c_col = di("c_col", [P, KT])
        self.w_ada1 = di("w_ada1", [D, 2 * D])
        self.b_ada1 = di("b_ada1", [1, 2 * D])
        self.ident_d = di("ident", [P, P])
        self.w_qkv = di("w_qkv", [6, P, KT, CW])
        self.qkw = di("qkw", [2, P])
        self.cs = di("cs", [2, S_ALL, P])
        self.anw = di("anw", [1, 1024])
        self.y = nc.dram_tensor("y_out", [S_OWN, D], F32, kind="ExternalOutput").ap()
        self.w_hg = di("w_hg", [8, 3, P, KT, CW])
        self.lb_a = di("lb_a", [2, P, 16])
        self.hnw = di("hnw", [1, 1024])
        self.hmask = di("hmask", [2, P, P])
        self.rmask_d = di("rmask", [1, S_OWN])
        self.halfm_d = di("halfm", [P, 2])
        self.w_ada2 = di("w_ada2", [D, 3 * D])
        self.b_ada2 = di("b_ada2", [1, 3 * D])
        self.w_ada3 = di("w_ada3", [D, D])
        self.b_g2col = di("b_g2col", [P, KT])
        self.w_out_t = di("w_out_t", [8, P, KT, CW])
        self.ln1 = di("ln1", [2, D])
        self.w_r = di("w_r", [P, KT, 32])
        self.b_r = di("b_r", [1, 32])
        self.w1_r = di("w1_r", [self.NE, 16, P, KT, CW])
        if 'moe_sp' not in stages:
            self.w2_r = di("w2_r", [self.NE, 2, 4, P, 8, 512])
        self.b1_r = di("b1_r", [P, 32, 32])
        self.b2 = di("b2", [32, D])
        self.ln2 = di("ln2", [2, D])
        if 'moe_sp' in stages:
            self.w2_s = di("w2_s", [self.NE, 8, P, KT, CW])
        self.b_ada3 = di("b_ada3", [1, D])
        self.utri_d = di("utri", [P, P])
        self.iota_d = di("iota", [1, S_OWN])
        self.slotidx_d = di("slotidx", [P, 8])
        if debug:
            do = lambda n, s: nc.dram_tensor(n, s, F32, kind="ExternalOutput").ap()
            self.dbg_h = do("dbg_h", [S_ALL, D])
            self.dbg_q = do("dbg_q", [S_OWN, 1024])
            self.dbg_k = do("dbg_k", [S_ALL, 256])
            self.dbg_oa = do("dbg_oa", [S_OWN, 1024])
            self.dbg_mod = do("dbg_mod", [P, 2 * D])
            self.dbg_or = do("dbg_or", [S_OWN, 1024])
            self.dbg_os = do("dbg_os", [S_OWN, 1024])
            self.dbg_x1 = do("dbg_x1", [S_OWN, D])
            self.dbg_h2 = do("dbg_h2", [S_OWN, D])
            self.dbg_lg = do("dbg_lg", [S_OWN, 32])
            self.dbg_gt = do("dbg_gt", [32, S_OWN])
            self.dbg_acc = do("dbg_acc", [P, 8 * D])
            self.dbg_acc2 = do("dbg_acc2", [P, 8 * D])
        self.build()

    def mm_group(self, e, out, pairs):
        last = None
        n = len(pairs)
        for i, (l, r) in enumerate(pairs):
            last = e.matmul(out, l, r, start=(i == 0), stop=(i == n - 1))
        return last

    def build(self):
        nc = self.nc
        with contextlib.ExitStack() as top:
            sb = lambda n, s, d=F32: top.enter_context(nc.sbuf_tensor(n, s, d))
            self.S = Sched(nc)
            self.S.alloc_sems(top)
            self.ident = sb("ident_s", [P, P])
            self.eps_t = sb("eps_t", [P, 1])
            self.gatesT = sb("gatesT", [32, S_OWN])
            self.g2col = sb("g2col", [P, KT])
            self.sparse = 'moe_sp' in self.stages
            if self.sparse:
                self.g2rep = sb("g2rep", [P, D])
                self.Mtm = sb("Mtm", [P, NT_OWN, 32], BF16)
                self.Mf = sb("Mf", [P, NT_OWN, 32])
            self.R1 = sb("R1", [P, KT, S_ALL], BF16)
            self.hT = self.R1
            self.acc = self.R1[:].rearrange("p a b -> p (a b)").bitcast(F32).rearrange("p (t d) -> p t d", t=NT_OWN)
            self.R2 = sb("R2", [P, 16, S_OWN], BF16)
            self.mixedT = self.R2
            self.h2T = self.R2
            self.h2tm = self.R2[:].rearrange("p a b -> p (a b)").rearrange("p (t d) -> p t d", t=NT_OWN)
            self.phase_A()
            if 'attn' in self.stages:
                self.phase_B1()
            if 'hgrn' in self.stages:
                self.phase_B2()
            if 'c' in self.stages:
                import os
                ccut = int(os.environ.get("C_CUT", "9"))
                self.phase_C1()
                with contextlib.ExitStack() as cs:
                    self.mod2 = cs.enter_context(nc.sbuf_tensor("mod2", [P, 3 * D], F32))
                    if ccut >= 2:
                        self.phase_C0()
                    if ccut >= 3:
                        self.phase_C2()
            if 'moe' in self.stages:
                self.phase_MoE()
                self.phase_final()
            if self.sparse:
                self.phase_MoE_sp()
                self.phase_final()

    def ada_part(self, S, st, w_ap, b_ap, ncols, dest, carep, plus_one_from, tag='mod', reuse=False):
        nc = self.nc
        if reuse:
            wbuf, bb, ps = self._ada_bufs
        else:
            wbuf = [st.enter_context(nc.sbuf_tensor(f"adaw{i}_{Sched._uid}", [P, KT, 512], BF16)) for i in range(2)]
            bb = [st.enter_context(nc.sbuf_tensor(f"adab{i}_{Sched._uid}", [P, 512], F32)) for i in range(2)]
            ps = [st.enter_context(nc.psum_tensor(f"adap{i}_{Sched._uid}", [P, 512], F32)) for i in range(2)]
            self._ada_bufs = (wbuf, bb, ps)
        for c in range(ncols // 512):
            s = c % 2
            S.dma('pool', wbuf[s][:], w_ap[:, c * 512:(c + 1) * 512].rearrange("(kt p) n -> p kt n", p=P),
                  writes=[('adaw', s)])
            S.dma('sp', bb[s][:], b_ap[:, c * 512:(c + 1) * 512].partition_broadcast(P), writes=[('adab', s)])
            S.op('pe', lambda e, s=s: self.mm_group(e, ps[s][:], [(carep[:, kt, :], wbuf[s][:, kt, :]) for kt in range(KT)]),
                 reads=[('adaw', s), 'carep'], writes=[('adap', s)])
            d = dest[:, c * 512:(c + 1) * 512]
            if c * 512 >= plus_one_from:
                S.op('dve', lambda e, s=s, d=d: e.scalar_tensor_tensor(d, ps[s][:], 1.0, bb[s][:], ALU.add, ALU.add),
                     reads=[('adap', s), ('adab', s)], writes=[(tag, c)])
            else:
                S.op('dve', lambda e, s=s, d=d: e.tensor_tensor(d, ps[s][:], bb[s][:], ALU.add),
                     reads=[('adap', s), ('adab', s)], writes=[(tag, c)])

    def ln_stats(self, S, st_t, mv_t, src, key_src, tag):
        def f(e):
            last = None
            for j in range(4):
                last = e.bn_stats(st_t[:, j, :], src[:, j * 512:(j + 1) * 512])
            return last
        S.op('dve', f, reads=[key_src], writes=[(tag, 'st')])
        S.op('dve', lambda e: e.bn_aggr(mv_t[:, 0:2], st_t[:].rearrange("p a b -> p (a b)")),
             reads=[(tag, 'st')], writes=[(tag, 'mv')])
        S.op('act', lambda e: e.activation(mv_t[:, 2:3], mv_t[:, 1:2], AF.Sqrt, bias=self.eps_t[:, 0:1], scale=1.0),
             reads=[(tag, 'mv')], writes=[(tag, 'sd')])
        r, v, t = mv_t[:, 2:3], mv_t[:, 4:5], mv_t[:, 5:6]
        S.op('dve', lambda e: e.reciprocal(r, r), reads=[(tag, 'sd')], writes=[(tag, 'rs')])
        S.op('dve', lambda e: e.tensor_scalar(v, mv_t[:, 1:2], EPS, None, ALU.add), reads=[(tag, 'mv')], writes=[(tag, 'v')])
        for _ in range(1):
            S.op('dve', lambda e: e.tensor_tensor(t, r, r, ALU.mult), reads=[(tag, 'rs')], writes=[(tag, 't')])
            S.op('dve', lambda e: e.tensor_scalar(t, t, v, -0.5, ALU.mult, ALU.mult), reads=[(tag, 't'), (tag, 'v')], writes=[(tag, 't')])
            S.op('dve', lambda e: e.scalar_tensor_tensor(r, t, 1.5, r, ALU.add, ALU.mult), reads=[(tag, 't'), (tag, 'rs')], writes=[(tag, 'rs')])
        S.op('dve', lambda e: e.tensor_scalar(mv_t[:, 3:4], mv_t[:, 0:1], mv_t[:, 2:3], -1.0, ALU.mult, ALU.mult),
             reads=[(tag, 'rs'), (tag, 'mv')], writes=[(tag, 'nb')])

    def phase_A(self):
        nc = self.nc
        with contextlib.ExitStack() as st:
            sb = lambda n, s, d=F32: st.enter_context(nc.sbuf_tensor(n, s, d))
            S = self.S
            S.op('dve', lambda e: e.memset(self.eps_t[:], EPS), writes=['eps'])
            S.dma('sp', self.ident[:], self.ident_d, writes=['ident'])
            carep = self.make_carep(S, st)[0]
            mod1 = sb("mod1", [P, 2 * D])
            self.ada_part(S, st, self.w_ada1, self.b_ada1, 2 * D, mod1, carep, D)
            modkeys = [('mod', c) for c in range(8)]
            if self.debug:
                S.dma('sp', self.dbg_mod, mod1[:], reads=modkeys, writes=['dbg_mod'])
            xin = [sb(f"xin{i}", [P, D]) for i in range(2)]
            hn = [sb(f"hn{i}", [P, D]) for i in range(2)]
            stt = [sb(f"stt{i}", [P, 4, 6]) for i in range(2)]
            mv = [sb(f"mv{i}", [P, 6]) for i in range(2)]
            pT = [st.enter_context(nc.psum_tensor(f"pT{i}", [P, 8, P], F32)) for i in range(3)]
            npt = 0
            for t in range(NT_ALL):
                s = t % 2
                S.dma('sp', xin[s][:], self.x[t * P:(t + 1) * P, :], writes=[('xin', s)])
                self.ln_stats(S, stt[s], mv[s], xin[s], ('xin', s), ('lnA', s))
                S.op('act', lambda e, s=s: e.activation(hn[s][:], xin[s][:], AF.Identity, bias=mv[s][:, 3:4], scale=mv[s][:, 2:3]),
                     reads=[('xin', s), (('lnA', s), 'nb'), (('lnA', s), 'rs')], writes=[('hn', s)])
                S.op('dve', lambda e, s=s: e.tensor_tensor(hn[s][:], hn[s][:], mod1[:, D:2 * D], ALU.mult),
                     reads=[('hn', s)] + modkeys, writes=[('hn', s)])
                S.op('dve', lambda e, s=s: e.tensor_tensor(hn[s][:], hn[s][:], mod1[:, 0:D], ALU.add),
                     reads=[('hn', s)] + modkeys, writes=[('hn', s)])
                if self.debug:
                    S.dma('sp', self.dbg_h[t * P:(t + 1) * P, :], hn[s][:], reads=[('hn', s)], writes=[('dbg_h', t)])
                for hf in range(2):
                    pi = npt % 3
                    npt += 1

                    def tr(e, s=s, hf=hf, pi=pi):
                        last = None
                        for k8 in range(8):
                            kt = hf * 8 + k8
                            last = e.transpose(pT[pi][:, k8, :], hn[s][:, kt * P:(kt + 1) * P], self.ident[:])
                        return last
                    S.op('pe', tr, reads=[('hn', s), 'ident'], writes=[('pT', pi)])
                    S.op('act', lambda e, t=t, hf=hf, pi=pi: e.activation(self.hT[:, hf * 8:(hf + 1) * 8, t * P:(t + 1) * P], pT[pi][:], AF.Copy),
                         reads=[('pT', pi)], writes=[('hT', t, hf)])
            S.emit(final_wait_keys=[('dbg_h', t) for t in range(NT_ALL)] if self.debug else [])

    def phase_B1(self):
        nc = self.nc
        with contextlib.ExitStack() as st:
            sb = lambda n, s, d=F32: st.enter_context(nc.sbuf_tensor(n, s, d))
            qT = sb("qT", [P, 8, S_OWN], BF16)
            kT = sb("kT", [P, 2, S_ALL], BF16)
            v_aug = sb("v_aug", [P, NT_ALL, 2, 132], BF16)
            self.B1a(qT, kT, v_aug)
            self.B1b(qT, kT, v_aug)

    def B1a(self, qT, kT, v_aug):
        nc = self.nc
        with contextlib.ExitStack() as st:
            sb = lambda n, s, d=F32: st.enter_context(nc.sbuf_tensor(n, s, d))
            S = self.S
            S.op('dve', lambda e: e.memset(v_aug[:, :, :, 128:129], 1.0), writes=['vones'])
            nw = sb("nw", [P, 2, P])
            S.dma('sp', nw[:, 0, :], self.qkw[0:1, :].partition_broadcast(P), writes=['nw0'])
            S.dma('sp', nw[:, 1, :], self.qkw[1:2, :].partition_broadcast(P), writes=['nw1'])
            wb = [sb(f"wqkv{i}", [P, KT, CW], BF16) for i in range(3)]
            cst = [sb(f"cst{i}", [P, 2, P]) for i in range(2)]
            sq = [sb(f"sq{i}", [P, 2, P]) for i in range(2)]
            qn = [sb(f"qn{i}", [P, 2, P]) for i in range(2)]
            t1 = [sb(f"t1{i}", [P, 2, P]) for i in range(2)]
            t2 = [sb(f"t2{i}", [P, 2, P]) for i in range(2)]
            ss = [sb(f"ss{i}", [P, 4]) for i in range(2)]
            pq = [st.enter_context(nc.psum_tensor(f"pq{i}", [P, CW], F32)) for i in range(2)]
            ptr = [st.enter_context(nc.psum_tensor(f"ptr{i}", [P, 2, P], F32)) for i in range(2)]
            it = 0
            for j in range(6):
                ws = j % 3
                S.dma('pool', wb[ws][:], self.w_qkv[j], writes=[('wb', ws)], max_dma_last_dim=4096)
                ntiles = NT_OWN if j < 4 else NT_ALL
                for t in range(ntiles):
                    s = it % 2
                    it += 1
                    S.op('pe', lambda e, s=s, t=t, ws=ws: self.mm_group(
                        e, pq[s][:], [(self.hT[:, kt, t * P:(t + 1) * P], wb[ws][:, kt, :]) for kt in range(KT)]),
                        reads=[('wb', ws), ('hT', t, 0), ('hT', t, 1)], writes=[('pq', s)])
                    pq3 = pq[s][:].rearrange("p (h d) -> p h d", h=2)
                    if j == 5:
                        S.op('act', lambda e, t=t, pq3=pq3: e.activation(v_aug[:, t, :, 0:128], pq3, AF.Copy),
                             reads=[('pq', s)], writes=[('v', t)])
                        continue
                    wi = 0 if j < 4 else 1
                    S.dma('sp', cst[s][:], self.cs[:, t * P:(t + 1) * P, :].rearrange("c t d -> t c d"), writes=[('cst', s)])
                    S.op('act', lambda e, s=s: e.activation(sq[s][:].rearrange("p h d -> p (h d)"), pq[s][:], AF.Square),
                         reads=[('pq', s)], writes=[('sq', s)])
                    S.op('dve', lambda e, s=s: e.tensor_reduce(ss[s][:, 0:2], sq[s][:], AX.X, ALU.add),
                         reads=[('sq', s)], writes=[('ss', s)])
                    S.op('act', lambda e, s=s: e.activation(ss[s][:, 2:4], ss[s][:, 0:2], AF.Sqrt, bias=self.eps_t[:, 0:1], scale=1.0 / 128),
                         reads=[('ss', s)], writes=[('sd', s)])
                    S.op('dve', lambda e, s=s: e.reciprocal(ss[s][:, 2:4], ss[s][:, 2:4]), reads=[('sd', s)], writes=[('rs', s)])
                    S.op('dve', lambda e, s=s, pq3=pq3: e.tensor_tensor(qn[s][:], pq3, ss[s][:, 2:4].unsqueeze(2).to_broadcast([P, 2, P]), ALU.mult),
                         reads=[('pq', s), ('rs', s)], writes=[('qn', s)])
                    S.op('dve', lambda e, s=s, wi=wi: e.tensor_tensor(qn[s][:], qn[s][:], nw[:, wi:wi + 1, :].to_broadcast([P, 2, P]), ALU.mult),
                         reads=[('qn', s), 'nw0', 'nw1'], writes=[('qn', s)])
                    S.op('dve', lambda e, s=s: e.tensor_tensor(t1[s][:], qn[s][:], cst[s][:, 0:1, :].to_broadcast([P, 2, P]), ALU.mult),
                         reads=[('qn', s), ('cst', s)], writes=[('t1', s)])
                    v5 = lambda a: a.rearrange("p h (a f d) -> p h a f d", a=2, f=2)
                    for f in range(2):
                        S.op('dve', lambda e, s=s, f=f: e.tensor_tensor(
                            v5(t2[s][:])[:, :, :, f, :], v5(qn[s][:])[:, :, :, 1 - f, :],
                            cst[s][:, 1, :].rearrange("p (a f d) -> p a f d", a=2, f=2)[:, :, f, :].unsqueeze(1).to_broadcast([P, 2, 2, 32]),
                            ALU.mult), reads=[('qn', s), ('cst', s)], writes=[('t2', s, f)])
                    S.op('dve', lambda e, s=s: e.tensor_tensor(t1[s][:], t1[s][:], t2[s][:], ALU.add),
                         reads=[('t1', s), ('t2', s, 0), ('t2', s, 1)], writes=[('t1', s)])
                    if self.debug:
                        dst = self.dbg_q[t * P:(t + 1) * P, j * CW:(j + 1) * CW] if j < 4 else self.dbg_k[t * P:(t + 1) * P, :]
                        S.dma('sp', dst, t1[s][:].rearrange("p h d -> p (h d)"), reads=[('t1', s)], writes=[('dbgqk', j, t)])

                    def tr(e, s=s):
                        e.transpose(ptr[s][:, 0, :], t1[s][:, 0, :], self.ident[:])
                        return e.transpose(ptr[s][:, 1, :], t1[s][:, 1, :], self.ident[:])
                    S.op('pe', tr, reads=[('t1', s)], writes=[('ptr', s)])
                    dstT = qT[:, 2 * j:2 * j + 2, t * P:(t + 1) * P] if j < 4 else kT[:, :, t * P:(t + 1) * P]
                    S.op('act', lambda e, s=s, dstT=dstT: e.activation(dstT, ptr[s][:], AF.Copy),
                         reads=[('ptr', s)], writes=[('qkT', j, t)])
            fk = [('dbgqk', j, t) for j in range(5) for t in range(NT_OWN if j < 4 else NT_ALL)] if self.debug else []
            S.emit(final_wait_keys=fk)

    def B1b(self, qT, kT, v_aug):
        nc = self.nc
        with contextlib.ExitStack() as st:
            sb = lambda n, s, d=F32: st.enter_context(nc.sbuf_tensor(n, s, d))
            S = self.S
            anw = sb("anw_rep", [P, 1024])
            S.dma('sp', anw[:], self.anw.partition_broadcast(P), writes=['anw'])
            zt = sb("zt", [P, 1024])
            S.op('dve', lambda e: e.memset(zt[:], 0.0), writes=['zt'])
            dk = []
            for t in range(NT_OWN if 'moe' not in self.stages else 0):
                S.dma('sp', self.y[t * P:(t + 1) * P, 1024:2048], zt[:], reads=['zt'], writes=[('yz', t)])
                dk.append(('yz', t))
            E = [sb(f"E{i}", [P, NT_ALL, 512], BF16) for i in range(2)]
            o = [sb(f"o{i}", [P, P]) for i in range(2)]
            on = [sb(f"on{i}", [P, P]) for i in range(2)]
            junk = [sb(f"junk{i}", [P, P]) for i in range(2)]
            sc = [sb(f"sc{i}", [P, 4]) for i in range(2)]
            pS = [st.enter_context(nc.psum_tensor(f"pS{i}", [P, 512], F32)) for i in range(3)]
            pO = [st.enter_context(nc.psum_tensor(f"pO{i}", [P, 132], F32)) for i in range(2)]
            pX = [st.enter_context(nc.psum_tensor(f"pX{i}", [P, P], F32)) for i in range(2)]
            ns = 0
            no = 0
            for h in range(8):
                kvh = h // 4
                for qc in range(2):
                    es = (h * 2 + qc) % 2
                    for j in range(NT_ALL):
                        s = ns % 3
                        ns += 1
                        S.op('pe', lambda e, s=s, j=j, h=h, qc=qc, kvh=kvh: e.matmul(
                            pS[s][:], kT[:, kvh, j * P:(j + 1) * P], qT[:, h, qc * 512:(qc + 1) * 512], start=True, stop=True),
                            reads=[], writes=[('pS', s)])
                        S.op('act', lambda e, s=s, j=j, es=es: e.activation(E[es][:, j, :], pS[s][:], AF.Exp, scale=float(1.0 / np.sqrt(128.0))),
                             reads=[('pS', s)], writes=[('E', es, j)])
                    for qt in range(4):
                        s = no % 2
                        no += 1
                        tq = qc * 4 + qt
                        S.op('pe', lambda e, s=s, es=es, qt=qt, kvh=kvh: self.mm_group(
                            e, pO[s][:, 0:129], [(E[es][:, j, qt * P:(qt + 1) * P], v_aug[:, j, kvh, 0:129]) for j in range(NT_ALL)]),
                            reads=[('E', es, j) for j in range(NT_ALL)], writes=[('pO', s)])
                        S.op('dve', lambda e, s=s: e.reciprocal(sc[s][:, 0:1], pO[s][:, 128:129]), reads=[('pO', s)], writes=[('rden', s)])
                        S.op('dve', lambda e, s=s: e.tensor_scalar(o[s][:], pO[s][:, 0:128], sc[s][:, 0:1], None, ALU.mult),
                             reads=[('pO', s), ('rden', s)], writes=[('o', s)])
                        S.op('act', lambda e, s=s: e.activation(junk[s][:], o[s][:], AF.Square, accum_out=sc[s][:, 1:2]),
                             reads=[('o', s)], writes=[('oss', s), ('junk', s)])
                        S.op('act', lambda e, s=s: e.activation(sc[s][:, 2:3], sc[s][:, 1:2], AF.Sqrt, bias=self.eps_t[:, 0:1], scale=1.0 / 128),
                             reads=[('oss', s)], writes=[('osd', s)])
                        S.op('dve', lambda e, s=s: e.reciprocal(sc[s][:, 2:3], sc[s][:, 2:3]), reads=[('osd', s)], writes=[('ors', s)])
                        S.op('dve', lambda e, s=s, h=h: e.scalar_tensor_tensor(on[s][:], o[s][:], sc[s][:, 2:3], anw[:, h * P:(h + 1) * P], ALU.mult, ALU.mult),
                             reads=[('o', s), ('ors', s), 'anw'], writes=[('on', s)])
                        if 'moe' not in self.stages:
                            S.dma('sp', self.y[tq * P:(tq + 1) * P, h * P:(h + 1) * P], on[s][:], reads=[('on', s)], writes=[('yo', h, tq)])
                            dk.append(('yo', h, tq))
                        if self.debug:
                            S.dma('sp', self.dbg_oa[tq * P:(tq + 1) * P, h * P:(h + 1) * P], on[s][:], reads=[('on', s)], writes=[('dbgoa', h, tq)])
                            dk.append(('dbgoa', h, tq))
                        S.op('pe', lambda e, s=s: e.transpose(pX[s][:], on[s][:], self.ident[:]), reads=[('on', s)], writes=[('pX', s)])
                        S.op('act', lambda e, s=s, h=h, tq=tq: e.activation(self.mixedT[:, h, tq * P:(tq + 1) * P], pX[s][:], AF.Copy),
                             reads=[('pX', s)], writes=[('mixedT', h, tq)])
            S.emit(final_wait_keys=dk)

    def phase_B2(self):
        nc = self.nc
        hT = self.hT
        with contextlib.ExitStack() as st:
            sb = lambda n, s, d=F32: st.enter_context(nc.sbuf_tensor(n, s, d))
            ps = lambda n, s: st.enter_context(nc.psum_tensor(n, s, F32))
            S = self.S
            N = S_OWN
            NCH = 16
            lba = sb("lba", [P, 2, 16]); lb = sb("lb", [P, 16]); oml = sb("oml", [P, 16])
            S.dma('sp', lba[:], self.lb_a.rearrange("s p c -> p s c"), writes=['lba'])
            S.op('dve', lambda e: e.tensor_tensor(oml[:], lba[:, 0, :], lba[:, 1, :], ALU.subtract), reads=['lba'], writes=['oml'])
            S.op('act', lambda e: e.activation(lb[:], oml[:], AF.Sigmoid), reads=['oml'], writes=['lb'])
            S.op('dve', lambda e: e.tensor_scalar(oml[:], lb[:], -1.0, 1.0, ALU.mult, ALU.add), reads=['lb'], writes=['oml'])
            msk = sb("hmask_s", [P, 2, P])
            S.dma('sp', msk[:], self.hmask.rearrange("m j i -> j m i"), writes=['msk'])
            rmask = sb("rmask_s", [P, N])
            S.dma('sp', rmask[:], self.rmask_d.partition_broadcast(P), writes=['rmask'])
            hnw = [sb(f"hnw{i}", [P, P]) for i in range(2)]
            wb = [sb(f"whg{i}", [P, KT, CW], BF16) for i in range(2)]
            qr = sb("qr", [P, N]); sgA = sb("sgA", [P, N]); sgB = sb("sgB", [P, N])
            v_tm = sb("v_tm", [P, NT_ALL, P], BF16); sg_tm = sb("sg_tm", [64, NCH, P], BF16)
            X2 = sb("X2", [P, N]); X3 = sb("X3", [P, N]); X4 = sb("X4", [P, N]); X5 = sb("X5", [P, N])
            kdec = [sb(f"kdec{i}", [P, N], BF16) for i in range(2)]
            qdec = [sb(f"qdec{i}", [P, N], BF16) for i in range(2)]
            kend_tm = sb("kend_tm", [P, 2, NT_OWN, P], BF16)
            halfm = sb("halfm_s", [P, 2])
            S.dma('sp', halfm[:], self.halfm_d, writes=['halfm'])
            Sbf = [sb(f"Sbf{i}", [P, NCH, P], BF16) for i in range(2)]
            S32 = sb("S32", [P, 2, P]); etot = sb("etot", [P, NCH]); scur = [0]
            scm_ = [[sb(f"scm{j}_{i}", [P, P], BF16) for i in range(2)] for j in range(2)]
            oh_ = [sb(f"oh{j}", [64, 2, P]) for j in range(2)]; oh2_ = [sb(f"oh2{j}", [64, 2, P]) for j in range(2)]
            junk_ = [sb(f"hjunk{j}", [64, P]) for j in range(2)]; sc_ = [sb(f"hsc{j}", [64, 8]) for j in range(2)]
            pP = [ps(f"pP{i}", [P, 512]) for i in range(2)]
            pV = ps("pV", [P, 4, P]); pTk = ps("pTk", [P, 4, P]); pU = ps("pU", [P, 4, P])
            pSc = ps("pSc", [P, 2, P]); pOh = ps("pOh", [64, 2, P]); pX = ps("pXh", [P, 2, 64])
            npp = [0]
            nwb = [0]
            dk = []

            def load_w(hh, c):
                s = nwb[0] % 2
                nwb[0] += 1
                S.dma('pool', wb[s][:], self.w_hg[hh, c], writes=[('whg', s)], max_dma_last_dim=4096)
                return s

            def proj_fm(ws, col0, tok0, ntok, func, dest, key):
                for c in range(ntok // 512):
                    s = npp[0] % 2
                    npp[0] += 1
                    S.op('pe', lambda e, s=s, c=c: self.mm_group(e, pP[s][:], [
                        (wb[ws][:, kt, col0:col0 + P], hT[:, kt, tok0 + c * 512:tok0 + (c + 1) * 512]) for kt in range(KT)]),
                        reads=[('whg', ws)], writes=[('pP', s)])
                    S.op('act', lambda e, s=s, c=c: e.activation(dest[:, c * 512:(c + 1) * 512], pP[s][:], func),
                         reads=[('pP', s)], writes=[(key, c)])

            def proj_tm(ws, col0, ntiles, func, dest, key):
                for g in range(ntiles // 4):
                    def f(e, g=g):
                        last = None
                        for q in range(4):
                            t = g * 4 + q
                            last = self.mm_group(e, pV[:, q, :], [(hT[:, kt, t * P:(t + 1) * P], wb[ws][:, kt, col0:col0 + P]) for kt in range(KT)])
                        return last
                    S.op('pe', f, reads=[('whg', ws)], writes=['pV'])
                    S.op('act', lambda e, g=g: e.activation(dest[:, g * 4:(g + 1) * 4, :], pV[:], func), reads=['pV'], writes=[(key, g)])

            import os
            cut = int(os.environ.get("HG_CUT", "9"))

            def gate_pass(hh, di, sg, sgkeys, own, first_state):
                if cut < 2:
                    return
                col = di * 8 + hh
                fg, kk, lf, b, bb = sg, X2, X3, X4, X5
                S.op('dve', lambda e: e.tensor_scalar(fg[:], sg[:], oml[:, col:col + 1], lb[:, col:col + 1], ALU.mult, ALU.add),
                     reads=sgkeys + ['oml', 'lb'], writes=['fg'])
                S.op('dve', lambda e: e.tensor_scalar(kk[:], fg[:], -1.0, 1.0, ALU.mult, ALU.add), reads=['fg'], writes=['X2'])
                S.op('act', lambda e: e.activation(lf[:], fg[:], AF.Ln), reads=['fg'], writes=['X3'])
                S.op('dve', lambda e: e.tensor_tensor_scan(b[:], rmask[:], lf[:], 0.0, ALU.mult, ALU.add), reads=['X3', 'rmask'], writes=['X4'])
                b3 = b[:].rearrange("p (c i) -> p c i", i=64)
                S.op('act', lambda e: e.activation(etot[:].unsqueeze(2), b3[:, :, 63:64], AF.Exp), reads=['X4'], writes=['etot'])
                if di == 0:
                    bbt, bbk = b, 'X4'
                else:
                    S.op('dve', lambda e: e.tensor_tensor(bb[:], lf[:], b[:], ALU.subtract), reads=['X3', 'X4'], writes=['X5'])
                    bb3 = bb[:].rearrange("p (c i) -> p c i", i=64)
                    S.op('dve', lambda e: e.tensor_tensor(bb3, bb3, b3[:, :, 63:64].to_broadcast([P, NCH, 64]), ALU.add), reads=['X5', 'X4'], writes=['X5'])
                    bbt, bbk = bb, 'X5'
                en = lf
                S.op('act', lambda e: e.activation(en[:], bbt[:], AF.Exp, scale=-1.0), reads=[bbk, 'X3'], writes=['X3'])
                S.op('dve', lambda e: e.tensor_tensor(kk[:], kk[:], en[:], ALU.mult), reads=['X2', 'X3'], writes=['X2'])
                if own:
                    S.op('act', lambda e: e.activation(kdec[di][:], kk[:], AF.Copy), reads=['X2'], writes=[('kdec', di)])
                    S.op('act', lambda e: e.activation(en[:], bbt[:], AF.Exp), reads=[bbk, 'X2'], writes=['X3'])
                    S.op('dve', lambda e: e.tensor_tensor(qdec[di][:], qr[:], en[:], ALU.mult), reads=['X3', ('qr', 0), ('qr', 1)], writes=[('qdec', di)])
                kend32 = b if di == 1 else bb
                kkey = 'X4' if di == 1 else 'X5'
                S.op('dve', lambda e: e.tensor_tensor(kend32[:].rearrange("p (c i) -> p c i", i=64), kk[:].rearrange("p (c i) -> p c i", i=64),
                                                      etot[:].unsqueeze(2).to_broadcast([P, NCH, 64]), ALU.mult),
                     reads=['X2', 'etot', 'X4', 'X5'], writes=[kkey])
                if cut < 3:
                    return
                for g in range(2):
                    def ftr(e, g=g):
                        last = None
                        for q in range(4):
                            t = g * 4 + q
                            last = e.transpose(pTk[:, q, :], kend32[:, t * P:(t + 1) * P], self.ident[:])
                        return last
                    S.op('pe', ftr, reads=[kkey], writes=['pTk'])
                    for hf in range(2):
                        S.op('act', lambda e, g=g, hf=hf: e.activation(kend_tm[:, hf, g * 4:(g + 1) * 4, :], pTk[:], AF.Copy, scale=halfm[:, hf:hf + 1]),
                             reads=['pTk', 'halfm'], writes=[('kend_tm', g, hf)])
                if cut < 4:
                    return
                tile0 = 0 if own else NT_OWN
                order = list(range(NCH)) if di == 0 else list(range(NCH - 1, -1, -1))
                started = not first_state
                for gi in range(4):
                    cs = order[gi * 4:(gi + 1) * 4]

                    def fu(e, cs=cs):
                        last = None
                        for q, c in enumerate(cs):
                            t, hf = c // 2, c % 2
                            last = e.matmul(pU[:, q, :], kend_tm[:, hf, t, :], v_tm[:, tile0 + t, :], start=True, stop=True)
                        return last
                    S.op('pe', fu, reads=[('kend_tm', g, hf) for g in range(2) for hf in range(2)] + [('v_tm', g) for g in range(4)], writes=['pU'])
                    for q, c in enumerate(cs):
                        if own:
                            if started:
                                S.op('act', lambda e, c=c, cu=scur[0]: e.activation(Sbf[di][:, c, :], S32[:, cu, :], AF.Copy), reads=[('S32', scur[0])], writes=[('Sbf', di, c)])
                            else:
                                S.op('dve', lambda e, c=c: e.memset(Sbf[di][:, c, :], 0.0), writes=[('Sbf', di, c)])
                        if started:
                            cu = scur[0]
                            S.op('dve', lambda e, q=q, c=c, cu=cu: e.scalar_tensor_tensor(S32[:, 1 - cu, :], S32[:, cu, :], etot[:, c:c + 1], pU[:, q, :], ALU.mult, ALU.add),
                                 reads=[('S32', cu), 'pU', 'etot'], writes=[('S32', 1 - cu)])
                            scur[0] = 1 - cu
                        else:
                            S.op('dve', lambda e, q=q, cu=scur[0]: e.tensor_copy(S32[:, cu, :], pU[:, q, :]), reads=['pU'], writes=[('S32', scur[0])])
                            started = True

            for hh in range(8):
                hs = hh % 2
                S.dma('sp', hnw[hs][:], self.hnw[:, hh * P:(hh + 1) * P].partition_broadcast(P), writes=[('hnw', hs)])
                wsA = load_w(hh, 0)
                proj_fm(wsA, 0, 0, N, AF.Silu, qr, 'qr')
                proj_fm(wsA, P, 0, N, AF.Sigmoid, sgA, 'sgA')
                wsB = load_w(hh, 1)
                proj_tm(wsB, P, NT_ALL, AF.Copy, v_tm, 'v_tm')
                proj_fm(wsB, 0, N, N, AF.Sigmoid, sgB, 'sgB')
                gate_pass(hh, 0, sgA, [('sgA', 0), ('sgA', 1)], True, True)
                gate_pass(hh, 1, sgB, [('sgB', 0), ('sgB', 1)], False, True)
                proj_fm(wsB, 0, 0, N, AF.Sigmoid, sgB, 'sgB')
                wsG = load_w(hh, 2)
                for g in range(4):
                    def fg_(e, g=g, wsG=wsG):
                        last = None
                        for q in range(4):
                            c = g * 4 + q
                            last = self.mm_group(e, pV[0:64, q, :], [(hT[:, kt, c * 64:(c + 1) * 64], wb[wsG][:, kt, 0:P]) for kt in range(KT)])
                        return last
                    S.op('pe', fg_, reads=[('whg', wsG)], writes=['pV'])
                    S.op('act', lambda e, g=g: e.activation(sg_tm[:, g * 4:(g + 1) * 4, :], pV[0:64, :, :], AF.Silu), reads=['pV'], writes=[('sg_tm', g)])
                gate_pass(hh, 1, sgB, [('sgB', 0), ('sgB', 1)], True, False)
                def out_tile(t, pb, hh=hh, hs=hs):
                    tsl = slice(t * P, (t + 1) * P)
                    scm, oh, oh2, junk, sc = scm_[pb], oh_[pb], oh2_[pb], junk_[pb], sc_[pb]

                    def fsc(e, tsl=tsl):
                        e.matmul(pSc[:, 0, :], kdec[0][:, tsl], qdec[0][:, tsl], start=True, stop=True)
                        return e.matmul(pSc[:, 1, :], kdec[1][:, tsl], qdec[1][:, tsl], start=True, stop=True)
                    S.op('pe', fsc, reads=[('kdec', 0), ('kdec', 1), ('qdec', 0), ('qdec', 1)], writes=['pSc'])
                    for di in range(2):
                        S.op('dve', lambda e, di=di: e.tensor_tensor(scm[di][:], pSc[:, di, :], msk[:, di, :], ALU.mult),
                             reads=['pSc', 'msk'], writes=[('scm', pb, di)])

                    def fo(e, t=t, tsl=tsl):
                        last = None
                        for hf in range(2):
                            c = 2 * t + hf
                            csl = slice(hf * 64, (hf + 1) * 64)
                            tk = slice(t * P + hf * 64, t * P + (hf + 1) * 64)
                            e.matmul(pOh[:, hf, :], scm[0][:, csl], v_tm[:, t, :], start=True, stop=False)
                            e.matmul(pOh[:, hf, :], scm[1][:, csl], v_tm[:, t, :], start=False, stop=False)
                            e.matmul(pOh[:, hf, :], qdec[0][:, tk], Sbf[0][:, c, :], start=False, stop=False)
                            last = e.matmul(pOh[:, hf, :], qdec[1][:, tk], Sbf[1][:, c, :], start=False, stop=True)
                        return last
                    S.op('pe', fo, reads=[('scm', pb, 0), ('scm', pb, 1), ('qdec', 0), ('qdec', 1)] + [('Sbf', di, c) for di in range(2) for c in (2 * t, 2 * t + 1)]
                         + [('v_tm', g) for g in range(4)], writes=['pOh'])
                    S.op('act', lambda e: e.activation(oh[:], pOh[:], AF.Copy), reads=['pOh'], writes=[('oh', pb)])
                    for hf in range(2):
                        S.op('act', lambda e, hf=hf: e.activation(junk[:], oh[:, hf, :], AF.Square, accum_out=sc[:, hf:hf + 1]), reads=[('oh', pb)], writes=[('hss', pb, hf), ('hjunk', pb)])
                    S.op('act', lambda e: e.activation(sc[:, 2:4], sc[:, 0:2], AF.Sqrt, bias=self.eps_t[0:64, 0:1], scale=1.0 / 128), reads=[('hss', pb, 0), ('hss', pb, 1)], writes=[('hsd', pb)])
                    S.op('dve', lambda e: e.reciprocal(sc[:, 2:4], sc[:, 2:4]), reads=[('hsd', pb)], writes=[('hrs', pb)])
                    for hf in range(2):
                        S.op('dve', lambda e, hs=hs, hf=hf: e.scalar_tensor_tensor(oh2[:, hf, :], oh[:, hf, :], sc[:, 2 + hf:3 + hf], hnw[hs][0:64, :], ALU.mult, ALU.mult),
                             reads=[('oh', pb), ('hrs', pb), ('hnw', hs)], writes=[('oh2', pb, hf)])
                    S.op('dve', lambda e, t=t: e.tensor_tensor(oh2[:], oh2[:], sg_tm[:, 2 * t:2 * t + 2, :], ALU.mult),
                         reads=[('oh2', pb, 0), ('oh2', pb, 1)] + [('sg_tm', g) for g in range(4)], writes=[('oh2', pb, 0), ('oh2', pb, 1)])
                    if self.debug:
                        for hf in range(2):
                            rs_ = slice(t * P + hf * 64, t * P + (hf + 1) * 64)
                            S.dma('sp', self.dbg_os[rs_, hh * P:(hh + 1) * P], oh[:, hf, :], reads=[('oh', pb)], writes=[('dbgos', hh, t, hf)])
                            S.dma('sp', self.dbg_or[rs_, hh * P:(hh + 1) * P], oh2[:, hf, :], reads=[('oh2', pb, hf)], writes=[('dbgor', hh, t, hf)])
                            dk.extend([('dbgos', hh, t, hf), ('dbgor', hh, t, hf)])

                    def ftx(e):
                        e.transpose(pX[:, 0, :], oh2[:, 0, :], self.ident[0:64, 0:64])
                        return e.transpose(pX[:, 1, :], oh2[:, 1, :], self.ident[0:64, 0:64])
                    S.op('pe', ftx, reads=[('oh2', pb, 0), ('oh2', pb, 1)], writes=['pXh'])
                    S.op('act', lambda e, hh=hh, tsl=tsl: e.activation(self.mixedT[:, 8 + hh, tsl], pX[:].rearrange("p a b -> p (a b)"), AF.Copy),
                         reads=['pXh'], writes=[('mixedT', 8 + hh, tsl.start)])
                for t in range(NT_OWN if cut >= 5 else 0):
                    out_tile(t, t % 2)
            S.emit(final_wait_keys=dk)

    def make_carep(self, S, st):
        nc = self.nc
        u = Sched._uid
        ccol = st.enter_context(nc.sbuf_tensor(f"ccol{u}", [P, KT], F32))
        cact = st.enter_context(nc.sbuf_tensor(f"cact{u}", [P, KT], F32))
        cabf = st.enter_context(nc.sbuf_tensor(f"cabf{u}", [P, KT], BF16))
        carep = st.enter_context(nc.sbuf_tensor(f"carep{u}", [P, KT, P], BF16))
        S.dma('sp', ccol[:], self.c_col, writes=['ccol'])
        S.op('act', lambda e: e.activation(cact[:], ccol[:], AF.Silu), reads=['ccol'], writes=['cact'])
        S.op('dve', lambda e: e.tensor_copy(carep[:], cact[:].unsqueeze(2).to_broadcast([P, KT, P])), reads=['cact'], writes=['carep'])
        S.op('dve', lambda e: e.tensor_copy(cabf[:], cact[:]), reads=['cact'], writes=['cabf'])
        return carep, cabf

    def phase_C0(self):
        nc = self.nc
        with contextlib.ExitStack() as st:
            S = self.S
            carep, cabf = self.make_carep(S, st)
            self.ada_part(S, st, self.w_ada2, self.b_ada2, 3 * D, self.mod2, carep, 2 * D)
            if self.sparse:
                self.ada_part(S, st, self.w_ada3, self.b_ada3, D, self.g2rep, carep, 10 ** 9, tag='g2', reuse=True)
                S.emit()
                return
            wg = [st.enter_context(nc.sbuf_tensor(f"wg2_{i}", [P, KT, 512], BF16)) for i in range(2)]
            bg = st.enter_context(nc.sbuf_tensor("bg2", [P, KT], F32))
            pg = st.enter_context(nc.psum_tensor("pg2", [P, KT], F32))
            S.dma('sp', bg[:], self.b_g2col, writes=['bg2'])
            for c in range(4):
                s = c % 2
                S.dma('pool', wg[s][:], self.w_ada3[:, c * 512:(c + 1) * 512].rearrange("(kt p) n -> p kt n", p=P), writes=[('wg2', s)])

                def f(e, s=s, c=c):
                    last = None
                    for q in range(4):
                        dt = c * 4 + q
                        last = self.mm_group(e, pg[:, dt:dt + 1], [(wg[s][:, kt, q * P:(q + 1) * P], cabf[:, kt:kt + 1]) for kt in range(KT)])
                    return last
                S.op('pe', f, reads=[('wg2', s), 'cabf'], writes=['pg2'])
            S.op('dve', lambda e: e.tensor_tensor(self.g2col[:], pg[:], bg[:], ALU.add), reads=['pg2', 'bg2'], writes=['g2col'])
            S.emit()

    def phase_C1(self):
        nc = self.nc
        with contextlib.ExitStack() as st:
            S = self.S
            wb = [st.enter_context(nc.sbuf_tensor(f"wo{i}", [P, KT, CW], BF16)) for i in range(3)]
            py = [st.enter_context(nc.psum_tensor(f"py{i}", [P, CW], F32)) for i in range(2)]
            it = 0
            for dc in range(8):
                ws = dc % 3
                S.dma('pool', wb[ws][:], self.w_out_t[dc], writes=[('wo', ws)], max_dma_last_dim=4096)
                for t in range(NT_OWN):
                    s = it % 2
                    it += 1
                    S.op('pe', lambda e, s=s, t=t, ws=ws: self.mm_group(
                        e, py[s][:], [(self.mixedT[:, mt, t * P:(t + 1) * P], wb[ws][:, mt, :]) for mt in range(16)]),
                        reads=[('wo', ws)], writes=[('py', s)])
                    S.op('act', lambda e, s=s, t=t, dc=dc: e.activation(self.acc[:, t, dc * CW:(dc + 1) * CW], py[s][:], AF.Copy),
                         reads=[('py', s)], writes=[('acc', t, dc)])
            S.emit()

    def phase_C2(self):
        nc = self.nc
        ALPHA = float(2.0 ** 0.25)
        with contextlib.ExitStack() as st:
            sb = lambda n, s, d=F32: st.enter_context(nc.sbuf_tensor(n, s, d))
            S = self.S
            mod2 = self.mod2
            g1r, sh2r, sc2r = mod2[:, 0:D], mod2[:, D:2 * D], mod2[:, 2 * D:3 * D]
            ln1 = sb("ln1r", [P, 2, D])
            S.dma('sp', ln1[:, 0, :], self.ln1[0:1, :].partition_broadcast(P), writes=['ln1g'])
            S.dma('sp', ln1[:, 1, :], self.ln1[1:2, :].partition_broadcast(P), writes=['ln1b'])
            wr = sb("wr", [P, KT, 32]); br = sb("br", [P, 32])
            S.dma('sp', wr[:], self.w_r, writes=['wr'])
            S.dma('sp', br[:], self.b_r.partition_broadcast(P), writes=['br'])
            xin = sb("xin_c", [P, D]); tt = sb("tt_c", [P, D]); h2 = sb("h2_c", [P, D])
            h2T32 = xin[:].rearrange("p (k j) -> p k j", k=KT)
            stt = sb("stt_c", [P, 4, 6]); mv = sb("mv_c", [P, 6]); mv2 = sb("mv2_c", [P, 6])
            lg = sb("lg", [P, 32]); top8 = sb("top8", [P, 8]); mask = sb("mask", [P, 32]); ex = sb("ex", [P, 32])
            gt = sb("gt", [P, 32]); sm = sb("sm", [P, 4])
            pT = [st.enter_context(nc.psum_tensor(f"pTc{i}", [P, 8, P], F32)) for i in range(3)]
            pL = st.enter_context(nc.psum_tensor("pL", [P, 32], F32))
            pG = st.enter_context(nc.psum_tensor("pG", [32, P], F32))
            npt = [0]
            dk = []

            def transposes(src, srckey, evac):
                for hf in range(2):
                    pi = npt[0] % 3
                    npt[0] += 1

                    def tr(e, hf=hf, pi=pi):
                        last = None
                        for k8 in range(8):
                            kt = hf * 8 + k8
                            last = e.transpose(pT[pi][:, k8, :], src[:, kt * P:(kt + 1) * P], self.ident[:])
                        return last
                    S.op('pe', tr, reads=[srckey], writes=[('pTc', pi)])
                    evac(hf, pi)

            import os
            ccut = int(os.environ.get("C_CUT", "9"))
            for t in range(NT_OWN):
                tsl = slice(t * P, (t + 1) * P)
                S.dma('sp', xin[:], self.x[tsl, :], writes=['xin'])
                S.op('dve', lambda e, t=t: e.tensor_tensor(tt[:], self.acc[:, t, :], g1r, ALU.mult), reads=[('acc', t)], writes=['tt'])
                S.op('dve', lambda e: e.scalar_tensor_tensor(tt[:], xin[:], ALPHA, tt[:], ALU.mult, ALU.add), reads=['tt', 'xin'], writes=['tt'])
                self.ln_stats(S, stt, mv, tt, 'tt', 'ln1')
                S.op('act', lambda e: e.activation(tt[:], tt[:], AF.Identity, bias=mv[:, 3:4], scale=mv[:, 2:3]),
                     reads=['tt', ('ln1', 'nb'), ('ln1', 'rs')], writes=['tt'])
                S.op('dve', lambda e: e.tensor_tensor(tt[:], tt[:], ln1[:, 0, :], ALU.mult), reads=['tt', 'ln1g'], writes=['tt'])
                S.op('dve', lambda e: e.tensor_tensor(tt[:], tt[:], ln1[:, 1, :], ALU.add), reads=['tt', 'ln1b'], writes=['tt'])
                if self.debug:
                    S.dma('sp', self.dbg_x1[tsl, :], tt[:], reads=['tt'], writes=[('dbgx1', t)]); dk.append(('dbgx1', t))
                if ccut < 5:
                    continue
                self.ln_stats(S, stt, mv2, tt, 'tt', 'ln2h')
                S.op('act', lambda e: e.activation(h2[:], tt[:], AF.Identity, bias=mv2[:, 3:4], scale=mv2[:, 2:3]),
                     reads=['tt', ('ln2h', 'nb'), ('ln2h', 'rs')], writes=['h2'])
                S.op('dve', lambda e: e.tensor_tensor(h2[:], h2[:], sc2r, ALU.mult), reads=['h2'], writes=['h2'])
                S.op('dve', lambda e: e.tensor_tensor(h2[:], h2[:], sh2r, ALU.add), reads=['h2'], writes=['h2'])
                if self.debug:
                    S.dma('sp', self.dbg_h2[tsl, :], h2[:], reads=['h2'], writes=[('dbgh2', t)]); dk.append(('dbgh2', t))

                if self.sparse:
                    S.op('act', lambda e, t=t: e.activation(self.h2tm[:, t, :], h2[:], AF.Copy), reads=['h2'], writes=[('h2tm', t)])

                def evac_h2(hf, pi, t=t, tsl=tsl):
                    if not self.sparse:
                        S.op('act', lambda e: e.activation(self.h2T[:, hf * 8:(hf + 1) * 8, tsl], pT[pi][:], AF.Copy),
                             reads=[('pTc', pi)], writes=[('h2T', t, hf)])
                    S.op('act', lambda e: e.activation(h2T32[:, hf * 8:(hf + 1) * 8, :], pT[pi][:], AF.Copy),
                         reads=[('pTc', pi), 'xin'], writes=[('h2T32', hf), 'xin'])
                if ccut < 6:
                    continue
                transposes(h2, 'h2', evac_h2)
                if ccut < 7:
                    continue
                S.op('pe', lambda e: self.mm_group(e, pL[:], [(h2T32[:, kt, :], wr[:, kt, :]) for kt in range(KT)]),
                     reads=[('h2T32', 0), ('h2T32', 1), 'wr', 'xin'], writes=['pL'])
                S.op('dve', lambda e: e.tensor_tensor(lg[:], pL[:], br[:], ALU.add), reads=['pL', 'br'], writes=['lg'])
                if self.debug:
                    S.dma('sp', self.dbg_lg[tsl, :], lg[:], reads=['lg'], writes=[('dbglg', t)]); dk.append(('dbglg', t))
                if ccut < 8:
                    continue
                S.op('dve', lambda e: e.max(top8[:], lg[:]), reads=['lg'], writes=['top8'])
                S.op('dve', lambda e: e.tensor_scalar(mask[:], lg[:], top8[:, 3:4], None, ALU.is_ge), reads=['lg', 'top8'], writes=['mask'])
                if self.sparse:
                    S.op('dve', lambda e, t=t: e.tensor_copy(self.Mtm[:, t, :], mask[:]), reads=['mask'], writes=[('Mtm', t)])
                    S.op('dve', lambda e, t=t: e.tensor_copy(self.Mf[:, t, :], mask[:]), reads=['mask'], writes=[('Mf', t)])
                S.op('dve', lambda e: e.tensor_scalar(sm[:, 0:1], top8[:, 0:1], -1.0, None, ALU.mult), reads=['top8'], writes=['negm'])
                S.op('act', lambda e: e.activation(ex[:], lg[:], AF.Exp, bias=sm[:, 0:1], scale=1.0), reads=['lg', 'negm'], writes=['ex'])
                S.op('dve', lambda e: e.tensor_tensor(ex[:], ex[:], mask[:], ALU.mult), reads=['ex', 'mask'], writes=['ex'])
                S.op('dve', lambda e: e.tensor_reduce(sm[:, 1:2], ex[:], AX.X, ALU.add), reads=['ex'], writes=['den'])
                S.op('dve', lambda e: e.reciprocal(sm[:, 2:3], sm[:, 1:2]), reads=['den'], writes=['rden'])
                S.op('dve', lambda e: e.tensor_scalar(gt[:], ex[:], sm[:, 2:3], None, ALU.mult), reads=['ex', 'rden'], writes=['gt'])
                S.op('pe', lambda e: e.transpose(pG[:], gt[:], self.ident[:]), reads=['gt'], writes=['pG'])
                S.op('act', lambda e, tsl=tsl: e.activation(self.gatesT[:, tsl], pG[:], AF.Copy), reads=['pG'], writes=[('gatesT', t)])
                if self.sparse:
                    S.op('dve', lambda e, t=t: e.tensor_scalar(self.acc[:, t, :], tt[:], ALPHA, None, ALU.mult), reads=['tt'], writes=[('acc', t)])
                    continue
                S.op('dve', lambda e: e.tensor_scalar(tt[:], tt[:], ALPHA, None, ALU.mult), reads=['tt'], writes=['tt'])

                def evac_acc(hf, pi, t=t):
                    S.op('act', lambda e: e.activation(self.acc[:, t, hf * 1024:(hf + 1) * 1024].rearrange("p (k j) -> p k j", k=8), pT[pi][:], AF.Copy),
                         reads=[('pTc', pi)], writes=[('acc', t)])
                transposes(tt, 'tt', evac_acc)
            if self.debug:
                S.dma('sp', self.dbg_gt, self.gatesT[:], reads=[('gatesT', t) for t in range(NT_OWN)], writes=['dbggt']); dk.append('dbggt')
                for t in range(NT_OWN):
                    S.dma('sp', self.dbg_acc[:, t * D:(t + 1) * D], self.acc[:, t, :], reads=[('acc', t)], writes=[('dbgacc', t)]); dk.append(('dbgacc', t))
            S.emit(final_wait_keys=dk)

    def phase_MoE(self):
        nc = self.nc
        with contextlib.ExitStack() as st:
            sb = lambda n, s, d=F32: st.enter_context(nc.sbuf_tensor(n, s, d))
            ps = lambda n, s: st.enter_context(nc.psum_tensor(n, s, F32))
            S = self.S
            h2T, acc, gatesT, g2col = self.h2T, self.acc, self.gatesT, self.g2col
            wb = [sb(f"wmoe{i}", [P, KT, CW], BF16) for i in range(3)]
            actT = sb("actT", [P, 8, S_OWN], BF16)
            glu = [sb(f"glu{i}", [P, 512]) for i in range(2)]
            sig = [sb(f"sig{i}", [P, 512]) for i in range(2)]
            lin1 = [sb(f"lin1{i}", [P, 512]) for i in range(2)]
            tg = [sb(f"tg{i}", [P, 512]) for i in range(2)]
            grep = sb("grep", [P, S_OWN]); gsel = sb("gsel", [32, S_OWN])
            b1 = sb("b1s", [P, 32, 32]); b2s = sb("b2s", [32, D]); ones32 = sb("ones32", [32, P])
            pGL = [ps(f"pGL{i}", [P, 2, 512]) for i in range(2)]
            pY = [ps(f"pY{i}", [P, 512]) for i in range(2)]
            pGR = ps("pGR", [P, 512])
            S.dma('sp', b1[:], self.b1_r, writes=['b1'])
            S.dma('sp', b2s[:], self.b2, writes=['b2s'])
            S.op('dve', lambda e: e.memset(ones32[:], 1.0), writes=['ones32'])
            S.op('dve', lambda e: e.tensor_scalar(b1[:, :, 16:32], b1[:, :, 16:32], 1.0, None, ALU.add), reads=['b1'], writes=['b1'])
            nwb = [0]; ngl = [0]; ny = [0]

            def acc_add(s, dt, th):
                accv = acc[:, 4 * th:4 * th + 4, dt * P:(dt + 1) * P]
                S.op('dve', lambda e: e.scalar_tensor_tensor(accv, pY[s][:].rearrange("p (a b) -> p a b", a=4), g2col[:, dt:dt + 1], accv, ALU.mult, ALU.add),
                     reads=[('pY', s), 'g2col'], writes=[('accT', dt, th)])

            for dt in range(KT):
                for th in range(2):
                    s = ny[0] % 2
                    ny[0] += 1
                    S.op('pe', lambda e, s=s, dt=dt, th=th: e.matmul(pY[s][:], b2s[:, dt * P:(dt + 1) * P], gatesT[:, th * 512:(th + 1) * 512], start=True, stop=True),
                         reads=['b2s'], writes=[('pY', s)])
                    acc_add(s, dt, th)

            for ex in range(self.NE):
                S.op('dve', lambda e, ex=ex: e.tensor_scalar(gsel[:], gatesT[:], self.ident[0:32, ex:ex + 1], None, ALU.mult), reads=[], writes=['gsel'])
                for th in range(2):
                    S.op('pe', lambda e, th=th: e.matmul(pGR[:], ones32[:], gsel[:, th * 512:(th + 1) * 512], start=True, stop=True),
                         reads=['gsel', 'ones32'], writes=['pGR'])
                    S.op('dve', lambda e, th=th: e.tensor_copy(grep[:, th * 512:(th + 1) * 512], pGR[:]), reads=['pGR'], writes=[('grep', th)])
                for g in range(2):
                    for c8 in range(8):
                        c = g * 8 + c8
                        ws = nwb[0] % 3
                        nwb[0] += 1
                        S.dma('pool', wb[ws][:], self.w1_r[ex, c], writes=[('wmoe', ws)], max_dma_last_dim=4096)
                        for th in range(2):
                            s = ngl[0] % 2
                            ngl[0] += 1
                            tks = slice(th * 512, (th + 1) * 512)

                            def fgl(e, s=s, ws=ws, tks=tks):
                                self.mm_group(e, pGL[s][:, 0, :], [(wb[ws][:, kt, 0:P], h2T[:, kt, tks]) for kt in range(KT)])
                                return self.mm_group(e, pGL[s][:, 1, :], [(wb[ws][:, kt, P:2 * P], h2T[:, kt, tks]) for kt in range(KT)])
                            S.op('pe', fgl, reads=[('wmoe', ws)], writes=[('pGL', s)])
                            S.op('dve', lambda e, s=s, ex=ex, c=c: e.tensor_scalar(glu[s][:], pGL[s][:, 0, :], b1[:, ex, c:c + 1], 7.0, ALU.add, ALU.min),
                                 reads=[('pGL', s), 'b1'], writes=[('glu', s)])
                            S.op('act', lambda e, s=s: e.activation(sig[s][:], glu[s][:], AF.Sigmoid, scale=1.702), reads=[('glu', s)], writes=[('sig', s)])
                            S.op('dve', lambda e, s=s, ex=ex, c=c: e.tensor_scalar(lin1[s][:], pGL[s][:, 1, :], b1[:, ex, 16 + c:17 + c], 8.0, ALU.add, ALU.min),
                                 reads=[('pGL', s), 'b1'], writes=[('lin1', s)])
                            S.op('dve', lambda e, s=s: e.tensor_tensor(tg[s][:], glu[s][:], sig[s][:], ALU.mult), reads=[('glu', s), ('sig', s)], writes=[('tg', s)])
                            S.op('dve', lambda e, s=s: e.scalar_tensor_tensor(tg[s][:], lin1[s][:], -6.0, tg[s][:], ALU.max, ALU.mult),
                                 reads=[('lin1', s), ('tg', s)], writes=[('tg', s)])
                            S.op('dve', lambda e, s=s, c8=c8, tks=tks, th=th: e.tensor_tensor(actT[:, c8, tks], tg[s][:], grep[:, tks], ALU.mult),
                                 reads=[('tg', s), ('grep', th)], writes=[('actT', c8, th)])
                    for dc in range(4):
                        ws = nwb[0] % 3
                        nwb[0] += 1
                        wv = wb[ws][:].rearrange("p a b -> p (a b)").rearrange("p (f d) -> p f d", f=8)
                        S.dma('pool', wv, self.w2_r[ex, g, dc], writes=[('wmoe', ws)], max_dma_last_dim=4096)
                        for dsub in range(4):
                            dt = dc * 4 + dsub
                            for th in range(2):
                                s = ny[0] % 2
                                ny[0] += 1
                                S.op('pe', lambda e, s=s, wv=wv, dsub=dsub, th=th: self.mm_group(
                                    e, pY[s][:], [(wv[:, ft, dsub * P:(dsub + 1) * P], actT[:, ft, th * 512:(th + 1) * 512]) for ft in range(8)]),
                                    reads=[('wmoe', ws)] + [('actT', ft, th) for ft in range(8)], writes=[('pY', s)])
                                acc_add(s, dt, th)
            if self.debug:
                dk = []
                for t in range(NT_OWN):
                    S.dma('sp', self.dbg_acc2[:, t * D:(t + 1) * D], acc[:, t, :], reads=[('accT', dt, th) for dt in range(KT) for th in range(2)], writes=[('dbgacc2', t)])
                    dk.append(('dbgacc2', t))
                S.emit(final_wait_keys=dk)
            else:
                S.emit()

    def phase_MoE_sp(self):
        nc = self.nc
        I32 = mybir.dt.int32
        QS, NQ = 256, 4
        with contextlib.ExitStack() as st:
            sb = lambda n, s, d=F32: st.enter_context(nc.sbuf_tensor(n, s, d))
            ps = lambda n, s: st.enter_context(nc.psum_tensor(n, s, F32))
            S = self.S
            acc, h2tm, gatesT, g2rep, Mtm, Mf = self.acc, self.h2tm, self.gatesT, self.g2rep, self.Mtm, self.Mf
            utf = sb("utri_f", [P, P]); utri = sb("utri_s", [P, P], BF16); ones128 = sb("ones128", [P, P], BF16)
            ones32 = sb("ones32s", [32, P]); iota = sb("iota_s", [P, S_OWN]); slotidx = sb("slotidx_s", [P, 8])
            b1 = sb("b1s", [P, 32, 32]); b2s = sb("b2s", [32, D])
            posm = sb("posm", [P, NT_OWN, 32]); posmT = sb("posmT", [32, S_OWN]); cnt = sb("cnt_i", [P, 32], I32)
            gsel = sb("gsel", [32, S_OWN]); grep = sb("grep", [P, S_OWN]); posrep = sb("posrep", [P, S_OWN])
            wb = [sb(f"wsp{i}", [P, KT, CW], BF16) for i in range(3)]
            Sq = sb("Sq", [P, NT_OWN, QS], BF16); STq = sb("STq", [P, 2, S_OWN], BF16)
            xg = sb("xg", [P, KT, QS], BF16); actq = sb("actq", [P, KT, QS], BF16)
            glu = [sb(f"sglu{i}", [P, QS]) for i in range(2)]; sig = [sb(f"ssig{i}", [P, QS]) for i in range(2)]
            lin1 = [sb(f"slin{i}", [P, QS]) for i in range(2)]; tg = [sb(f"stg{i}", [P, QS]) for i in range(2)]
            ysb = [[sb(f"ysb{i}_{j}", [P, QS], BF16) for j in range(2)] for i in range(2)]
            tmpb = [sb(f"tmpb{i}", [P, 512]) for i in range(2)]
            pGR = ps("pGRs", [P, 512]); pmd_t = ps("pmdt", [P, 512])
            pmd = pmd_t[:, 0:32]; pmt = pmd_t[0:32, 256:384]
            py_t = ps("pys_t", [P, 2, QS]); po_t = ps("pos_t", [P, 2, QS])
            pg = [ps(f"pgs{i}", [P, 2, QS]) for i in range(2)]; pGL = [ps(f"pGLs{i}", [P, 2, QS]) for i in range(2)]
            po = [po_t[:, 0, :], pmd_t[:, 0:QS]]
            S.dma('sp', utf[:], self.utri_d, writes=['utf'])
            S.op('dve', lambda e: e.tensor_copy(utri[:], utf[:]), reads=['utf'], writes=['utri'])
            S.op('dve', lambda e: e.memset(ones128[:], 1.0), writes=['ones128'])
            S.op('dve', lambda e: e.memset(ones32[:], 1.0), writes=['ones32'])
            S.dma('sp', iota[:], self.iota_d.partition_broadcast(P), writes=['iota'])
            S.dma('sp', slotidx[:], self.slotidx_d, writes=['slotidx'])
            S.dma('sp', b1[:], self.b1_r, writes=['b1'])
            S.dma('sp', b2s[:], self.b2, writes=['b2s'])
            S.op('dve', lambda e: e.tensor_scalar(b1[:, :, 16:32], b1[:, :, 16:32], 1.0, None, ALU.add), reads=['b1'], writes=['b1'])
            for t in range(NT_OWN):
                tsl = slice(t * P, (t + 1) * P)

                def fpos(e, t=t):
                    last = e.matmul(pmd, utri[:], Mtm[:, t, :], start=True, stop=(t == 0))
                    for t2 in range(t):
                        last = e.matmul(pmd, ones128[:], Mtm[:, t2, :], start=False, stop=(t2 == t - 1))
                    return last
                S.op('pe', fpos, reads=['utri', 'ones128'] + [('Mtm', x) for x in range(t + 1)], writes=['pmd'])
                S.op('dve', lambda e, t=t: e.scalar_tensor_tensor(posm[:, t, :], pmd, 1.0, Mf[:, t, :], ALU.add, ALU.mult),
                     reads=['pmd', ('Mf', t)], writes=[('posm', t)])
                S.op('dve', lambda e, t=t: e.tensor_scalar(posm[:, t, :], posm[:, t, :], -1.0, None, ALU.add), reads=[('posm', t)], writes=[('posm', t)])
                S.op('pe', lambda e, t=t: e.transpose(pmt, posm[:, t, :], self.ident[:]), reads=[('posm', t)], writes=['pmd'])
                S.op('act', lambda e, tsl=tsl: e.activation(posmT[:, tsl], pmt, AF.Copy), reads=['pmd'], writes=[('posmT', tsl.start)])
            S.op('pe', lambda e: self.mm_group(e, pmd, [(ones128[:], Mtm[:, t, :]) for t in range(NT_OWN)]),
                 reads=['ones128'] + [('Mtm', x) for x in range(NT_OWN)], writes=['pmd'])
            S.op('dve', lambda e: e.tensor_copy(cnt[:], pmd), reads=['pmd'], writes=['cnt'])
            posmT_keys = [('posmT', t * P) for t in range(NT_OWN)]
            spcut = int(os.environ.get("SP_CUT", "9"))
            nb = 0
            for t in range(NT_OWN if spcut >= 2 else 0):
                tsl = slice(t * P, (t + 1) * P)
                for dc in range(4):
                    s = nb % 2
                    nb += 1
                    dsl = slice(dc * 512, (dc + 1) * 512)
                    S.op('pe', lambda e, tsl=tsl, dsl=dsl: e.matmul(pGR[:], gatesT[:, tsl], b2s[:, dsl], start=True, stop=True), reads=['b2s'], writes=['pGR'])
                    S.op('dve', lambda e, s=s, dsl=dsl: e.tensor_tensor(tmpb[s][:], pGR[:], g2rep[:, dsl], ALU.mult), reads=['pGR'], writes=[('tmpb', s)])
                    S.op('dve', lambda e, s=s, t=t, dsl=dsl: e.tensor_tensor(acc[:, t, dsl], acc[:, t, dsl], tmpb[s][:], ALU.add),
                         reads=[('tmpb', s)], writes=[('accs', t, dc // 2)])
            cnt_w = [0, 0, 0, 0, 0]

            def unit(ex, q):
                S.cond_begin(cnt[0:1, ex:ex + 1], q * int(os.environ.get('THR_STEP', QS)), 'cnt', ('cnt', ex), dma_uncond=(q == 0))
                for t in range(NT_OWN):
                    S.op('dve', lambda e, t=t: e.tensor_scalar(Sq[:, t, :], iota[:, q * QS:(q + 1) * QS], posm[:, t, ex:ex + 1], None, ALU.is_equal),
                         reads=['iota', ('posm', t)], writes=[('Sq', t)])
                for sh in range(2 if spcut >= 4 else 0):
                    j = 2 * q + sh
                    S.op('dve', lambda e, sh=sh, j=j: e.scalar_tensor_tensor(STq[:, sh, :], posrep[:], slotidx[:, j:j + 1], grep[:], ALU.is_equal, ALU.mult),
                         reads=['posrep', 'grep', 'slotidx'], writes=[('STq', sh)])
                for kp in range(8 if spcut >= 5 else 0):
                    s = cnt_w[2] % 2
                    cnt_w[2] += 1

                    def fgat(e, s=s, kp=kp):
                        last = None
                        for j in range(2):
                            kt = 2 * kp + j
                            last = self.mm_group(e, pg[s][:, j, :], [(h2tm[:, t, kt * P:(kt + 1) * P], Sq[:, t, :]) for t in range(NT_OWN)])
                        return last
                    S.op('pe', fgat, reads=[('Sq', t) for t in range(NT_OWN)], writes=[('pg', s)])
                    if os.environ.get("NO_GEVAC") is None:
                        S.op('act', lambda e, s=s, kp=kp: e.activation(xg[:, 2 * kp:2 * kp + 2, :], pg[s][:], AF.Copy), reads=[('pg', s)], writes=[('xg', kp)])
                xgk = [('xg', kp) for kp in range(8)]
                for c in range(16 if spcut >= 6 else 0):
                    ws = cnt_w[0] % 3
                    cnt_w[0] += 1
                    S.dma('pool', wb[ws][:], self.w1_r[ex, c], writes=[('wsp', ws)], max_dma_last_dim=4096)
                    s = cnt_w[1] % 2
                    cnt_w[1] += 1

                    def fgl(e, s=s, ws=ws):
                        self.mm_group(e, pGL[s][:, 0, :], [(wb[ws][:, kt, 0:P], xg[:, kt, :]) for kt in range(KT)])
                        return self.mm_group(e, pGL[s][:, 1, :], [(wb[ws][:, kt, P:2 * P], xg[:, kt, :]) for kt in range(KT)])
                    S.op('pe', fgl, reads=[('wsp', ws)] + xgk, writes=[('pGLs', s)])
                    S.op('dve', lambda e, s=s, c=c: e.tensor_scalar(glu[s][:], pGL[s][:, 0, :], b1[:, ex, c:c + 1], 7.0, ALU.add, ALU.min),
                         reads=[('pGLs', s), 'b1'], writes=[('sglu', s)])
                    S.op('act', lambda e, s=s: e.activation(sig[s][:], glu[s][:], AF.Sigmoid, scale=1.702), reads=[('sglu', s)], writes=[('ssig', s)])
                    S.op('dve', lambda e, s=s, c=c: e.tensor_scalar(lin1[s][:], pGL[s][:, 1, :], b1[:, ex, 16 + c:17 + c], 8.0, ALU.add, ALU.min),
                         reads=[('pGLs', s), 'b1'], writes=[('slin', s)])
                    S.op('dve', lambda e, s=s: e.tensor_tensor(tg[s][:], glu[s][:], sig[s][:], ALU.mult), reads=[('sglu', s), ('ssig', s)], writes=[('stg', s)])
                    S.op('dve', lambda e, s=s, c=c: e.scalar_tensor_tensor(actq[:, c, :], lin1[s][:], -6.0, tg[s][:], ALU.max, ALU.mult),
                         reads=[('slin', s), ('stg', s)], writes=[('actq', c)])
                actk = [('actq', c) for c in range(16)]
                for dc2 in range(8 if spcut >= 7 else 0):
                    ws = cnt_w[0] % 3
                    cnt_w[0] += 1
                    S.dma('pool', wb[ws][:], self.w2_s[ex, dc2], writes=[('wsp', ws)], max_dma_last_dim=4096)
                    dsl = slice(dc2 * CW, (dc2 + 1) * CW)
                    yb = ysb[dc2 % 2]
                    def fpy(e, ws=ws):
                        self.mm_group(e, py_t[:, 0, :], [(actq[:, ft, 0:P], wb[ws][:, ft, :]) for ft in range(KT)])
                        return self.mm_group(e, py_t[:, 1, :], [(actq[:, ft, P:2 * P], wb[ws][:, ft, :]) for ft in range(KT)])
                    S.op('pe', fpy, reads=[('wsp', ws)] + actk, writes=['pys'])
                    for sh in range(2):
                        S.op('dve', lambda e, sh=sh, yb=yb, dsl=dsl: e.tensor_tensor(yb[sh][:], py_t[:, sh, :], g2rep[:, dsl], ALU.mult),
                             reads=['pys'], writes=[('ysb', dc2 % 2, sh)])
                    for t in range(NT_OWN):
                        s = cnt_w[4] % 2
                        cnt_w[4] += 1
                        tsl = slice(t * P, (t + 1) * P)
                        S.op('pe', lambda e, s=s, tsl=tsl, yb=yb: self.mm_group(e, po[s], [(STq[:, 0, tsl], yb[0][:]), (STq[:, 1, tsl], yb[1][:])]),
                             reads=[('STq', 0), ('STq', 1), ('ysb', dc2 % 2, 0), ('ysb', dc2 % 2, 1)], writes=[('pos', s)])
                        S.op('dve', lambda e, s=s, t=t, dsl=dsl: e.tensor_tensor(acc[:, t, dsl], po[s], acc[:, t, dsl], ALU.add),
                             reads=[('pos', s)], writes=[('accs', t, dc2 // 4)])
                S.cond_end()

            for ex in range(self.NE if spcut >= 3 else 0):
                for src_t, srck, dst, dk_ in ((gatesT, [], grep, 'grep'), (posmT, posmT_keys, posrep, 'posrep')):
                    S.op('dve', lambda e, src_t=src_t, ex=ex: e.tensor_scalar(gsel[:], src_t[:], self.ident[0:32, ex:ex + 1], None, ALU.mult), reads=srck, writes=['gsel'])
                    for th in range(2):
                        S.op('pe', lambda e, th=th: e.matmul(pGR[:], ones32[:], gsel[:, th * 512:(th + 1) * 512], start=True, stop=True),
                             reads=['gsel', 'ones32'], writes=['pGR'])
                        S.op('dve', lambda e, th=th, dst=dst: e.tensor_copy(dst[:, th * 512:(th + 1) * 512], pGR[:]), reads=['pGR'], writes=[dk_])
                for q in range(NQ if spcut >= 4 else 0):
                    unit(ex, q)
            dk = []
            if self.debug:
                for t in range(NT_OWN):
                    S.dma('sp', self.dbg_acc2[:, t * D:(t + 1) * D], acc[:, t, :], reads=[('accs', t, 0), ('accs', t, 1)], writes=[('dbgacc2', t)])
                    dk.append(('dbgacc2', t))
            S.emit(final_wait_keys=dk)

    def phase_final(self):
        nc = self.nc
        with contextlib.ExitStack() as st:
            sb = lambda n, s, d=F32: st.enter_context(nc.sbuf_tensor(n, s, d))
            S = self.S
            acc = self.acc
            ln2 = sb("ln2r", [P, 2, D])
            S.dma('sp', ln2[:, 0, :], self.ln2[0:1, :].partition_broadcast(P), writes=['ln2g'])
            S.dma('sp', ln2[:, 1, :], self.ln2[1:2, :].partition_broadcast(P), writes=['ln2b'])
            xo = [sb(f"xo{i}", [P, D]) for i in range(2)]
            stt = [sb(f"stt_f{i}", [P, 4, 6]) for i in range(2)]
            mv = [sb(f"mv_f{i}", [P, 6]) for i in range(2)]
            pT = [st.enter_context(nc.psum_tensor(f"pTf{i}", [P, 8, P], F32)) for i in range(3)]
            npt = 0
            dk = []
            for t in range(NT_OWN):
                s = t % 2
                if self.sparse:
                    S.op('act', lambda e, s=s, t=t: e.activation(xo[s][:], acc[:, t, :], AF.Copy), reads=[], writes=[('xo', s)])
                for hf in range(2 if not self.sparse else 0):
                    pi = npt % 3
                    npt += 1

                    def tr(e, hf=hf, pi=pi, t=t):
                        last = None
                        for k8 in range(8):
                            dt = hf * 8 + k8
                            last = e.transpose(pT[pi][:, k8, :], acc[:, t, dt * P:(dt + 1) * P], self.ident[:])
                        return last
                    S.op('pe', tr, reads=[], writes=[('pTf', pi)])
                    S.op('act', lambda e, s=s, hf=hf, pi=pi: e.activation(xo[s][:, hf * 1024:(hf + 1) * 1024].rearrange("p (k j) -> p k j", k=8), pT[pi][:], AF.Copy),
                         reads=[('pTf', pi)], writes=[('xo', s)])
                self.ln_stats(S, stt[s], mv[s], xo[s], ('xo', s), ('lnF', s))
                S.op('act', lambda e, s=s: e.activation(xo[s][:], xo[s][:], AF.Identity, bias=mv[s][:, 3:4], scale=mv[s][:, 2:3]),
                     reads=[('xo', s), (('lnF', s), 'nb'), (('lnF', s), 'rs')], writes=[('xo', s)])
                S.op('dve', lambda e, s=s: e.tensor_tensor(xo[s][:], xo[s][:], ln2[:, 0, :], ALU.mult), reads=[('xo', s), 'ln2g'], writes=[('xo', s)])
                S.op('dve', lambda e, s=s: e.tensor_tensor(xo[s][:], xo[s][:], ln2[:, 1, :], ALU.add), reads=[('xo', s), 'ln2b'], writes=[('xo', s)])
                S.dma('sp', self.y[t * P:(t + 1) * P, :], xo[s][:], reads=[('xo', s)], writes=[('y', t)])
                dk.append(('y', t))
            S.emit(final_wait_keys=dk)


def rope_tables():
    S = 2048
    t = np.arange(S)
    row = (t // 64 - (S // 64) // 2).astype(np.float32)
    col = (t % 64 - 32).astype(np.float32)
    inv = (10000.0 ** (-np.arange(0, 64, 2, dtype=np.float32) / 64.0)).astype(np.float32)
    ar = row[:, None] * inv[None, :]
    ac = col[:, None] * inv[None, :]
    cos = np.concatenate([np.cos(ar), np.cos(ar), np.cos(ac), np.cos(ac)], axis=1)
    sin = np.concatenate([-np.sin(ar), np.sin(ar), -np.sin(ac), np.sin(ac)], axis=1)
    return cos.astype(np.float32), sin.astype(np.float32)


def tile_w(w):
    K, N = w.shape
    return np.ascontiguousarray(w.reshape(KT, P, N // CW, CW).transpose(2, 1, 0, 3))


def prep_shared(inp, n_experts=32, sparse=None):
    l = 0
    NE = n_experts
    w1 = inp["w_exp_in"][l][:NE]
    w1 = np.ascontiguousarray(w1.reshape(NE, KT, P, 2, 16, P).transpose(0, 4, 2, 1, 3, 5)).reshape(NE, 16, P, KT, CW)
    w2 = inp["w_exp_out"][l][:NE]
    if sparse is not True:
        w2 = np.ascontiguousarray(w2.reshape(NE, 2, 8, P, 4, 512).transpose(0, 1, 4, 3, 2, 5))
    b1 = np.ascontiguousarray(inp["b_exp_in"][l].reshape(32, 32, P).transpose(2, 0, 1))
    w2s = inp["w_exp_out"][l][:NE]
    if sparse is not False:
        w2s = np.ascontiguousarray(w2s.reshape(NE, KT, P, 8, CW).transpose(0, 3, 2, 1, 4))
    out = {"w2_s": w2s, "w1_r": w1, "w2_r": w2, "b1_r": b1, "b2": np.ascontiguousarray(inp["b_exp_out"][l]),
            "ln2": np.ascontiguousarray(np.stack([inp["ln2_g"][l], inp["ln2_b"][l]]))}
    if sparse is True:
        del out["w2_r"]
    if sparse is False:
        del out["w2_s"]
    return out


def prep_core(inp, core):
    b, half = core // 2, core % 2
    l = 0
    x = inp["x"][b]
    cos, sin = rope_tables()
    if half == 1:
        x = x[::-1]
        cos, sin = cos[::-1], sin[::-1]
    w_in = inp["w_in"][l]
    m = {
        "x_loc": np.ascontiguousarray(x),
        "c_col": np.ascontiguousarray(inp["c"][b].reshape(KT, P).T),
        "w_ada1": np.ascontiguousarray(inp["w_ada"][l][:, 0:2 * D]),
        "b_ada1": np.ascontiguousarray(inp["b_ada"][l][None, 0:2 * D]),
        "ident": np.eye(P, dtype=np.float32),
        "w_qkv": tile_w(w_in[:, 0:1536]),
        "qkw": np.stack([inp["q_norm_w"][l] * np.float32(1.0), inp["k_norm_w"][l]]).astype(np.float32),
        "cs": np.ascontiguousarray(np.stack([cos, sin])),
        "anw": np.ascontiguousarray(inp["attn_norm_w"][l][None, :]),
    }
    o_qr, o_ffw, o_fbw, o_i, o_g = 1536, 2560, 3584, 4608, 5632
    o_fa, o_fb = (o_ffw, o_fbw) if half == 0 else (o_fbw, o_ffw)
    cols = []
    for hh in range(8):
        sl = lambda o: w_in[:, o + hh * P:o + (hh + 1) * P]
        cols += [sl(o_qr), sl(o_fa), sl(o_fb), sl(o_i), sl(o_g), sl(o_g)]
    m["w_hg"] = tile_w(np.concatenate(cols, axis=1)).reshape(8, 3, P, KT, CW)
    lbr = inp["hgrn_lb"]
    dirs = (0, 1) if half == 0 else (1, 0)
    la = np.stack([np.concatenate([lbr[dirs[0], sl_].reshape(8, P).T, lbr[dirs[1], sl_].reshape(8, P).T], axis=1) for sl_ in range(2)])
    m["lb_a"] = np.ascontiguousarray(la.astype(np.float32))
    m["hnw"] = np.ascontiguousarray(inp["hgrn_norm_w"][l][None, :])
    jj, ii = np.meshgrid(np.arange(P), np.arange(P), indexing="ij")
    same = (jj // 64) == (ii // 64)
    m["hmask"] = np.stack([(same & (jj <= ii)), (same & (jj >= ii))]).astype(np.float32)
    m["rmask"] = (np.arange(S_OWN) % 64 != 0).astype(np.float32)[None, :]
    w_ada, b_ada = inp["w_ada"][l], inp["b_ada"][l]
    m["w_ada2"] = np.ascontiguousarray(w_ada[:, 2 * D:5 * D])
    m["b_ada2"] = np.ascontiguousarray(b_ada[None, 2 * D:5 * D])
    m["w_ada3"] = np.ascontiguousarray(w_ada[:, 5 * D:6 * D])
    m["b_g2col"] = np.ascontiguousarray(b_ada[5 * D:6 * D].reshape(KT, P).T)
    m["w_out_t"] = tile_w(inp["w_out"][l])
    m["ln1"] = np.ascontiguousarray(np.stack([inp["ln1_g"][l], inp["ln1_b"][l]]))
    m["w_r"] = np.ascontiguousarray(inp["w_router"][l].reshape(KT, P, 32).transpose(1, 0, 2))
    m["b_r"] = np.ascontiguousarray(inp["b_router"][l][None, :])
    m["b_ada3"] = np.ascontiguousarray(b_ada[None, 5 * D:6 * D])
    kk, mm_ = np.meshgrid(np.arange(P), np.arange(P), indexing="ij")
    m["utri"] = (kk < mm_).astype(np.float32)
    m["iota"] = np.arange(S_OWN, dtype=np.float32)[None, :]
    m["slotidx"] = (np.arange(P)[:, None] + 128 * np.arange(8)[None, :]).astype(np.float32)
    m["halfm"] = np.stack([(np.arange(P) < 64), (np.arange(P) >= 64)], axis=1).astype(np.float32)
    return m


def kernel(**inputs):
    inp = {k: np.asarray(v) for k, v in inputs.items()}
    n = 8
    prog = Prog(['attn', 'hgrn', 'c', 'moe_sp'], debug=False)
    shared = prep_shared(inp, sparse=True)
    in_maps = []
    for c in range(n):
        m = prep_core(inp, c)
        m.update(shared)
        in_maps.append(m)
    res = run_bass_kernel_spmd(prog.nc, in_maps, core_ids=list(range(n)))
    out = np.zeros((4, 2048, 2048), np.float32)
    for c in range(n):
        b, half = c // 2, c % 2
        y = np.asarray(res.results[c]["y_out"], dtype=np.float32)
        if half == 0:
            out[b, 0:1024] = y
        else:
            out[b, 1024:2048] = y[::-1]
    return out
```
